# Optimizing a Trainium2 kernel written in Bass

```python
import math
import jax, jax.numpy as jnp
from jax import lax
import numpy as np

D_MODEL = 1024
BATCH = 8
SEQ = 8192
DEPTH = 1

HEAD_DIM = 64
ROT_DIM = HEAD_DIM // 4
ROPE_THETA = 500000.0
NSA_HEADS = 8
NSA_KV_GROUPS = 2
NSA_HPG = NSA_HEADS // NSA_KV_GROUPS
CMP_BLOCK = 32
CMP_STRIDE = 16
CMP_HIDDEN = 256
SLC_BLOCK = 64
SLC_TOPK = 16
WINDOW = 512
NSA_QBLOCK = 64
FORCED_SCORE = 1e4
HGRN_HEADS = 4
HGRN_DK = 64
HGRN_DV = 64
HGRN_CHUNK = 64
MEM_HEADS = 4
MEM_TOKENS = 256
NSA_WIDTH = NSA_HEADS * HEAD_DIM
NSA_KV_WIDTH = NSA_KV_GROUPS * HEAD_DIM
HGRN_WIDTH = HGRN_HEADS * HGRN_DK
MEM_WIDTH = MEM_HEADS * HEAD_DIM
MIX_WIDTH = NSA_WIDTH + HGRN_WIDTH + MEM_WIDTH
IN_SIZES = (NSA_WIDTH, NSA_KV_WIDTH, NSA_KV_WIDTH, NSA_KV_WIDTH, NSA_KV_WIDTH, NSA_KV_WIDTH, NSA_KV_WIDTH,
            3 * NSA_HEADS, HGRN_WIDTH, HGRN_WIDTH, HGRN_WIDTH, HGRN_WIDTH, MEM_WIDTH)
IN_WIDTH = sum(IN_SIZES)
D_FF = 2816
EPS = 1e-6
MASK_VALUE = -1e30

kernel_name = "hybrid_nsa_hgrn2_memory_macaron"


def split_points():
    pts, acc = [], 0
    for s in IN_SIZES[:-1]:
        acc += s
        pts.append(acc)
    return pts


def rms_norm(x, gain):
    xf = x.astype(jnp.float32)
    y = xf * lax.rsqrt(jnp.mean(xf * xf, axis=-1, keepdims=True) + EPS)
    return (y * gain.astype(jnp.float32)).astype(x.dtype)


def swiglu(x, w_gate, w_up, w_down):
    return (jax.nn.silu(x @ w_gate) * (x @ w_up)) @ w_down


def masked_softmax(s, mask):
    s = jnp.where(mask, s.astype(jnp.float32), MASK_VALUE)
    p = jax.nn.softmax(s, axis=-1)
    return jnp.where(mask, p, 0.0)


def rope_tables(seq):
    pos = jnp.arange(seq, dtype=jnp.float32)
    inv = ROPE_THETA ** (-(jnp.arange(0, ROT_DIM, 2, dtype=jnp.float32) / ROT_DIM))
    ang = pos[:, None] * inv[None, :]
    return jnp.cos(ang), jnp.sin(ang)


def partial_rope(x, cos, sin):
    half = ROT_DIM // 2
    x1 = x[..., :half].astype(jnp.float32)
    x2 = x[..., half:ROT_DIM].astype(jnp.float32)
    c = cos[:, None, :]
    s = sin[:, None, :]
    r = jnp.concatenate([x1 * c - x2 * s, x2 * c + x1 * s], axis=-1).astype(x.dtype)
    return jnp.concatenate([r, x[..., ROT_DIM:]], axis=-1)


def compress_blocks(t, pos_emb, w1, w2):
    B, S, G, dh = t.shape
    n_cmp = (S - CMP_BLOCK) // CMP_STRIDE + 1
    idx = jnp.arange(n_cmp)[:, None] * CMP_STRIDE + jnp.arange(CMP_BLOCK)[None, :]
    blocks = t[:, idx] + pos_emb[:, None, :]
    blocks = blocks.transpose(0, 1, 3, 2, 4).reshape(B, n_cmp, G, CMP_BLOCK * dh)
    return jax.nn.silu(blocks @ w1) @ w2


def nsa_mixer(q, k_cmp, v_cmp, k_slc, v_slc, k_win, v_win, gates,
              cmp_pos_k, cmp_w1_k, cmp_w2_k, cmp_pos_v, cmp_w1_v, cmp_w2_v,
              q_gain, k_gain, cos, sin):
    B, S = q.shape[:2]
    G, HPG, dh, QB = NSA_KV_GROUPS, NSA_HPG, HEAD_DIM, NSA_QBLOCK
    scale = HEAD_DIM ** -0.5
    q = partial_rope(rms_norm(q, q_gain), cos, sin)
    k_slc = partial_rope(rms_norm(k_slc, k_gain), cos, sin)
    k_win = partial_rope(rms_norm(k_win, k_gain), cos, sin)
    kc = rms_norm(compress_blocks(partial_rope(k_cmp, cos, sin), cmp_pos_k, cmp_w1_k, cmp_w2_k), k_gain)
    vc = compress_blocks(v_cmp, cmp_pos_v, cmp_w1_v, cmp_w2_v)
    n_cmp = kc.shape[1]
    kc = kc.transpose(0, 2, 1, 3)
    vc = vc.transpose(0, 2, 1, 3)
    cmp_start = jnp.arange(n_cmp) * CMP_STRIDE
    cmp_end = cmp_start + CMP_BLOCK - 1
    n_slc = S // SLC_BLOCK
    top_k = min(SLC_TOPK, n_slc)
    slc_start = jnp.arange(n_slc) * SLC_BLOCK
    overlap = ((cmp_start[:, None] < slc_start[None, :] + SLC_BLOCK)
               & (cmp_start[:, None] + CMP_BLOCK > slc_start[None, :])).astype(jnp.float32)
    ks_blocks = k_slc.reshape(B, n_slc, SLC_BLOCK, G, dh).transpose(0, 3, 1, 2, 4)
    vs_blocks = v_slc.reshape(B, n_slc, SLC_BLOCK, G, dh).transpose(0, 3, 1, 2, 4)
    kw_pad = jnp.pad(k_win, ((0, 0), (WINDOW, 0), (0, 0), (0, 0)))
    vw_pad = jnp.pad(v_win, ((0, 0), (WINDOW, 0), (0, 0), (0, 0)))
    qg = q.reshape(B, S, G, HPG, dh)
    b_ix = jnp.arange(B)[:, None, None, None]
    g_ix = jnp.arange(G)[None, :, None, None]
    blk_j = jnp.arange(n_slc)

    def block(i):
        s0 = i * QB
        t = s0 + jnp.arange(QB)
        qb = lax.dynamic_slice_in_dim(qg, s0, QB, axis=1).transpose(0, 2, 3, 1, 4)
        sc = jnp.einsum('bghqd,bgnd->bghqn', qb, kc) * scale
        pc = masked_softmax(sc, cmp_end[None, :] <= t[:, None])
        o_cmp = jnp.einsum('bghqn,bgnd->bghqd', pc.astype(vc.dtype), vc)
        imp = jnp.einsum('bghqn,nj->bgqj', pc, overlap)
        cur = t // SLC_BLOCK
        valid = blk_j[None, :] <= cur[:, None]
        forced = (blk_j[None, :] == 0) | (blk_j[None, :] == cur[:, None]) | (blk_j[None, :] == cur[:, None] - 1)
        imp = jnp.where(valid, jnp.where(forced, FORCED_SCORE, imp), -1.0)
        top_val, top_idx = lax.top_k(imp, top_k)
        ks = ks_blocks[b_ix, g_ix, top_idx]
        vs = vs_blocks[b_ix, g_ix, top_idx]
        key_pos = top_idx[..., None] * SLC_BLOCK + jnp.arange(SLC_BLOCK)
        mask_s = (key_pos <= t[None, None, :, None, None]) & (top_val >= 0.0)[..., None]
        ss = jnp.einsum('bghqd,bgqksd->bghqks', qb, ks) * scale
        ss = ss.reshape(B, G, HPG, QB, top_k * SLC_BLOCK)
        ps = masked_softmax(ss, mask_s.reshape(B, G, 1, QB, top_k * SLC_BLOCK))
        o_slc = jnp.einsum('bghqn,bgqnd->bghqd', ps.astype(vs.dtype),
                           vs.reshape(B, G, QB, top_k * SLC_BLOCK, dh))
        kw = lax.dynamic_slice_in_dim(kw_pad, s0, WINDOW + QB, axis=1).transpose(0, 2, 1, 3)
        vw = lax.dynamic_slice_in_dim(vw_pad, s0, WINDOW + QB, axis=1).transpose(0, 2, 1, 3)
        wpos = s0 - WINDOW + jnp.arange(WINDOW + QB)
        diff = t[:, None] - wpos[None, :]
        mask_w = (diff >= 0) & (diff < WINDOW) & (wpos[None, :] >= 0)
        sw = jnp.einsum('bghqd,bgkd->bghqk', qb, kw) * scale
        pw = masked_softmax(sw, mask_w)
        o_win = jnp.einsum('bghqk,bgkd->bghqd', pw.astype(vw.dtype), vw)
        gb = lax.dynamic_slice_in_dim(gates, s0, QB, axis=1)
        gb = gb.reshape(B, QB, G, HPG, 3).transpose(0, 2, 3, 1, 4)
        out = gb[..., 0:1] * o_cmp + gb[..., 1:2] * o_slc + gb[..., 2:3] * o_win
        return out.transpose(0, 3, 1, 2, 4).reshape(B, QB, NSA_WIDTH)

    outs = lax.map(block, jnp.arange(S // QB))
    return outs.transpose(1, 0, 2, 3).reshape(B, S, NSA_WIDTH)


def hgrn2_mixer(q, f, i, g, lower_bound, out_gain):
    B, S, _ = q.shape
    H, dk, dv, C = HGRN_HEADS, HGRN_DK, HGRN_DV, HGRN_CHUNK
    nC = S // C
    scale = dk ** -0.5
    lb = lower_bound.astype(jnp.float32)
    fgate = lb + (1.0 - lb) * jax.nn.sigmoid(f.astype(jnp.float32))
    log_f = jnp.log(fgate)
    k = 1.0 - fgate
    qa = jax.nn.silu(q.astype(jnp.float32)) * scale

    def to_chunks(a, d):
        return a.reshape(B, nC, C, H, d).transpose(1, 0, 3, 2, 4)

    xs = (to_chunks(qa, dk), to_chunks(k, dk), to_chunks(i.astype(jnp.float32), dv), to_chunks(log_f, dk))
    causal = jnp.tril(jnp.ones((C, C), dtype=bool))

    def step(state, inp):
        qc, kc, vc, lc = inp
        b = jnp.cumsum(lc, axis=2)
        inter = jnp.einsum('bhtk,bhkv->bhtv', qc * jnp.exp(b), state)
        diff = b[:, :, :, None, :] - b[:, :, None, :, :]
        decay = jnp.exp(jnp.where(causal[:, :, None], diff, MASK_VALUE))
        attn = jnp.einsum('bhtk,bhsk,bhtsk->bhts', qc, kc, decay)
        intra = jnp.einsum('bhts,bhsv->bhtv', attn, vc)
        b_last = b[:, :, -1:, :]
        new_state = jnp.exp(b_last[:, :, 0, :])[..., None] * state + \
            jnp.einsum('bhsk,bhsv->bhkv', kc * jnp.exp(b_last - b), vc)
        return new_state, inter + intra

    state0 = jnp.zeros((B, H, dk, dv), jnp.float32)
    _, o = lax.scan(step, state0, xs)
    o = o.transpose(1, 0, 3, 2, 4).reshape(B, S, H, dv)
    o = rms_norm(o, out_gain.reshape(H, dv)) * jax.nn.silu(g.astype(jnp.float32).reshape(B, S, H, dv))
    return o.reshape(B, S, HGRN_WIDTH).astype(q.dtype)


def memory_mixer(q, mem_n, w_k, w_v, q_gain, k_gain):
    B, S, _ = q.shape
    M = mem_n.shape[1]
    qh = rms_norm(q.reshape(B, S, MEM_HEADS, HEAD_DIM), q_gain)
    kh = rms_norm((mem_n @ w_k).reshape(B, M, MEM_HEADS, HEAD_DIM), k_gain)
    vh = (mem_n @ w_v).reshape(B, M, MEM_HEADS, HEAD_DIM)
    s = jnp.einsum('bshd,bmhd->bhsm', qh, kh).astype(jnp.float32) * (HEAD_DIM ** -0.5)
    p = jax.nn.softmax(s, axis=-1)
    o = jnp.einsum('bhsm,bmhd->bshd', p.astype(vh.dtype), vh)
    return o.reshape(B, S, MEM_WIDTH)


def setup_inputs(seed: int = 0) -> dict:
    key = jax.random.key(seed)
    ks = iter(jax.random.split(key, 40))

    def nrm(shape, scale):
        return jax.random.normal(next(ks), shape, jnp.float32) * scale

    def gain(shape):
        return 1.0 + 0.02 * jax.random.normal(next(ks), shape, jnp.float32)

    L = DEPTH
    return {
        "x": nrm((BATCH, SEQ, D_MODEL), 1.0),
        "mem": nrm((BATCH, MEM_TOKENS, D_MODEL), 1.0),
        "ffn1_norm": gain((L, D_MODEL)),
        "ffn1_w_gate": nrm((L, D_MODEL, D_FF), D_MODEL ** -0.5),
        "ffn1_w_up": nrm((L, D_MODEL, D_FF), D_MODEL ** -0.5),
        "ffn1_w_down": nrm((L, D_FF, D_MODEL), D_FF ** -0.5),
        "mix_norm": gain((L, D_MODEL)),
        "w_in": nrm((L, D_MODEL, IN_WIDTH), D_MODEL ** -0.5),
        "w_out": nrm((L, MIX_WIDTH, D_MODEL), MIX_WIDTH ** -0.5),
        "nsa_q_norm": gain((L, HEAD_DIM)),
        "nsa_k_norm": gain((L, HEAD_DIM)),
        "cmp_pos_k": nrm((L, CMP_BLOCK, HEAD_DIM), 0.02),
        "cmp_w1_k": nrm((L, CMP_BLOCK * HEAD_DIM, CMP_HIDDEN), (CMP_BLOCK * HEAD_DIM) ** -0.5),
        "cmp_w2_k": nrm((L, CMP_HIDDEN, HEAD_DIM), CMP_HIDDEN ** -0.5),
        "cmp_pos_v": nrm((L, CMP_BLOCK, HEAD_DIM), 0.02),
        "cmp_w1_v": nrm((L, CMP_BLOCK * HEAD_DIM, CMP_HIDDEN), (CMP_BLOCK * HEAD_DIM) ** -0.5),
        "cmp_w2_v": nrm((L, CMP_HIDDEN, HEAD_DIM), CMP_HIDDEN ** -0.5),
        "nsa_out_norm": gain((L, NSA_WIDTH)),
        "hgrn_lb_logits": nrm((L + 1, HGRN_WIDTH), 0.1),
        "hgrn_out_norm": gain((L, HGRN_WIDTH)),
        "mem_norm": gain((L, D_MODEL)),
        "mem_w_k": nrm((L, D_MODEL, MEM_WIDTH), D_MODEL ** -0.5),
        "mem_w_v": nrm((L, D_MODEL, MEM_WIDTH), D_MODEL ** -0.5),
        "mem_q_norm": gain((L, HEAD_DIM)),
        "mem_k_norm": gain((L, HEAD_DIM)),
        "mem_out_norm": gain((L, MEM_WIDTH)),
        "ffn2_norm": gain((L, D_MODEL)),
        "ffn2_w_gate": nrm((L, D_MODEL, D_FF), D_MODEL ** -0.5),
        "ffn2_w_up": nrm((L, D_MODEL, D_FF), D_MODEL ** -0.5),
        "ffn2_w_down": nrm((L, D_FF, D_MODEL), D_FF ** -0.5),
    }


def reference(x, mem, ffn1_norm, ffn1_w_gate, ffn1_w_up, ffn1_w_down, mix_norm, w_in, w_out,
              nsa_q_norm, nsa_k_norm, cmp_pos_k, cmp_w1_k, cmp_w2_k, cmp_pos_v, cmp_w1_v, cmp_w2_v,
              nsa_out_norm, hgrn_lb_logits, hgrn_out_norm,
              mem_norm, mem_w_k, mem_w_v, mem_q_norm, mem_k_norm, mem_out_norm,
              ffn2_norm, ffn2_w_gate, ffn2_w_up, ffn2_w_down):
    B, S, _ = x.shape
    cos, sin = rope_tables(S)
    lower_bounds = jnp.cumsum(jax.nn.softmax(hgrn_lb_logits.astype(jnp.float32), axis=0), axis=0)
    pts = split_points()
    for l in range(DEPTH):
        x = x + 0.5 * swiglu(rms_norm(x, ffn1_norm[l]), ffn1_w_gate[l], ffn1_w_up[l], ffn1_w_down[l])
        h = rms_norm(x, mix_norm[l])
        proj = h @ w_in[l]
        (q_a, k_c, v_c, k_s, v_s, k_w, v_w, g_a,
         q_h, f_h, i_h, g_h, q_m) = jnp.split(proj, pts, axis=-1)
        kv = lambda t: t.reshape(B, S, NSA_KV_GROUPS, HEAD_DIM)
        y_nsa = nsa_mixer(q_a.reshape(B, S, NSA_HEADS, HEAD_DIM), kv(k_c), kv(v_c), kv(k_s), kv(v_s),
                          kv(k_w), kv(v_w), jax.nn.sigmoid(g_a).reshape(B, S, NSA_HEADS, 3),
                          cmp_pos_k[l], cmp_w1_k[l], cmp_w2_k[l], cmp_pos_v[l], cmp_w1_v[l], cmp_w2_v[l],
                          nsa_q_norm[l], nsa_k_norm[l], cos, sin)
        y_hgrn = hgrn2_mixer(q_h, f_h, i_h, g_h, lower_bounds[l], hgrn_out_norm[l])
        mem_n = rms_norm(mem, mem_norm[l])
        y_mem = memory_mixer(q_m, mem_n, mem_w_k[l], mem_w_v[l], mem_q_norm[l], mem_k_norm[l])
        mixed = jnp.concatenate([rms_norm(y_nsa, nsa_out_norm[l]), y_hgrn.astype(x.dtype),
                                 rms_norm(y_mem, mem_out_norm[l])], axis=-1)
        x = x + mixed @ w_out[l]
        x = x + 0.5 * swiglu(rms_norm(x, ffn2_norm[l]), ffn2_w_gate[l], ffn2_w_up[l], ffn2_w_down[l])
    return x
```

```python
import numpy as np
from contextlib import ExitStack
import concourse.bass as bass
import concourse.mybir as mybir
from concourse.bass_utils import run_bass_kernel_spmd

F32 = mybir.dt.float32
BF16 = mybir.dt.bfloat16
AF = mybir.ActivationFunctionType
ALU = mybir.AluOpType
AX = mybir.AxisListType

D = 1024
DFF = 2816
NFC = 22
T = 512
INW = 2584
NEG = -30000.0
EPS = 1e-6
GROUPS = [(0, 512), (512, 512), (1024, 280), (1304, 512), (1816, 512), (2328, 256)]


class Op:
    __slots__ = ("eng", "idx", "fn", "deps", "signal", "sigval", "dma", "ndma", "dmaval", "name", "force")


class Sched:
    SAME_ENG_DIST = 4

    def __init__(self, nc):
        self.nc = nc
        self.engs = {"pe": nc.tensor, "act": nc.scalar, "dve": nc.vector, "pool": nc.gpsimd, "sp": nc.sync}
        self.ops = {e: [] for e in self.engs}
        self.last_w = {}
        self.rd_eng = {}
        self.rd_dma = {}
        self.dma_count = {}
        self.dma_last = {}
        self.pending_barrier = {}
        self.out_dma_ops = []

    def add(self, eng, fn, r=(), w=(), dma=None, ndma=1, name=None, is_out=False, force=False):
        op = Op()
        op.force = force
        op.eng = eng
        op.idx = len(self.ops[eng])
        op.fn = fn
        op.signal = False
        op.sigval = None
        op.dma = dma
        op.ndma = ndma
        op.dmaval = None
        op.name = name
        w = list(w) + [k for k in r if k.startswith("ps")]
        r = [k for k in r if not k.startswith("ps")]
        deps = []
        for k in r:
            p = self.last_w.get(k)
            if p is not None:
                deps.append(p)
        for k in w:
            p = self.last_w.get(k)
            if p is not None:
                deps.append(p)
            d = self.rd_eng.get(k)
            if d:
                deps.extend(d.values())
            d = self.rd_dma.get(k)
            if d:
                deps.extend(d)
        if eng in self.pending_barrier:
            deps.extend(self.pending_barrier.pop(eng))
        op.deps = self._filter(op, deps)
        if dma is not None:
            c = self.dma_count.get(dma, 0) + ndma
            self.dma_count[dma] = c
            op.dmaval = 16 * c
            self.dma_last[dma] = op
            if is_out:
                self.out_dma_ops.append(op)
        for k in r:
            if dma is not None:
                self.rd_dma.setdefault(k, []).append(op)
            else:
                self.rd_eng.setdefault(k, {})[eng] = op
        for k in w:
            self.last_w[k] = op
            self.rd_eng[k] = {}
            self.rd_dma[k] = []
        self.ops[eng].append(op)
        return op

    def _filter(self, op, deps):
        best_eng = {}
        best_dma = {}
        for p in deps:
            if p is op:
                continue
            if p.dma is not None:
                q = best_dma.get(p.dma)
                if q is None or p.dmaval > q.dmaval:
                    best_dma[p.dma] = p
                continue
            if p.eng == op.eng and op.dma is None:
                if op.eng == "pe" and not op.force:
                    continue
                if op.idx - p.idx > self.SAME_ENG_DIST:
                    continue
            q = best_eng.get(p.eng)
            if q is None or p.idx > q.idx:
                best_eng[p.eng] = p
        out = list(best_eng.values()) + list(best_dma.values())
        for p in out:
            if p.dma is None:
                p.signal = True
        return out

    def barrier(self):
        snap = []
        for e, lst in self.ops.items():
            for p in reversed(lst):
                if p.dma is None and p.fn is not None:
                    snap.append(p)
                    break
        snap.extend(self.dma_last.values())
        for e in self.engs:
            self.pending_barrier[e] = list(snap)

    def finish(self):
        op = Op()
        op.eng = "sp"
        op.idx = len(self.ops["sp"])
        op.fn = None
        op.signal = False
        op.dma = None
        op.ndma = 0
        op.dmaval = None
        op.name = "final"
        op.force = False
        op.sigval = None
        op.deps = self._filter(op, list(self.out_dma_ops))
        self.ops["sp"].append(op)

    def emit(self, stack):
        nc = self.nc
        for e, lst in self.ops.items():
            c = 0
            for p in lst:
                if p.signal:
                    c += 1
                    p.sigval = c
        esem = {e: stack.enter_context(nc.semaphore("s_" + e)) for e in self.engs if e != "sp"}
        dsem = {k: stack.enter_context(nc.semaphore("d_%d" % i)) for i, k in enumerate(self.dma_count)}

        def body(ename):
            def f(eh):
                wm = {}
                for p in self.ops[ename]:
                    for d in p.deps:
                        if d.dma is not None:
                            sem, val = dsem[d.dma], d.dmaval
                        else:
                            sem, val = esem[d.eng], d.sigval
                        key = id(sem)
                        if wm.get(key, 0) >= val:
                            continue
                        wm[key] = val
                        eh.wait_ge(sem, val)
                    if p.fn is None:
                        continue
                    ins = p.fn(eh)
                    if p.dma is not None:
                        if not isinstance(ins, (list, tuple)):
                            ins = [ins]
                        assert len(ins) == p.ndma, (p.name, len(ins), p.ndma)
                        for i_ in ins:
                            i_.then_inc(dsem[p.dma], 16)
                    elif p.signal:
                        if isinstance(ins, (list, tuple)):
                            ins = ins[-1]
                        ins.then_inc(esem[ename], 1)
            return f

        with nc.Block() as blk:
            blk.sync(body("sp"))
            blk.tensor(body("pe"))
            blk.scalar(body("act"))
            blk.vector(body("dve"))
            blk.gpsimd(body("pool"))


def host_consts(S):
    NT = S // T
    c = {}
    pos = np.arange(S, dtype=np.float32)
    inv = (np.float32(500000.0) ** (-(np.arange(0, 16, 2, dtype=np.float32) / np.float32(16)))).astype(np.float32)
    ang = (pos[:, None] * inv[None, :]).astype(np.float32)
    c["c_cos"] = np.cos(ang).astype(np.float32)
    c["c_sin"] = np.sin(ang).astype(np.float32)
    c["c_ident"] = np.eye(128, dtype=np.float32)
    p = np.arange(128)
    same = (p[:, None] // 64) == (p[None, :] // 64)
    c["c_tri"] = (same & (p[:, None] <= p[None, :])).astype(np.float32)
    c["c_upp"] = (same & (p[:, None] > p[None, :])).astype(np.float32)
    c["c_hmask"] = ((p[:, None] % 64) <= np.arange(64)[None, :]).astype(np.float32)
    kk = p[:, None]
    qq = p[None, :]
    c["c_caus"] = np.where(kk <= qq, 0.0, NEG).astype(np.float32)
    c["c_winb"] = np.where(qq < kk, 0.0, NEG).astype(np.float32)
    r = p[:, None, None]
    v = np.arange(4)[None, :, None]
    q = np.arange(512)[None, None, :]
    c["c_cmpb"] = np.where(16 * r + 15 <= 512 * v + q, 0.0, NEG).astype(np.float32)
    npr = np.arange(512)
    n = npr - 1
    j = np.arange(128)
    ovl = ((16 * n[:, None] < 64 * j[None, :] + 64) & (16 * n[:, None] + 32 > 64 * j[None, :])).astype(np.float32)
    ova = np.zeros((512, 130), np.float32)
    ova[:, :128] = ovl
    ova[:, 128] = 1.0
    ova[0, :] = 0.0
    c["c_ovl"] = ova.reshape(4, 128, 130).transpose(1, 0, 2).copy()
    m = np.arange(256)[None, :]
    hq = (p[:, None] >= 64).astype(np.int64)
    d = m - 126 - hq
    c["c_keepw"] = (d <= -2).astype(np.float32)
    c["c_addw"] = (np.where(d == 0, 2e4, 0.0) + np.where(d == -1, 4e4, 0.0) + np.where(d > 0, -1.0, 0.0)).astype(np.float32)
    jp = np.arange(32)[:, None, None]
    cc = np.arange(16)[None, :, None]
    k = np.arange(128)[None, None, :]
    c["c_esel"] = (jp == 2 * cc + k // 64).astype(np.float32)
    rr = np.arange(24)[:, None, None]
    hb = np.arange(24)[None, :, None]
    c["c_selg"] = np.broadcast_to((rr == hb), (24, 24, 64)).astype(np.float32).copy()
    c["c_bd64"] = (same.astype(np.float32) / 64.0).astype(np.float32)
    return c


CONST_SHAPES = None


def build(S, dbg=None):
    NT = S // T
    NCH = S // 128
    nc = bass.Bass("TRN2", target_bir_lowering=False)
    st = ExitStack()
    Sd = Sched(nc)

    def dram_in(name, shape, dt=F32):
        return nc.dram_tensor(name, list(shape), dt, kind="ExternalInput")

    def dram_scr(name, shape, dt=BF16):
        return nc.dram_tensor(name, list(shape), dt, kind="Internal")

    x_d = dram_in("x", [S, D])
    mem_d = dram_in("mem", [256, D])
    out_d = nc.dram_tensor("out", [S, D], F32, kind="ExternalOutput")
    wnames = {
        "ffn1_w_gate": [D, DFF], "ffn1_w_up": [D, DFF], "ffn1_w_down": [DFF, D],
        "ffn2_w_gate": [D, DFF], "ffn2_w_up": [D, DFF], "ffn2_w_down": [DFF, D],
        "w_in": [D, INW], "w_out": [D, D], "cmp_w1_k": [2048, 256], "cmp_w1_v": [2048, 256],
        "mem_w_k": [D, 256], "mem_w_v": [D, 256],
    }
    wd_ = {k: dram_in(k, v) for k, v in wnames.items() if k != "w_in"}
    for gi_, (c0_, gw_) in enumerate(GROUPS):
        wd_["w_in_g%d" % gi_] = dram_in("w_in_g%d" % gi_, [D, gw_])
    ws_ = {k: dram_scr("s_" + k, [v[0] * v[1]]) for k, v in wnames.items()}
    ws_in = [dram_scr("s_w_in_g%d" % gi_, [1024 * 512]) for gi_, (c0_, gw_) in enumerate(GROUPS)]
    vecs = {
        "ffn1_norm": D, "mix_norm": D, "ffn2_norm": D, "mem_norm": D, "nsa_q_norm": 64, "nsa_k_norm": 64,
        "nsa_out_norm": 512, "hgrn_out_norm": 256, "mem_q_norm": 64, "mem_k_norm": 64, "mem_out_norm": 256,
    }
    vd_ = {k: dram_in(k, [v]) for k, v in vecs.items()}
    lb_d = dram_in("hgrn_lb_logits", [2, 256])
    posk_d = dram_in("cmp_pos_k", [32, 64])
    posv_d = dram_in("cmp_pos_v", [32, 64])
    w2k_d = dram_in("cmp_w2_k", [256, 64])
    w2v_d = dram_in("cmp_w2_v", [256, 64])
    hc = host_consts(512)
    cshape = {k: v.shape for k, v in hc.items()}
    cshape["c_cos"] = (S, 8)
    cshape["c_sin"] = (S, 8)
    cd_ = {k: dram_in(k, v) for k, v in cshape.items()}
    dbg_d = {}
    if dbg:
        for k, shp in dbg.items():
            if k.startswith("_"):
                continue
            dbg_d[k] = nc.dram_tensor("dbg_" + k, list(shp), F32, kind="ExternalOutput")

    def DAP(t, off, pat):
        return bass.AP(t, off, [list(x) for x in pat])

    def sbt(name, shape, dt):
        return st.enter_context(nc.sbuf_tensor(name, list(shape), dt))

    psum = [st.enter_context(nc.psum_tensor("ps%d" % i, [128, 512], F32)) for i in range(8)]
    psum_b = [p.bitcast(BF16) for p in psum]
    pa_ctr = [0]
    pb_ctr = [0]

    def PA():
        i = pa_ctr[0] % 5
        pa_ctr[0] += 1
        return i, "ps%d" % i

    def PB():
        i = 5 + pb_ctr[0] % 3
        pb_ctr[0] += 1
        return i, "ps%d" % i

    def A(eng, meth, r=(), w=(), **kw):
        return Sd.add(eng, lambda e, kw=kw, meth=meth: getattr(e, meth)(**kw), r=r, w=w)

    def MM(out, lhsT, rhs, start, stop, r, w, force=False, **kw):
        return Sd.add("pe", lambda e: e.matmul(out, lhsT=lhsT, rhs=rhs, start=start, stop=stop, **kw), r=r, w=w, force=force)

    def TR(out, in_, ident, r, w):
        return Sd.add("pe", lambda e: e.transpose(out=out, in_=in_, identity=ident), r=r, w=w)

    def DMA(out, in_, r, w, key, eng="sp", is_out=False):
        return Sd.add(eng, lambda e: e.dma_start(out=out, in_=in_), r=r, w=w, dma=key, is_out=is_out)

    def ACT(out, in_, func, r, w, **kw):
        return Sd.add("act", lambda e: e.activation(out=out, in_=in_, func=func, **kw), r=r, w=w)

    rr_ctr = [0]

    def any_copy(out, in_, r, w, psum_src=False):
        engs = ["dve", "act"] if psum_src else ["dve", "act", "pool"]
        e = engs[rr_ctr[0] % len(engs)]
        rr_ctr[0] += 1
        if e == "act":
            return ACT(out, in_, AF.Copy, r, w)
        return A(e, "tensor_copy", r=r, w=w, out=out, in_=in_)

    with ExitStack() as pst:
        stg_f = [pst.enter_context(nc.sbuf_tensor("stgf%d" % i, [128, DFF], F32)) for i in range(2)]
        stg_b = [pst.enter_context(nc.sbuf_tensor("stgb%d" % i, [128, DFF], BF16)) for i in range(2)]
        cnt = [0]

        def cast_rows(name, rows, C, writer, wkey=None):
            for rc in range(rows // 128):
                s = cnt[0] % 2
                cnt[0] += 1
                src = DAP(wd_[name], rc * 128 * C, [[C, 128], [1, C]])
                DMA(stg_f[s][:, 0:C], src, r=[], w=["stgf%d" % s], key="stgf%d" % s)
                any_copy(stg_b[s][:, 0:C], stg_f[s][:, 0:C], r=["stgf%d" % s], w=["stgb%d" % s])
                outs = writer(rc, stg_b[s])
                Sd.add("sp", lambda e, outs=outs: [e.dma_start(out=o, in_=i) for (o, i) in outs],
                       r=["stgb%d" % s], w=[wkey or ("scr_" + name)], dma="stgo%d" % s, ndma=len(outs))

        def w_gu(name):
            def wr(dc, sb_):
                o = DAP(ws_[name], dc * 128, [[1024, 128], [131072, NFC], [1, 128]])
                return [(o, sb_[:, 0:DFF].rearrange("p (a b) -> p a b", b=128))]
            return wr

        def w_plain(name, C):
            def wr(rc, sb_):
                o = DAP(ws_[name], rc * 128 * C, [[C, 128], [1, C]])
                return [(o, sb_[:, 0:C])]
            return wr

        def w_in_wr(dc, sb_):
            res = []
            for gi_, (c0, gw) in enumerate(GROUPS):
                o = DAP(ws_in[gi_], dc * gw, [[8 * gw, 128], [1, gw]])
                res.append((o, sb_[:, c0:c0 + gw]))
            return res

        for f in ("ffn1", "ffn2"):
            cast_rows(f + "_w_gate", D, DFF, w_gu(f + "_w_gate"))
            cast_rows(f + "_w_up", D, DFF, w_gu(f + "_w_up"))
            cast_rows(f + "_w_down", DFF, D, w_plain(f + "_w_down", D))
        for gi_, (c0_, gw_) in enumerate(GROUPS):
            def wr_g(dc, sb_, gi_=gi_, gw_=gw_):
                return [(DAP(ws_in[gi_], dc * gw_, [[8 * gw_, 128], [1, gw_]]), sb_[:, 0:gw_])]
            cast_rows("w_in_g%d" % gi_, D, gw_, wr_g, wkey="scr_w_in")
        cast_rows("w_out", D, D, w_plain("w_out", D))
        for nm in ("cmp_w1_k", "cmp_w1_v", "mem_w_k", "mem_w_v"):
            cast_rows(nm, wnames[nm][0], 256, w_plain(nm, 256))
    Sd.barrier()
    WKEYS = ["scr_" + k for k in wnames]
    if dbg and "_dumpg" in dbg:
        g3raw = nc.dram_tensor("dbg_g3raw", [128, 4096], BF16, kind="ExternalOutput")
        DMA(g3raw.ap(), DAP(ws_in[dbg["_dumpg"]], 0, [[4096, 128], [1, 4096]]), r=["scr_w_in"], w=[], key="dbg_g3raw", is_out=True)

    xt = sbt("xt", [128, 4, D], F32)
    hT = sbt("hT", [128, 8, T], BF16)
    actT = sbt("actT", [128, 11, T], BF16)
    wgu = [sbt("wgu%d" % i, [128, 2, 8, 128], BF16) for i in range(2)]
    wdb = [sbt("wdb%d" % i, [128, D], BF16) for i in range(2)]
    winb = sbt("winb", [128, 8, 512], BF16)
    KTs = sbt("KTs", [128, S], BF16)
    Vs = sbt("Vs", [128, NCH, 192], BF16)
    KTw = sbt("KTw", [128, 2, T], BF16)
    Vw = sbt("Vw", [128, 8, 192], BF16)
    KcT = sbt("KcT", [128, 512], BF16)
    Vc = sbt("Vc", [128, 4, 192], BF16)
    QT = sbt("QT", [128, 4, T], BF16)
    QmT = sbt("QmT", [128, 2, T], BF16)
    KmT = sbt("KmT", [128, 2, 256], BF16)
    Vm = sbt("Vm", [128, 2, 2, 192], BF16)
    NET = 6
    ETb = [sbt("ET%d" % i, [128, T], BF16) for i in range(NET)]
    YT = sbt("YT", [128, 4, T], F32)
    YM = sbt("YM", [128, 2, T], F32)
    tA = sbt("tA", [128, T], F32)
    tB = sbt("tB", [128, T], F32)
    tC = sbt("tC", [128, T], F32)
    tD = sbt("tD", [128, T], F32)
    tE = sbt("tE", [128, T], F32)
    bA = sbt("bA", [128, T], BF16)
    bB = sbt("bB", [128, T], BF16)
    bC = sbt("bC", [128, T], BF16)
    NMT = sbt("NMT", [32, 2, 4, T], BF16)
    impS = sbt("impS", [128, 2, 4, 128], F32)
    tk1 = sbt("tk1", [128, 128], F32)
    tk2 = sbt("tk2", [128, 128], F32)
    tkm = sbt("tkm", [128, 16], F32)
    negm = sbt("negm", [128, 128], BF16)
    sm1 = sbt("sm1", [128, 64], F32)
    sm2 = sbt("sm2", [128, 64], F32)
    sm3 = sbt("sm3", [128, 64], F32)
    Rk = [sbt("Rk%d" % g, [128, 530], BF16) for g in range(2)]
    Rv = [sbt("Rv%d" % g, [128, 530], BF16) for g in range(2)]
    hidT = sbt("hidT", [128, 2, 2, 2, 32], BF16)
    w2k = sbt("w2k", [128, 2, 64], BF16)
    w2v = sbt("w2v", [128, 2, 64], BF16)
    b1 = sbt("b1", [128, 4], F32)
    posS = sbt("posS", [128, 2, 16], F32)
    posSb = sbt("posSb", [128, 2, 16], BF16)
    HQ = sbt("HQ", [128, 2, T], BF16)
    HK = sbt("HK", [128, 2, T], BF16)
    HG = sbt("HG", [128, 2, T], BF16)
    VB = sbt("VB", [128, 4, 256], BF16)
    KHs = [sbt("KH%d" % i, [128, 256], BF16) for i in range(4)]
    STb = sbt("STb", [128, 9, 2, 64], BF16)
    ST = sbt("ST", [128, 2, 64], F32)
    AT = sbt("AT", [128, 4, 256], BF16)
    dec = sbt("dec", [128, 4, 8], F32)
    GT = sbt("GT", [24, T], BF16)
    gsig = sbt("gsig", [128, 24], BF16)
    ssq = sbt("ssq", [128, 16], F32)
    rstd = sbt("rstd", [128, 16], F32)
    cs_c = sbt("cs_c", [128, 4, 8], F32)
    cs_s = sbt("cs_s", [128, 4, 8], F32)
    f32c = {}
    for k in ("c_ident", "c_tri", "c_upp"):
        f32c[k] = sbt("sb_" + k, [128, 128], F32)
    hmask = sbt("hmask", [128, 64], F32)
    keepw = sbt("keepw", [128, 256], F32)
    addw = sbt("addw", [128, 256], F32)
    identb = sbt("identb", [128, 128], BF16)
    causb = sbt("causb", [128, 128], BF16)
    winbias = sbt("winbias", [128, 128], BF16)
    cmpb = sbt("cmpb", [128, 4, 512], BF16)
    ovl = sbt("ovl", [128, 4, 130], BF16)
    esel = sbt("esel", [32, 16, 128], BF16)
    selg = sbt("selg", [24, 24, 64], BF16)
    bd64 = sbt("bd64", [128, 128], BF16)
    onesb = sbt("onesb", [128, 128], BF16)
    onesf = sbt("onesf", [128, 2], F32)
    gcol = sbt("gcol", [128, 4, 8], F32)
    gq = sbt("gq", [128, 64], F32)
    gk = sbt("gk", [128, 64], F32)
    gmq = sbt("gmq", [128, 64], F32)
    gmk = sbt("gmk", [128, 64], F32)
    gkcol = sbt("gkcol", [128, 1], F32)
    gnsa = sbt("gnsa", [128, 4], F32)
    ghg = sbt("ghg", [128, 2], F32)
    gmo = sbt("gmo", [128, 2], F32)
    LBt = sbt("LBt", [128, 2, 256], F32)
    LB0 = sbt("LB0", [128, 256], F32)
    LB1 = sbt("LB1", [128, 256], F32)

    stage_ctr = [0]

    def load_const(dst, src_ap, key, eng="sp"):
        DMA(dst, src_ap, r=[], w=[key], key="c_" + key, eng=eng)

    def load_const_bf(dst_bf, name, shape_free, key):
        npart = cshape[name][0]
        nfree = int(np.prod(cshape[name][1:]))
        src = DAP(cd_[name], 0, [[nfree, npart], [1, nfree]])
        DMA(tA[0:npart, 0:nfree] if nfree <= T else None, src, r=[], w=["tA"], key="c_stage")
        A("dve", "tensor_copy", r=["tA"], w=[key], out=dst_bf, in_=tA[0:npart, 0:nfree])

    for k in ("c_ident", "c_tri", "c_upp"):
        load_const(f32c[k][:], cd_[k].ap(), k)
    load_const(hmask[:], cd_["c_hmask"].ap(), "hmask")
    load_const(keepw[:], cd_["c_keepw"].ap(), "keepw")
    load_const(addw[:], cd_["c_addw"].ap(), "addw")
    A("dve", "tensor_copy", r=["c_ident"], w=["identb"], out=identb[:], in_=f32c["c_ident"][:])
    load_const_bf(causb[:], "c_caus", None, "causb")
    load_const_bf(winbias[:], "c_winb", None, "winbias")
    load_const_bf(bd64[:], "c_bd64", None, "bd64")
    for v in range(4):
        src = DAP(cd_["c_cmpb"], v * 512, [[2048, 128], [1, 512]])
        DMA(tA[:, :], src, r=[], w=["tA"], key="c_stage")
        A("dve", "tensor_copy", r=["tA"], w=["cmpb"], out=cmpb[:, v, :], in_=tA[:, :])
    for c4 in range(4):
        src = DAP(cd_["c_ovl"], c4 * 130, [[520, 128], [1, 130]])
        DMA(tA[:, 0:130], src, r=[], w=["tA"], key="c_stage")
        A("dve", "tensor_copy", r=["tA"], w=["ovl"], out=ovl[:, c4, :], in_=tA[:, 0:130])
    for c4 in range(4):
        src = DAP(cd_["c_esel"], c4 * 512, [[2048, 32], [1, 512]])
        DMA(tA[0:32, 0:512], src, r=[], w=["tA"], key="c_stage")
        A("dve", "tensor_copy", r=["tA"], w=["esel"], out=esel[:, c4 * 4:(c4 + 1) * 4, :].rearrange("p a b -> p (a b)"),
          in_=tA[0:32, 0:512])
    for c4 in range(3):
        src = DAP(cd_["c_selg"], c4 * 512, [[1536, 24], [1, 512]])
        DMA(tA[0:24, 0:512], src, r=[], w=["tA"], key="c_stage")
        A("dve", "tensor_copy", r=["tA"], w=["selg"], out=selg[:, c4 * 8:(c4 + 1) * 8, :].rearrange("p a b -> p (a b)"),
          in_=tA[0:24, 0:512])
    A("dve", "memset", w=["onesb"], ap=onesb[:], constant=1.0)
    A("dve", "memset", w=["onesf"], ap=onesf[:], constant=1.0)
    for gi, nm in enumerate(("ffn1_norm", "mix_norm", "ffn2_norm", "mem_norm")):
        load_const(gcol[:, gi, :], DAP(vd_[nm], 0, [[1, 128], [128, 8]]), "gcol", eng="pool")
    for tl, nm in ((gq, "nsa_q_norm"), (gk, "nsa_k_norm"), (gmq, "mem_q_norm"), (gmk, "mem_k_norm")):
        load_const(tl[:], DAP(vd_[nm], 0, [[0, 128], [1, 64]]), "g64_" + nm, eng="pool")
    for hh in range(2):
        load_const(gkcol[hh * 64:(hh + 1) * 64, :], DAP(vd_["nsa_k_norm"], 0, [[1, 64], [1, 1]]), "gkcol", eng="pool")
    for g in range(2):
        load_const(gnsa[g * 64:(g + 1) * 64, :], DAP(vd_["nsa_out_norm"], g * 256, [[1, 64], [64, 4]]), "gnsa", eng="pool")
    load_const(ghg[:], DAP(vd_["hgrn_out_norm"], 0, [[1, 128], [128, 2]]), "ghg", eng="pool")
    load_const(gmo[:], DAP(vd_["mem_out_norm"], 0, [[1, 128], [128, 2]]), "gmo", eng="pool")
    load_const(LBt[:].rearrange("p a b -> p (a b)"), DAP(lb_d, 0, [[0, 128], [1, 512]]), "LBt", eng="pool")
    A("dve", "tensor_tensor", r=["LBt"], w=["LB0"], out=LB0[:], in0=LBt[:, 1, :], in1=LBt[:, 0, :], op=ALU.subtract)
    ACT(LB0[:], LB0[:], AF.Exp, r=["LB0"], w=["LB0"])
    A("dve", "tensor_scalar", r=["LB0"], w=["LB0"], out=LB0[:], in0=LB0[:], scalar1=1.0, scalar2=None, op0=ALU.add)
    A("dve", "reciprocal", r=["LB0"], w=["LB0"], out=LB0[:], in_=LB0[:])
    A("dve", "tensor_scalar", r=["LB0"], w=["LB1"], out=LB1[:], in0=LB0[:], scalar1=-1.0, scalar2=1.0, op0=ALU.mult, op1=ALU.add)
    for (w2, w2d, key) in ((w2k, w2k_d, "w2k"), (w2v, w2v_d, "w2v")):
        DMA(tA[:, 0:128].rearrange("p (a b) -> p a b", b=64), DAP(w2d, 0, [[64, 128], [8192, 2], [1, 64]]), r=[], w=["tA"], key="c_stage")
        A("dve", "tensor_copy", r=["tA"], w=[key], out=w2[:], in_=tA[:, 0:128].rearrange("p (a b) -> p a b", b=64))
    for kv, pd in ((0, posk_d), (1, posv_d)):
        for par in range(2):
            Sd.add("pool", lambda e, kv=kv, pd=pd, par=par: e.dma_start(
                out=posS[par * 64:(par + 1) * 64, kv, :], in_=DAP(pd, par * 64, [[1, 64], [128, 16]]),
                allow_slow_non_contiguous=True), r=[], w=["posS"], dma="c_posS")
    A("dve", "tensor_copy", r=["posS"], w=["posSb"], out=posSb[:], in_=posS[:])
    A("pool", "memset", w=["Vs"], ap=Vs[:, :, 64:128], constant=1.0)
    A("pool", "memset", w=["Vw"], ap=Vw[:, :, 64:128], constant=1.0)
    A("pool", "memset", w=["Vc"], ap=Vc[:], constant=0.0)
    A("pool", "memset", w=["Vm"], ap=Vm[:, :, :, 64:128], constant=1.0)
    A("pool", "memset", w=["KcT"], ap=KcT[:], constant=0.0)
    for g in range(2):
        A("pool", "memset", w=["Rk%d" % g], ap=Rk[g][:], constant=0.0)
        A("pool", "memset", w=["Rv%d" % g], ap=Rv[g][:], constant=0.0)
    A("pool", "memset", w=["ST"], ap=ST[:], constant=0.0)
    A("pool", "memset", w=["STb"], ap=STb[:], constant=0.0)

    def norm_transpose(src, src_key, nsub, dstT, dst_key, gidx):
        for sub in range(nsub):
            ACT(tA[:, :].bitcast(BF16), src[:, sub, :], AF.Square, r=[src_key], w=["tA", "ssq"], accum_out=ssq[:, sub:sub + 1])
        ACT(rstd[:, 0:nsub], ssq[:, 0:nsub], AF.Ln, r=["ssq"], w=["rstd"], scale=1.0 / D, bias=eps_col[:, 0:1])
        ACT(rstd[:, 0:nsub], rstd[:, 0:nsub], AF.Exp, r=["rstd"], w=["rstd"], scale=-0.5)
        for sub in range(nsub):
            hb = bA if sub % 2 == 0 else bB
            hk = "bA" if sub % 2 == 0 else "bB"
            for half in range(2):
                A("dve", "tensor_scalar", r=[src_key, "rstd"], w=[hk], out=hb[:, :], in0=src[:, sub, half * 512:(half + 1) * 512],
                  scalar1=rstd[:, sub:sub + 1], scalar2=None, op0=ALU.mult)
                pi, pk = PA()
                for q4 in range(4):
                    TR(psum_b[pi][:, q4 * 128:(q4 + 1) * 128], hb[:, q4 * 128:(q4 + 1) * 128], identb[:], r=[hk, "identb"], w=[pk])
                A("dve", "tensor_tensor", r=[pk, "gcol"], w=[dst_key],
                  out=dstT[:, half * 4:(half + 1) * 4, sub * 128:(sub + 1) * 128],
                  in0=psum_b[pi][:, 0:512].rearrange("p (a b) -> p a b", b=128),
                  in1=gcol[:, gidx, half * 4:(half + 1) * 4].unsqueeze(2).broadcast_to([128, 4, 128]), op=ALU.mult)

    eps_col = sbt("eps_col", [128, 1], F32)
    A("dve", "memset", w=["eps_col"], ap=eps_col[:], constant=EPS)

    slot_ctr = {"wgu": 0, "wdb": 0}

    def ffn(pref, gidx):
        wg_s, wu_s, wd_s = ws_[pref + "_w_gate"], ws_[pref + "_w_up"], ws_[pref + "_w_down"]
        kg, ku, kd = "scr_" + pref + "_w_gate", "scr_" + pref + "_w_up", "scr_" + pref + "_w_down"
        norm_transpose(xt, "xt", 4, hT, "hT", gidx)
        for half in range(2):
            for fcl in range(11):
                fc = half * 11 + fcl
                s = slot_ctr["wgu"] % 2
                slot_ctr["wgu"] += 1
                wk = "wgu%d" % s
                Sd.add("sp", lambda e, s=s, fc=fc: [
                    e.dma_start(out=wgu[s][:, 0, :, :].rearrange("p a b -> p (a b)"), in_=DAP(wg_s, fc * 131072, [[1024, 128], [1, 1024]])),
                    e.dma_start(out=wgu[s][:, 1, :, :].rearrange("p a b -> p (a b)"), in_=DAP(wu_s, fc * 131072, [[1024, 128], [1, 1024]]))],
                    r=[kg, ku], w=[wk], dma=wk, ndma=2)
                pg, pgk = PA()
                pu, puk = PA()
                for dc in range(8):
                    MM(psum[pg][:, :], wgu[s][:, 0, dc, :], hT[:, dc, :], dc == 0, dc == 7, r=[wk, "hT"], w=[pgk])
                for dc in range(8):
                    MM(psum[pu][:, :], wgu[s][:, 1, dc, :], hT[:, dc, :], dc == 0, dc == 7, r=[wk, "hT"], w=[puk])
                sg = tB if fcl % 2 == 0 else tC
                sgk = "tB" if fcl % 2 == 0 else "tC"
                ACT(sg[:, :], psum[pg][:, :], AF.Silu, r=[pgk], w=[sgk])
                A("dve", "tensor_tensor", r=[sgk, puk], w=["actT"], out=actT[:, fcl, :], in0=sg[:, :], in1=psum[pu][:, :], op=ALU.mult)
            down_like(actT, "actT", 11, lambda fcl, half=half: [(slice(0, 128), DAP(wd_s, (half * 11 + fcl) * 128 * D, [[D, 128], [1, D]]))], kd, 0.5)

    def down_like(srcT, src_key, nchunk, wsrc, wkey, scale):
        for ps_ in range(2):
            banks = [PA() for _ in range(4)]
            for c in range(nchunk):
                s = slot_ctr["wdb"] % 2
                slot_ctr["wdb"] += 1
                wk = "wdb%d" % s
                parts = wsrc(c)
                Sd.add("sp", lambda e, s=s, parts=parts: [e.dma_start(out=wdb[s][sl, :], in_=ap) for (sl, ap) in parts],
                       r=[wkey], w=[wk], dma=wk, ndma=len(parts))
                for si in range(2):
                    sub = ps_ * 2 + si
                    for ch in range(2):
                        bi, bk = banks[si * 2 + ch]
                        MM(psum[bi][:, :], srcT[:, c, sub * 128:(sub + 1) * 128], wdb[s][:, ch * 512:(ch + 1) * 512],
                           c == 0, c == nchunk - 1, r=[src_key, wk], w=[bk])
            for si in range(2):
                sub = ps_ * 2 + si
                for ch in range(2):
                    bi, bk = banks[si * 2 + ch]
                    A("dve", "scalar_tensor_tensor", r=[bk, "xt"], w=["xt"], out=xt[:, sub, ch * 512:(ch + 1) * 512],
                      in0=psum[bi][:, :], scalar=scale, in1=xt[:, sub, ch * 512:(ch + 1) * 512], op0=ALU.mult, op1=ALU.add)

    def headnorm(src, src_key, nh, gain_t, gain_key, dst, dst_key):
        ACT(tE[:, 0:nh * 64].rearrange("p (a b) -> p a b", b=64), src, AF.Square, r=[src_key], w=["tE"])
        A("dve", "tensor_reduce", r=["tE"], w=["ssq"], out=ssq[:, 0:nh], in_=tE[:, 0:nh * 64].rearrange("p (a b) -> p a b", b=64),
          axis=AX.X, op=ALU.add)
        ACT(rstd[:, 0:nh], ssq[:, 0:nh], AF.Ln, r=["ssq"], w=["rstd"], scale=1.0 / 64, bias=eps_col[:, 0:1])
        ACT(rstd[:, 0:nh], rstd[:, 0:nh], AF.Exp, r=["rstd"], w=["rstd"], scale=-0.5)
        A("dve", "tensor_tensor", r=[src_key, "rstd"], w=[dst_key], out=dst, in0=src,
          in1=rstd[:, 0:nh].unsqueeze(2).broadcast_to([128, nh, 64]), op=ALU.mult)
        A("dve", "tensor_tensor", r=[dst_key, gain_key], w=[dst_key], out=dst, in0=dst,
          in1=gain_t[:, :].unsqueeze(1).broadcast_to([128, nh, 64]), op=ALU.mult)

    def rope(buf, key, nh, sub):
        c = cs_c[:, sub, :].unsqueeze(1).broadcast_to([128, nh, 8])
        s_ = cs_s[:, sub, :].unsqueeze(1).broadcast_to([128, nh, 8])
        x1 = buf[:, :, 0:8]
        x2 = buf[:, :, 8:16]
        v1 = sm1[:, 0:nh * 8].rearrange("p (a b) -> p a b", b=8)
        v2 = sm2[:, 0:nh * 8].rearrange("p (a b) -> p a b", b=8)
        v3 = sm3[:, 0:nh * 8].rearrange("p (a b) -> p a b", b=8)
        A("dve", "tensor_tensor", r=[key, "cs"], w=["sm1"], out=v1, in0=x1, in1=s_, op=ALU.mult)
        A("dve", "tensor_tensor", r=[key, "cs"], w=["sm2"], out=v2, in0=x2, in1=s_, op=ALU.mult)
        A("dve", "tensor_tensor", r=[key, "cs"], w=["sm3"], out=v3, in0=x1, in1=c, op=ALU.mult)
        A("dve", "tensor_tensor", r=["sm3", "sm2"], w=["sm3"], out=v3, in0=v3, in1=v2, op=ALU.subtract)
        A("dve", "tensor_tensor", r=[key, "cs"], w=["sm2"], out=v2, in0=x2, in1=c, op=ALU.mult)
        A("dve", "tensor_tensor", r=["sm2", "sm1"], w=[key], out=x2, in0=v2, in1=v1, op=ALU.add)
        A("dve", "tensor_copy", r=["sm3", key], w=[key], out=x1, in_=v3)

    DMA(xt[:, 0:2, :], DAP(mem_d, 0, [[D, 128], [128 * D, 2], [1, D]]), r=[], w=["xt"], key="xt")
    norm_transpose(xt, "xt", 2, hT, "hT", 3)
    for nm, dstk in (("mem_w_k", 0), ("mem_w_v", 1)):
        DMA(winb[:, :, 0:256], DAP(ws_[nm], 0, [[256, 128], [128 * 256, 8], [1, 256]]), r=["scr_" + nm], w=["winb"], key="winb")
        for sub in range(2):
            pi, pk = PA()
            for dc in range(8):
                MM(psum[pi][:, 0:256], hT[:, dc, sub * 128:(sub + 1) * 128], winb[:, dc, 0:256], dc == 0, dc == 7, r=["hT", "winb"], w=[pk])
            if dstk == 0:
                dstv = tD[:, 0:256].rearrange("p (a b) -> p a b", b=64)
                headnorm(psum[pi][:, 0:256].rearrange("p (a b) -> p a b", b=64), pk, 4, gmk, "g64_mem_k_norm", dstv, "tD")
                A("dve", "tensor_copy", r=["tD"], w=["bC"], out=bC[:, 0:256], in_=tD[:, 0:256])
                p2, p2k = PA()
                for pm in range(2):
                    TR(psum_b[p2][:, pm * 128:(pm + 1) * 128], bC[:, pm * 128:(pm + 1) * 128], identb[:], r=["bC", "identb"], w=[p2k])
                A("dve", "tensor_copy", r=[p2k], w=["KmT"], out=KmT[:, :, sub * 128:(sub + 1) * 128],
                  in_=psum_b[p2][:, 0:256].rearrange("p (a b) -> p a b", b=128))
            else:
                for hh_ in range(2):
                    A("dve", "tensor_copy", r=[pk], w=["Vm"], out=Vm[:, sub, :, hh_ * 128:hh_ * 128 + 64],
                      in_=psum[pi][:, 0:256].rearrange("p (a h d) -> p a h d", h=2, d=64)[:, :, hh_, :])
    for kv, nm in ((0, "cmp_w1_k"), (1, "cmp_w1_v")):
        DMA(winb[:, :, :].rearrange("p a b -> p (a b)").rearrange("p (a b) -> p a b", b=256),
            DAP(ws_[nm], 0, [[256, 128], [128 * 256, 16], [1, 256]]), r=["scr_" + nm], w=["winb"], key="winb")
        w1v_ = winb[:, :, :].rearrange("p a b -> p (a b)").rearrange("p (a b) -> p a b", b=256)
        pi, pk = PA()
        for hc_ in range(2):
            for a in range(16):
                MM(psum[pi][:, hc_ * 2:hc_ * 2 + 1], w1v_[:, a, hc_ * 128:(hc_ + 1) * 128], posSb[:, kv, a:a + 1], a == 0, a == 15,
                   r=["winb", "posSb"], w=[pk])
        A("dve", "tensor_copy", r=[pk], w=["b1"], out=b1[:, kv * 2:kv * 2 + 2], in_=psum[pi][:, 0:4:2])

    et_ctr = [0]

    def next_et():
        i = et_ctr[0] % NET
        et_ctr[0] += 1
        return ETb[i], "ET%d" % i

    def dbg_out(name, ap_sb, key, dram_ap):
        if dbg and name in dbg:
            DMA(dram_ap, ap_sb, r=[key], w=[], key="dbg_" + name, is_out=True)

    stop_after = (dbg or {}).get("_stop", None)

    for ti in range(NT):
        t0 = ti * T
        DMA(xt[:, :, :], DAP(x_d, t0 * D, [[D, 128], [128 * D, 4], [1, D]]), r=[], w=["xt"], key="xt")
        DMA(cs_c[:, :, :], DAP(cd_["c_cos"], t0 * 8, [[8, 128], [1024, 4], [1, 8]]), r=[], w=["cs"], key="cs")
        DMA(cs_s[:, :, :], DAP(cd_["c_sin"], t0 * 8, [[8, 128], [1024, 4], [1, 8]]), r=[], w=["cs"], key="cs")
        ffn("ffn1", 0)
        if dbg and "x1" in dbg:
            DMA(DAP(dbg_d["x1"], t0 * D, [[D, 128], [128 * D, 4], [1, D]]), xt[:, :, :], r=["xt"], w=[], key="dbg_x1", is_out=True)
        if stop_after == "ffn1":
            continue
        norm_transpose(xt, "xt", 4, hT, "hT", 1)
        wslot = ti % 2
        pproj = {}
        if stop_after == "mixnorm":
            continue
        for gi, (c0, gw) in enumerate(GROUPS):
            if dbg and "_maxg" in dbg and gi >= dbg["_maxg"]:
                continue
            if dbg and "_skipg" in dbg and gi in dbg["_skipg"]:
                continue
            DMA(winb[:, :, 0:gw], DAP(ws_in[(dbg or {}).get("_srcg", gi)], 0, [[8 * gw, 128], [gw, 8], [1, gw]]), r=["scr_w_in"], w=["winb"], key="winb")
            for sub in range(4):
                if dbg and "_nomm" in dbg:
                    continue
                if dbg and "_onlysub" in dbg and sub not in dbg["_onlysub"]:
                    continue
                pi, pk = PA()
                dcs = (dbg or {}).get("_dcs", list(range(8)))
                ncs = (dbg or {}).get("_ncs", 2)
                cw = (gw + ncs - 1) // ncs
                cw += cw % 2
                for c_lo in range(0, gw, cw):
                    c_hi = min(gw, c_lo + cw)
                    for dc in dcs:
                        MM(psum[pi][:, c_lo:c_hi], hT[:, dc, sub * 128:(sub + 1) * 128], winb[:, dc, c_lo:c_hi], dc == dcs[0], dc == dcs[-1], r=["hT", "winb"], w=[pk])
                P_ = psum[pi]
                tsl = slice(sub * 128, (sub + 1) * 128)
                if dbg and "_groups" in dbg and gi not in dbg["_groups"]:
                    continue
                if gi == 0:
                    qv = tD[:, :].rearrange("p (a b) -> p a b", b=64)
                    headnorm(P_[:, 0:512].rearrange("p (a b) -> p a b", b=64), pk, 8, gq, "g64_nsa_q_norm", qv, "tD")
                    rope(qv, "tD", 8, sub)
                    for g_ in range(2):
                        A("dve", "tensor_copy", r=["tD"], w=["bC"],
                          out=bC[:, :].rearrange("p (j g d) -> p j g d", g=2, d=64)[:, :, g_, :],
                          in_=tD[:, g_ * 256:(g_ + 1) * 256].rearrange("p (j d) -> p j d", d=64))
                    p2, p2k = PA()
                    for j in range(4):
                        TR(psum_b[p2][:, j * 128:(j + 1) * 128], bC[:, j * 128:(j + 1) * 128], identb[:], r=["bC", "identb"], w=[p2k])
                    any_copy(QT[:, :, tsl], psum_b[p2][:, 0:512].rearrange("p (a b) -> p a b", b=128), r=[p2k], w=["QT"], psum_src=True)
                elif gi == 1:
                    A("dve", "tensor_copy", r=[pk], w=["tD"], out=tD[:, 0:256], in_=P_[:, 0:256])
                    rope(tD[:, 0:128].rearrange("p (a b) -> p a b", b=64), "tD", 2, sub)
                    for dup in range(2):
                        A("dve", "tensor_copy", r=["tD"], w=["bC"],
                          out=bC[:, :].rearrange("p (a u d) -> p a u d", u=2, d=64)[:, :, dup, :],
                          in_=tD[:, 0:256].rearrange("p (a d) -> p a d", d=64))
                    p2, p2k = PA()
                    for q4 in range(4):
                        TR(psum_b[p2][:, q4 * 128:(q4 + 1) * 128], bC[:, q4 * 128:(q4 + 1) * 128], identb[:], r=["bC", "identb"], w=[p2k])
                    for q4 in range(4):
                        Rt = (Rk if q4 < 2 else Rv)[q4 % 2]
                        Rkey = ("Rk%d" if q4 < 2 else "Rv%d") % (q4 % 2)
                        m0 = 16 + sub * 128
                        any_copy(Rt[0:64, m0:m0 + 128], psum_b[p2][0:64, q4 * 128:(q4 + 1) * 128], r=[p2k], w=[Rkey], psum_src=True)
                        any_copy(Rt[64:128, m0 - 1:m0 + 127], psum_b[p2][64:128, q4 * 128:(q4 + 1) * 128], r=[p2k], w=[Rkey], psum_src=True)
                    kv_ = tD[:, 256:384].rearrange("p (a b) -> p a b", b=64)
                    headnorm(P_[:, 256:384].rearrange("p (a b) -> p a b", b=64), pk, 2, gk, "g64_nsa_k_norm", kv_, "tD")
                    rope(kv_, "tD", 2, sub)
                    A("dve", "tensor_copy", r=["tD"], w=["bC"], out=bC[:, 256:384], in_=tD[:, 256:384])
                    p3, p3k = PA()
                    TR(psum_b[p3][:, 0:128], bC[:, 256:384], identb[:], r=["bC", "identb"], w=[p3k])
                    any_copy(KTs[:, t0 + sub * 128:t0 + (sub + 1) * 128], psum_b[p3][:, 0:128], r=[p3k], w=["KTs"], psum_src=True)
                    cchunk = ti * 4 + sub
                    any_copy(Vs[:, cchunk, 0:64], P_[:, 384:448], r=[pk], w=["Vs"], psum_src=True)
                    any_copy(Vs[:, cchunk, 128:192], P_[:, 448:512], r=[pk], w=["Vs"], psum_src=True)
                elif gi == 2:
                    kv_ = tD[:, 0:128].rearrange("p (a b) -> p a b", b=64)
                    headnorm(P_[:, 0:128].rearrange("p (a b) -> p a b", b=64), pk, 2, gk, "g64_nsa_k_norm", kv_, "tD")
                    rope(kv_, "tD", 2, sub)
                    A("dve", "tensor_copy", r=["tD"], w=["bC"], out=bC[:, 0:128], in_=tD[:, 0:128])
                    ACT(gsig[:, :], P_[:, 256:280], AF.Sigmoid, r=[pk], w=["gsig"])
                    p3, p3k = PA()
                    TR(psum_b[p3][:, 0:128], bC[:, 0:128], identb[:], r=["bC", "identb"], w=[p3k])
                    TR(psum_b[p3][0:24, 128:256], gsig[:, :], identb[:], r=["gsig", "identb"], w=[p3k])
                    any_copy(KTw[:, wslot, tsl], psum_b[p3][:, 0:128], r=[p3k], w=["KTw"], psum_src=True)
                    any_copy(GT[:, tsl], psum_b[p3][0:24, 128:256], r=[p3k], w=["GT"], psum_src=True)
                    wch = wslot * 4 + sub
                    any_copy(Vw[:, wch, 0:64], P_[:, 128:192], r=[pk], w=["Vw"], psum_src=True)
                    any_copy(Vw[:, wch, 128:192], P_[:, 192:256], r=[pk], w=["Vw"], psum_src=True)
                elif gi == 3:
                    pproj[(3, sub)] = (pi, pk)
                    ACT(tB[:, 0:256], P_[:, 256:512], AF.Sigmoid, r=[pk], w=["tB"])
                    A("dve", "tensor_tensor", r=["tB", "LB1"], w=["tB"], out=tB[:, 0:256], in0=tB[:, 0:256], in1=LB1[:, :], op=ALU.mult)
                    A("dve", "tensor_tensor", r=["tB", "LB0"], w=["tB"], out=tB[:, 0:256], in0=tB[:, 0:256], in1=LB0[:, :], op=ALU.add)
                    ACT(tC[:, 0:256], tB[:, 0:256], AF.Ln, r=["tB"], w=["tC"])
                    A("dve", "tensor_scalar", r=["tB"], w=["tB"], out=tB[:, 0:256], in0=tB[:, 0:256], scalar1=-1.0, scalar2=1.0,
                      op0=ALU.mult, op1=ALU.add)
                    pbk_i, pbk = PA()
                    MM(psum[pbk_i][:, 0:256], f32c["c_tri"][:], tC[:, 0:256], True, True, r=["c_tri", "tC"], w=[pbk])
                    MM(psum[pbk_i][:, 256:512], f32c["c_upp"][:], tC[:, 0:256], True, True, r=["c_upp", "tC"], w=[pbk])
                    pdk_i, pdk = PA()
                    for ch in range(2):
                        for pp in range(2):
                            col = (ch * 2 + pp) * 2
                            MM(psum[pdk_i][:, col:col + 2], tC[ch * 64:(ch + 1) * 64, pp * 128:(pp + 1) * 128], onesf[ch * 64:(ch + 1) * 64, 0:2],
                               True, True, r=["tC", "onesf"], w=[pdk], force=True)
                    ACT(dec[:, sub, :], psum[pdk_i][:, 0:8], AF.Exp, r=[pdk], w=["dec"])
                    ACT(tC[:, 256:512], psum[pbk_i][:, 0:256], AF.Exp, r=[pbk, "tC"], w=["tC"])
                    ACT(tE[:, 0:256], psum[pbk_i][:, 0:256], AF.Exp, r=[pbk], w=["tE"], scale=-1.0)
                    ACT(tE[:, 256:512], psum[pbk_i][:, 256:512], AF.Exp, r=[pbk], w=["tE"])
                    ACT(tC[:, 0:256], P_[:, 0:256], AF.Silu, r=[pk, "tC"], w=["tC"])
                    A("dve", "scalar_tensor_tensor", r=["tC"], w=["bC"], out=bC[:, 0:256], in0=tC[:, 0:256], scalar=0.125, in1=tC[:, 256:512],
                      op0=ALU.mult, op1=ALU.mult)
                    A("dve", "tensor_tensor", r=["tB", "tE"], w=["bC"], out=bC[:, 256:512], in0=tB[:, 0:256], in1=tE[:, 0:256], op=ALU.mult)
                    A("dve", "tensor_tensor", r=["tB", "tE"], w=["KH%d" % sub], out=KHs[sub][:, :], in0=tB[:, 0:256], in1=tE[:, 256:512], op=ALU.mult)
                    p3, p3k = PA()
                    for q4 in range(4):
                        TR(psum_b[p3][:, q4 * 128:(q4 + 1) * 128], bC[:, q4 * 128:(q4 + 1) * 128], identb[:], r=["bC", "identb"], w=[p3k])
                    any_copy(HQ[:, :, tsl], psum_b[p3][:, 0:256].rearrange("p (a b) -> p a b", b=128), r=[p3k], w=["HQ"], psum_src=True)
                    any_copy(HK[:, :, tsl], psum_b[p3][:, 256:512].rearrange("p (a b) -> p a b", b=128), r=[p3k], w=["HK"], psum_src=True)
                elif gi == 4:
                    any_copy(VB[:, sub, :], P_[:, 0:256], r=[pk], w=["VB"], psum_src=True)
                    ACT(bC[:, 0:256], P_[:, 256:512], AF.Silu, r=[pk], w=["bC"])
                    p3, p3k = PA()
                    for q4 in range(2):
                        TR(psum_b[p3][:, q4 * 128:(q4 + 1) * 128], bC[:, q4 * 128:(q4 + 1) * 128], identb[:], r=["bC", "identb"], w=[p3k])
                    any_copy(HG[:, :, tsl], psum_b[p3][:, 0:256].rearrange("p (a b) -> p a b", b=128), r=[p3k], w=["HG"], psum_src=True)
                else:
                    dstv = tD[:, 0:256].rearrange("p (a b) -> p a b", b=64)
                    headnorm(P_[:, 0:256].rearrange("p (a b) -> p a b", b=64), pk, 4, gmq, "g64_mem_q_norm", dstv, "tD")
                    A("dve", "tensor_copy", r=["tD"], w=["bC"], out=bC[:, 0:256], in_=tD[:, 0:256])
                    p3, p3k = PA()
                    for pm in range(2):
                        TR(psum_b[p3][:, pm * 128:(pm + 1) * 128], bC[:, pm * 128:(pm + 1) * 128], identb[:], r=["bC", "identb"], w=[p3k])
                    any_copy(QmT[:, :, tsl], psum_b[p3][:, 0:256].rearrange("p (a b) -> p a b", b=128), r=[p3k], w=["QmT"], psum_src=True)

        if stop_after == "proj":
            continue
        for kv, nm in ((0, "cmp_w1_k"), (1, "cmp_w1_v")):
            DMA(winb[:, :, :].rearrange("p a b -> p (a b)").rearrange("p (a b) -> p a b", b=256),
                DAP(ws_[nm], 0, [[256, 128], [128 * 256, 16], [1, 256]]), r=["scr_" + nm], w=["winb"], key="winb")
            w1v_ = winb[:, :, :].rearrange("p a b -> p (a b)").rearrange("p (a b) -> p a b", b=256)
            R_ = Rk if kv == 0 else Rv
            ph, phk = PA()
            for g in range(2):
                Rkey = ("Rk%d" if kv == 0 else "Rv%d") % g
                for hc_ in range(2):
                    col = (g * 2 + hc_) * 32
                    for a in range(16):
                        MM(psum[ph][:, col:col + 32], w1v_[:, a, hc_ * 128:(hc_ + 1) * 128], R_[g][:, 2 * a:2 * a + 16 * 31 + 1:16],
                           a == 0, a == 15, r=["winb", Rkey], w=[phk])
            for g in range(2):
                for hc_ in range(2):
                    col = (g * 2 + hc_) * 32
                    ACT(hidT[:, kv, g, hc_, :], psum[ph][:, col:col + 32], AF.Silu, r=[phk, "b1"], w=["hidT"],
                        bias=b1[:, kv * 2 + hc_:kv * 2 + hc_ + 1])
            if kv == 0:
                pk_i, pkk = PA()
                for g in range(2):
                    for hc_ in range(2):
                        MM(psum[pk_i][g * 64:(g + 1) * 64, 0:32], w2k[:, hc_, :], hidT[:, 0, g, hc_, :], hc_ == 0, hc_ == 1,
                           r=["w2k", "hidT"], w=[pkk], tile_position=(0, g * 64))
                A("dve", "tensor_copy", r=[pkk], w=["tD"], out=tD[:, 0:32], in_=psum[pk_i][:, 0:32])
                A("dve", "tensor_tensor", r=["tD"], w=["bC"], out=bC[:, 0:32], in0=tD[:, 0:32], in1=tD[:, 0:32], op=ALU.mult)
                pn, pnk = PA()
                MM(psum[pn][:, 0:32], bd64[:], bC[:, 0:32], True, True, r=["bd64", "bC"], w=[pnk])
                ACT(tD[:, 32:64], psum[pn][:, 0:32], AF.Ln, r=[pnk, "tD"], w=["tD"], bias=eps_col[:, 0:1])
                ACT(tD[:, 32:64], tD[:, 32:64], AF.Exp, r=["tD"], w=["tD"], scale=-0.5)
                A("dve", "scalar_tensor_tensor", r=["tD", "gkcol"], w=["KcT"], out=KcT[:, 32 * ti:32 * ti + 32], in0=tD[:, 0:32],
                  scalar=gkcol[:, 0:1], in1=tD[:, 32:64], op0=ALU.mult, op1=ALU.mult)
            else:
                pv_i, pvk = PA()
                base = 32 * (ti % 4)
                for g in range(2):
                    for hc_ in range(2):
                        MM(psum[pv_i][base:base + 32, g * 64:(g + 1) * 64], hidT[:, 1, g, hc_, :], w2v[:, hc_, :], hc_ == 0, hc_ == 1,
                           r=["w2v", "hidT"], w=[pvk], tile_position=(0, base))
                cch = ti // 4
                A("dve", "tensor_copy", r=[pvk], w=["Vc"], out=Vc[base:base + 32, cch, 0:64], in_=psum[pv_i][base:base + 32, 0:64])
                A("dve", "tensor_copy", r=[pvk], w=["Vc"], out=Vc[base:base + 32, cch, 128:192], in_=psum[pv_i][base:base + 32, 64:128])
                A("pool", "memset", r=[], w=["Vc"], ap=Vc[base:base + 32, cch, 64:128], constant=1.0)
                if ti == 0:
                    A("pool", "memset", r=[], w=["Vc"], ap=Vc[0:1, 0, :], constant=0.0)
        for g in range(2):
            A("pool", "tensor_copy", r=["Rk%d" % g], w=["Rk%d" % g], out=Rk[g][:, 0:16], in_=Rk[g][:, 512:528])
            A("pool", "tensor_copy", r=["Rv%d" % g], w=["Rv%d" % g], out=Rv[g][:, 0:16], in_=Rv[g][:, 512:528])

        if stop_after == "compress":
            continue
        def combine(ob, obk, g, dst, gate_idx, first, guard=False):
            vs, ds = g * 64, 64 - g * 64
            if guard:
                A("dve", "tensor_scalar", r=[obk], w=["tA"], out=tA[ds:ds + 64, :], in0=psum[ob][ds:ds + 64, :], scalar1=1e-30, scalar2=None, op0=ALU.max)
                A("dve", "reciprocal", r=["tA"], w=["tA"], out=tA[ds:ds + 64, :], in_=tA[ds:ds + 64, :])
            else:
                A("dve", "reciprocal", r=[obk], w=["tA"], out=tA[ds:ds + 64, :], in_=psum[ob][ds:ds + 64, :])
            if gate_idx is None:
                A("dve", "tensor_tensor", r=[obk, "tA"], w=["YM"], out=dst, in0=psum[ob][vs:vs + 64, :], in1=tA[ds:ds + 64, :], op=ALU.mult)
                return
            A("dve", "tensor_tensor", r=[obk, "tA"], w=["tB"], out=tB[vs:vs + 64, :], in0=psum[ob][vs:vs + 64, :], in1=tA[ds:ds + 64, :], op=ALU.mult)
            pi, pk = PA()
            MM(psum[pi][vs:vs + 64, :], selg[:, gate_idx, :], GT[:, :], True, True, r=["selg", "GT"], w=[pk])
            if first:
                A("dve", "tensor_tensor", r=["tB", pk], w=["YT"], out=dst, in0=tB[vs:vs + 64, :], in1=psum[pi][vs:vs + 64, :], op=ALU.mult)
            else:
                A("dve", "tensor_tensor", r=["tB", pk], w=["tC"], out=tC[vs:vs + 64, :], in0=tB[vs:vs + 64, :], in1=psum[pi][vs:vs + 64, :], op=ALU.mult)
                A("pool", "tensor_tensor", r=["tC", "YT"], w=["YT"], out=dst, in0=dst, in1=tC[vs:vs + 64, :], op=ALU.add)

        def v_lhsT(cache, pitch_, vcol, o_start, o_end, g):
            if g == 0:
                return bass.AP(cache, vcol, [[pitch_, 128], [o_end - vcol, 2], [1, 64]])
            return bass.AP(cache, o_start, [[pitch_, 128], [vcol - o_start, 2], [1, 64]])

        nch_c = ti // 4 + 1
        for j in range(4):
            for g in range(2):
                gs = slice(g * 64, (g + 1) * 64)
                ob, obk = PB()
                ets = []
                for c in range(nch_c):
                    last = (c == nch_c - 1)
                    pi, pk = PA()
                    MM(psum[pi][:, :], KcT[gs, c * 128:(c + 1) * 128], QT[gs, j, :], True, not last, r=["KcT", "QT"], w=[pk])
                    if last:
                        MM(psum[pi][:, :], identb[:], cmpb[:, ti % 4, :], False, True, r=["identb", "cmpb"], w=[pk], force=True)
                    et, etk = next_et()
                    ACT(et[:, :], psum[pi][:, :], AF.Exp, r=[pk], w=[etk], scale=0.125)
                    ets.append((et, etk))
                    lh = Vc[:, c, g * 64:g * 64 + 128]
                    MM(psum[ob][:, :], lh, et[:, :], c == 0, last, r=["Vc", etk], w=[obk])
                for sp in range(2):
                    pi, pk = PA()
                    for s2 in range(2):
                        sub = sp * 2 + s2
                        for c in range(nch_c):
                            et, etk = ets[c]
                            MM(psum[pi][:, s2 * 130:(s2 + 1) * 130], et[:, sub * 128:(sub + 1) * 128], ovl[:, c, :], c == 0, c == nch_c - 1,
                               r=[etk, "ovl"], w=[pk])
                    for s2 in range(2):
                        sub = sp * 2 + s2
                        A("dve", "tensor_scalar", r=[pk], w=["sm1"], out=sm1[:, 0:1], in0=psum[pi][:, s2 * 130 + 128:s2 * 130 + 129],
                          scalar1=1e-30, scalar2=None, op0=ALU.max)
                        A("dve", "reciprocal", r=["sm1"], w=["sm1"], out=sm1[:, 0:1], in_=sm1[:, 0:1])
                        if j == 0:
                            A("dve", "tensor_scalar", r=[pk, "sm1"], w=["impS"], out=impS[:, g, sub, :], in0=psum[pi][:, s2 * 130:s2 * 130 + 128],
                              scalar1=sm1[:, 0:1], scalar2=None, op0=ALU.mult)
                        else:
                            A("dve", "scalar_tensor_tensor", r=[pk, "sm1", "impS"], w=["impS"], out=impS[:, g, sub, :],
                              in0=psum[pi][:, s2 * 130:s2 * 130 + 128], scalar=sm1[:, 0:1], in1=impS[:, g, sub, :], op0=ALU.mult, op1=ALU.add)
                combine(ob, obk, g, YT[gs, j, :], (g * 4 + j) * 3 + 0, True, guard=True)

        if stop_after == "C":
            continue
        nm_ = (4 * ti + 3) // 16 + 1
        for g in range(2):
            for sub in range(4):
                off = 126 - 2 * (ti * 4 + sub)
                A("dve", "tensor_tensor", r=["impS", "keepw"], w=["tk1"], out=tk1[:, :], in0=impS[:, g, sub, :], in1=keepw[:, off:off + 128], op=ALU.mult)
                A("dve", "tensor_tensor", r=["tk1", "addw"], w=["tk1"], out=tk1[:, :], in0=tk1[:, :], in1=addw[:, off:off + 128], op=ALU.add)
                A("dve", "memset", r=["tk1"], w=["tk1"], ap=tk1[:, 0:1], constant=1e4)
                A("dve", "max", r=["tk1"], w=["tkm"], out=tkm[:, 0:8], in_=tk1[:, :])
                A("dve", "match_replace", r=["tk1", "tkm"], w=["tk2"], out=tk2[:, :], in_to_replace=tkm[:, 0:8], in_values=tk1[:, :], imm_value=-2.0)
                A("dve", "max", r=["tk2", "tkm"], w=["tkm"], out=tkm[:, 8:16], in_=tk2[:, :])
                A("dve", "tensor_scalar", r=["tk1", "tkm"], w=["negm"], out=negm[:, :], in0=tk1[:, :], scalar1=tkm[:, 15:16], scalar2=NEG,
                  op0=ALU.is_lt, op1=ALU.mult)
                pi, pk = PA()
                for m in range(nm_):
                    TR(psum_b[pi][0:32, m * 128:(m + 1) * 128], negm[:, m * 32:(m + 1) * 32], identb[:], r=["negm", "identb"], w=[pk])
                any_copy(NMT[:, g, 0:nm_, sub * 128:(sub + 1) * 128], psum_b[pi][0:32, 0:nm_ * 128].rearrange("p (a b) -> p a b", b=128),
                         r=[pk], w=["NMT"], psum_src=True)

        if stop_after == "K":
            continue
        wpitch = 8 * 128 + 128
        for j in range(4):
            for g in range(2):
                gs = slice(g * 64, (g + 1) * 64)
                ob, obk = PB()
                rs = [r for r in (-1, 0, -4, -3, -2, 1, 2, 3) if 4 * ti + r >= 0]
                for idx, r in enumerate(rs):
                    c = 4 * ti + r
                    slot, within = (c // 4) % 2, c % 4
                    kl = KTw[gs, slot, within * 128:(within + 1) * 128]
                    if r < 0:
                        qa, qb, bq, bias, bkey = 0, 128 * (r + 5), 128 * (r + 4), winbias, "winbias"
                    else:
                        qa, qb, bq, bias, bkey = 128 * r, 512, 128 * r, causb, "causb"
                    pi, pk = PA()
                    MM(psum[pi][:, bq:bq + 128], kl, QT[gs, j, bq:bq + 128], True, False, r=["KTw", "QT"], w=[pk])
                    MM(psum[pi][:, bq:bq + 128], identb[:], bias[:], False, True, r=["identb", bkey], w=[pk], force=True)
                    if r < 0 and bq > 0:
                        MM(psum[pi][:, 0:bq], kl, QT[gs, j, 0:bq], True, True, r=["KTw", "QT"], w=[pk], force=True)
                    if r >= 0 and bq + 128 < 512:
                        MM(psum[pi][:, bq + 128:512], kl, QT[gs, j, bq + 128:512], True, True, r=["KTw", "QT"], w=[pk], force=True)
                    et, etk = next_et()
                    ACT(et[:, qa:qb], psum[pi][:, qa:qb], AF.Exp, r=[pk], w=[etk], scale=0.125)
                    lh = Vw[:, slot * 4 + within, g * 64:g * 64 + 128]
                    MM(psum[ob][:, qa:qb], lh, et[:, qa:qb], idx == 0, idx == len(rs) - 1, r=["Vw", etk], w=[obk])
                combine(ob, obk, g, YT[gs, j, :], (g * 4 + j) * 3 + 2, False)

        if stop_after == "W":
            continue
        for hm in range(4):
            pm, hh = hm // 2, hm % 2
            hs = slice(hh * 64, hh * 64 + 64)
            ob, obk = PB()
            for c in range(2):
                pi, pk = PA()
                MM(psum[pi][:, :], KmT[hs, pm, c * 128:(c + 1) * 128], QmT[hs, pm, :], True, True, r=["KmT", "QmT"], w=[pk])
                et, etk = next_et()
                ACT(et[:, :], psum[pi][:, :], AF.Exp, r=[pk], w=[etk], scale=0.125)
                lh = Vm[:, c, pm, hh * 64:hh * 64 + 128]
                MM(psum[ob][:, :], lh, et[:, :], c == 0, c == 1, r=["Vm", etk], w=[obk])
            combine(ob, obk, hh, YM[hs, pm, :], None, True)

        if stop_after == "M":
            continue
        A("pool", "tensor_copy", r=["STb"], w=["STb"], out=STb[:, 0, :, :], in_=STb[:, 8, :, :])
        pos_ = [PB(), PB()]
        for sub in range(4):
            pU, pUk = PA()
            for ch in range(2):
                for pp in range(2):
                    col = ch * 2 + pp
                    MM(psum[pU][:, col * 128:(col + 1) * 128], KHs[sub][ch * 64:(ch + 1) * 64, pp * 128:(pp + 1) * 128],
                       VB[ch * 64:(ch + 1) * 64, sub, pp * 128:(pp + 1) * 128], True, True, r=["KH%d" % sub, "VB"], w=[pUk], force=True)
            pa_, pak = PA()
            for ch in range(2):
                c = sub * 2 + ch
                for pp in range(2):
                    for hh in range(2):
                        hs = slice(hh * 64, hh * 64 + 64)
                        MM(psum[pa_][ch * 64:(ch + 1) * 64, (pp * 2 + hh) * 64:(pp * 2 + hh + 1) * 64], HK[hs, pp, c * 64:(c + 1) * 64],
                           HQ[hs, pp, c * 64:(c + 1) * 64], True, True, r=["HK", "HQ"], w=[pak], force=True)
            A("dve", "tensor_tensor", r=[pak, "hmask"], w=["AT"], out=AT[:, sub, :].rearrange("p (a b) -> p a b", b=64),
              in0=psum[pa_][:, 0:256].rearrange("p (a b) -> p a b", b=64), in1=hmask[:, :].unsqueeze(1).broadcast_to([128, 4, 64]), op=ALU.mult)
            for ch in range(2):
                c = sub * 2 + ch
                for pp in range(2):
                    for hh in range(2):
                        hs = slice(hh * 64, hh * 64 + 64)
                        cc_ = slice(c * 64, (c + 1) * 64)
                        MM(psum[pos_[pp][0]][hs, cc_], STb[hs, c, pp, :], HQ[hs, pp, cc_], True, False, r=["STb", "HQ"], w=[pos_[pp][1]], force=True)
                        MM(psum[pos_[pp][0]][hs, cc_], VB[ch * 64:(ch + 1) * 64, sub, pp * 128 + hh * 64:pp * 128 + hh * 64 + 64],
                           AT[ch * 64:(ch + 1) * 64, sub, (pp * 2 + hh) * 64:(pp * 2 + hh + 1) * 64], False, True, r=["VB", "AT"], w=[pos_[pp][1]], force=True)
                for pp in range(2):
                    col = ch * 2 + pp
                    for hh in range(2):
                        hs = slice(hh * 64, hh * 64 + 64)
                        A("dve", "scalar_tensor_tensor", r=["ST", pUk, "dec"], w=["ST"], out=ST[hs, pp, :], in0=ST[hs, pp, :],
                          scalar=dec[hs, sub, col * 2:col * 2 + 1], in1=psum[pU][hs, col * 128 + hh * 64:col * 128 + hh * 64 + 64],
                          op0=ALU.mult, op1=ALU.add)
                A("pool", "tensor_copy", r=["ST", "STb"], w=["STb"], out=STb[:, c + 1, :, :], in_=ST[:, :, :])
        for pp in range(2):
            ob, obk = pos_[pp]
            ACT(bA[:, :], psum[ob][:, :], AF.Square, r=[obk], w=["bA"])
            pn, pnk = PA()
            MM(psum[pn][:, :], bd64[:], bA[:, :], True, True, r=["bd64", "bA"], w=[pnk])
            ACT(tA[:, :], psum[pn][:, :], AF.Ln, r=[pnk], w=["tA"], bias=eps_col[:, 0:1])
            ACT(tA[:, :], tA[:, :], AF.Exp, r=["tA"], w=["tA"], scale=-0.5)
            A("dve", "tensor_tensor", r=[obk, "tA"], w=["tB"], out=tB[:, :], in0=psum[ob][:, :], in1=tA[:, :], op=ALU.mult)
            A("dve", "scalar_tensor_tensor", r=["tB", "ghg", "HG"], w=["hT"], out=hT[:, 4 + pp, :], in0=tB[:, :], scalar=ghg[:, pp:pp + 1],
              in1=HG[:, pp, :], op0=ALU.mult, op1=ALU.mult)

        if stop_after == "H":
            continue
        spitch = NCH * 128 + 128
        nchs = 4 * ti + 4
        for j in range(4):
            for g in range(2):
                gs = slice(g * 64, (g + 1) * 64)
                ob, obk = PB()
                for c in range(nchs):
                    m, cc = c // 16, c % 16
                    r = c - 4 * ti
                    kl = KTs[gs, c * 128:(c + 1) * 128]
                    pi, pk = PA()
                    if r < 0:
                        qa = 0
                        MM(psum[pi][:, :], kl, QT[gs, j, :], True, False, r=["KTs", "QT"], w=[pk])
                        MM(psum[pi][:, :], esel[:, cc, :], NMT[:, g, m, :], False, True, r=["esel", "NMT"], w=[pk], force=True)
                    else:
                        qa = 128 * r
                        MM(psum[pi][:, qa:qa + 128], kl, QT[gs, j, qa:qa + 128], True, False, r=["KTs", "QT"], w=[pk])
                        MM(psum[pi][:, qa:qa + 128], esel[:, cc, :], NMT[:, g, m, qa:qa + 128], False, False, r=["esel", "NMT"], w=[pk], force=True)
                        MM(psum[pi][:, qa:qa + 128], identb[:], causb[:], False, True, r=["identb", "causb"], w=[pk], force=True)
                        if qa + 128 < 512:
                            MM(psum[pi][:, qa + 128:512], kl, QT[gs, j, qa + 128:512], True, False, r=["KTs", "QT"], w=[pk], force=True)
                            MM(psum[pi][:, qa + 128:512], esel[:, cc, :], NMT[:, g, m, qa + 128:512], False, True, r=["esel", "NMT"], w=[pk], force=True)
                    et, etk = next_et()
                    ACT(et[:, qa:512], psum[pi][:, qa:512], AF.Exp, r=[pk], w=[etk], scale=0.125)
                    lh = Vs[:, c, g * 64:g * 64 + 128]
                    MM(psum[ob][:, qa:512], lh, et[:, qa:512], c == 0, c == nchs - 1, r=["Vs", etk], w=[obk])
                combine(ob, obk, g, YT[gs, j, :], (g * 4 + j) * 3 + 1, False)

        if stop_after == "S":
            continue
        pn, pnk = PA()
        for j in range(4):
            bx, bxk = (bA, "bA") if j % 2 == 0 else (bB, "bB")
            ACT(bx[:, :], YT[:, j, :], AF.Square, r=["YT"], w=[bxk])
            MM(psum[pn][:, :], onesb[:], bx[:, :], j == 0, j == 3, r=["onesb", bxk], w=[pnk])
        ACT(tA[:, :], psum[pn][:, :], AF.Ln, r=[pnk], w=["tA"], scale=1.0 / 512, bias=eps_col[:, 0:1])
        ACT(tA[:, :], tA[:, :], AF.Exp, r=["tA"], w=["tA"], scale=-0.5)
        for j in range(4):
            A("dve", "scalar_tensor_tensor", r=["YT", "gnsa", "tA"], w=["hT"], out=hT[:, j, :], in0=YT[:, j, :], scalar=gnsa[:, j:j + 1],
              in1=tA[:, :], op0=ALU.mult, op1=ALU.mult)
        pn, pnk = PA()
        for pm in range(2):
            bx, bxk = (bA, "bA") if pm % 2 == 0 else (bB, "bB")
            ACT(bx[:, :], YM[:, pm, :], AF.Square, r=["YM"], w=[bxk])
            MM(psum[pn][:, :], onesb[:], bx[:, :], pm == 0, pm == 1, r=["onesb", bxk], w=[pnk])
        ACT(tA[:, :], psum[pn][:, :], AF.Ln, r=[pnk], w=["tA"], scale=1.0 / 256, bias=eps_col[:, 0:1])
        ACT(tA[:, :], tA[:, :], AF.Exp, r=["tA"], w=["tA"], scale=-0.5)
        for pm in range(2):
            A("dve", "scalar_tensor_tensor", r=["YM", "gmo", "tA"], w=["hT"], out=hT[:, 6 + pm, :], in0=YM[:, pm, :], scalar=gmo[:, pm:pm + 1],
              in1=tA[:, :], op0=ALU.mult, op1=ALU.mult)
        if dbg and "mixT" in dbg:
            for c8 in range(8):
                A("dve", "tensor_copy", r=["hT"], w=["tD"], out=tD[:, :], in_=hT[:, c8, :])
                DMA(DAP(dbg_d["mixT"], c8 * 128 * S + t0, [[S, 128], [1, T]]), tD[:, :], r=["tD"], w=[], key="dbg_mixT", is_out=True)

        if stop_after == "norm":
            continue
        def wout_src(c):
            if c < 4:
                return [(slice(g * 64, g * 64 + 64), DAP(ws_["w_out"], ((g * 4 + c) * 64) * D, [[D, 64], [1, D]])) for g in range(2)]
            return [(slice(0, 128), DAP(ws_["w_out"], (512 + (c - 4) * 128) * D, [[D, 128], [1, D]]))]
        down_like(hT, "hT", 8, wout_src, "scr_w_out", 1.0)
        if dbg and "x2" in dbg:
            DMA(DAP(dbg_d["x2"], t0 * D, [[D, 128], [128 * D, 4], [1, D]]), xt[:, :, :], r=["xt"], w=[], key="dbg_x2", is_out=True)
        ffn("ffn2", 2)
        DMA(DAP(out_d, t0 * D, [[D, 128], [128 * D, 4], [1, D]]), xt[:, :, :], r=["xt"], w=[], key="out", is_out=True)

    Sd.finish()
    with nc.allow_non_contiguous_dma(reason="small constant / layout loads"):
        Sd.emit(st)
    return nc, st


_CACHE = {}


def _get(S):
    if S not in _CACHE:
        _CACHE[S] = build(S)
    return _CACHE[S]


def make_in_map(inputs, b, S):
    m = {"x": np.ascontiguousarray(inputs["x"][b]), "mem": np.ascontiguousarray(inputs["mem"][b])}
    for k in ("ffn1_w_gate", "ffn1_w_up", "ffn1_w_down", "ffn2_w_gate", "ffn2_w_up", "ffn2_w_down", "w_in", "w_out",
              "cmp_w1_k", "cmp_w1_v", "mem_w_k", "mem_w_v", "ffn1_norm", "mix_norm", "ffn2_norm", "mem_norm", "nsa_q_norm",
              "nsa_k_norm", "nsa_out_norm", "hgrn_out_norm", "mem_q_norm", "mem_k_norm", "mem_out_norm",
              "cmp_pos_k", "cmp_pos_v", "cmp_w2_k", "cmp_w2_v"):
        m[k] = np.ascontiguousarray(np.asarray(inputs[k])[0], dtype=np.float32)
    m["hgrn_lb_logits"] = np.ascontiguousarray(inputs["hgrn_lb_logits"], dtype=np.float32)
    w_in_full = m.pop("w_in")
    for gi_, (c0_, gw_) in enumerate(GROUPS):
        m["w_in_g%d" % gi_] = np.ascontiguousarray(w_in_full[:, c0_:c0_ + gw_])
    m.update(host_consts(S))
    return m


def kernel(**inputs):
    x = np.asarray(inputs["x"])
    B, S, _ = x.shape
    nc, _st = _get(S)
    in_maps = [make_in_map(inputs, b, S) for b in range(B)]
    res = run_bass_kernel_spmd(nc, in_maps, core_ids=list(range(B)))
    return np.stack([np.asarray(r["out"]) for r in res.results], axis=0).astype(np.float32)
```

```python
import numpy as np
from contextlib import ExitStack
import concourse.bass as bass
import concourse.mybir as mybir
from concourse.bass_utils import run_bass_kernel_spmd

F32 = mybir.dt.float32
BF16 = mybir.dt.bfloat16
AF = mybir.ActivationFunctionType
ALU = mybir.AluOpType
AX = mybir.AxisListType

D = 1024
DFF = 2816
NFC = 22
T = 512
INW = 2584
NEG = -30000.0
EPS = 1e-6
GROUPS = [(0, 512), (512, 512), (1024, 280), (1304, 512), (1816, 512), (2328, 256)]


class Op:
    __slots__ = ("eng", "idx", "fn", "deps", "signal", "sigval", "dma", "ndma", "dmaval", "name", "force")


class Sched:
    SAME_ENG_DIST = 4

    def __init__(self, nc):
        self.nc = nc
        self.engs = {"pe": nc.tensor, "act": nc.scalar, "dve": nc.vector, "pool": nc.gpsimd, "sp": nc.sync}
        self.ops = {e: [] for e in self.engs}
        self.last_w = {}
        self.rd_eng = {}
        self.rd_dma = {}
        self.dma_count = {}
        self.dma_last = {}
        self.pending_barrier = {}
        self.out_dma_ops = []

    def add(self, eng, fn, r=(), w=(), dma=None, ndma=1, name=None, is_out=False, force=False):
        op = Op()
        op.force = force
        op.eng = eng
        op.idx = len(self.ops[eng])
        op.fn = fn
        op.signal = False
        op.sigval = None
        op.dma = dma
        op.ndma = ndma
        op.dmaval = None
        op.name = name
        w = list(w) + [k for k in r if k.startswith("ps")]
        r = [k for k in r if not k.startswith("ps")]
        deps = []
        for k in r:
            p = self.last_w.get(k)
            if p is not None:
                deps.append(p)
        for k in w:
            p = self.last_w.get(k)
            if p is not None:
                deps.append(p)
            d = self.rd_eng.get(k)
            if d:
                deps.extend(d.values())
            d = self.rd_dma.get(k)
            if d:
                deps.extend(d)
        if eng in self.pending_barrier:
            deps.extend(self.pending_barrier.pop(eng))
        op.deps = self._filter(op, deps)
        if dma is not None:
            c = self.dma_count.get(dma, 0) + ndma
            self.dma_count[dma] = c
            op.dmaval = 16 * c
            self.dma_last[dma] = op
            if is_out:
                self.out_dma_ops.append(op)
        for k in r:
            if dma is not None:
                self.rd_dma.setdefault(k, []).append(op)
            else:
                self.rd_eng.setdefault(k, {})[eng] = op
        for k in w:
            self.last_w[k] = op
            self.rd_eng[k] = {}
            self.rd_dma[k] = []
        self.ops[eng].append(op)
        return op

    def _filter(self, op, deps):
        best_eng = {}
        best_dma = {}
        for p in deps:
            if p is op:
                continue
            if p.dma is not None:
                q = best_dma.get(p.dma)
                if q is None or p.dmaval > q.dmaval:
                    best_dma[p.dma] = p
                continue
            if p.eng == op.eng and op.dma is None:
                if op.eng == "pe" and not op.force:
                    continue
                if op.idx - p.idx > self.SAME_ENG_DIST:
                    continue
            q = best_eng.get(p.eng)
            if q is None or p.idx > q.idx:
                best_eng[p.eng] = p
        out = list(best_eng.values()) + list(best_dma.values())
        for p in out:
            if p.dma is None:
                p.signal = True
        return out

    def barrier(self):
        snap = []
        for e, lst in self.ops.items():
            for p in reversed(lst):
                if p.dma is None and p.fn is not None:
                    snap.append(p)
                    break
        snap.extend(self.dma_last.values())
        for e in self.engs:
            self.pending_barrier[e] = list(snap)

    def finish(self):
        op = Op()
        op.eng = "sp"
        op.idx = len(self.ops["sp"])
        op.fn = None
        op.signal = False
        op.dma = None
        op.ndma = 0
        op.dmaval = None
        op.name = "final"
        op.force = False
        op.sigval = None
        op.deps = self._filter(op, list(self.out_dma_ops))
        self.ops["sp"].append(op)

    def emit(self, stack):
        nc = self.nc
        for e, lst in self.ops.items():
            c = 0
            for p in lst:
                if p.signal:
                    c += 1
                    p.sigval = c
        esem = {e: stack.enter_context(nc.semaphore("s_" + e)) for e in self.engs if e != "sp"}
        dsem = {k: stack.enter_context(nc.semaphore("d_%d" % i)) for i, k in enumerate(self.dma_count)}

        def body(ename):
            def f(eh):
                wm = {}
                for p in self.ops[ename]:
                    for d in p.deps:
                        if d.dma is not None:
                            sem, val = dsem[d.dma], d.dmaval
                        else:
                            sem, val = esem[d.eng], d.sigval
                        key = id(sem)
                        if wm.get(key, 0) >= val:
                            continue
                        wm[key] = val
                        eh.wait_ge(sem, val)
                    if p.fn is None:
                        continue
                    ins = p.fn(eh)
                    if p.dma is not None:
                        if not isinstance(ins, (list, tuple)):
                            ins = [ins]
                        assert len(ins) == p.ndma, (p.name, len(ins), p.ndma)
                        for i_ in ins:
                            i_.then_inc(dsem[p.dma], 16)
                    elif p.signal:
                        if isinstance(ins, (list, tuple)):
                            ins = ins[-1]
                        ins.then_inc(esem[ename], 1)
            return f

        with nc.Block() as blk:
            blk.sync(body("sp"))
            blk.tensor(body("pe"))
            blk.scalar(body("act"))
            blk.vector(body("dve"))
            blk.gpsimd(body("pool"))


def host_consts(S):
    NT = S // T
    c = {}
    pos = np.arange(S, dtype=np.float32)
    inv = (np.float32(500000.0) ** (-(np.arange(0, 16, 2, dtype=np.float32) / np.float32(16)))).astype(np.float32)
    ang = (pos[:, None] * inv[None, :]).astype(np.float32)
    c["c_cos"] = np.cos(ang).astype(np.float32)
    c["c_sin"] = np.sin(ang).astype(np.float32)
    c["c_ident"] = np.eye(128, dtype=np.float32)
    p = np.arange(128)
    same = (p[:, None] // 64) == (p[None, :] // 64)
    c["c_tri"] = (same & (p[:, None] <= p[None, :])).astype(np.float32)
    c["c_upp"] = (same & (p[:, None] > p[None, :])).astype(np.float32)
    c["c_hmask"] = ((p[:, None] % 64) <= np.arange(64)[None, :]).astype(np.float32)
    kk = p[:, None]
    qq = p[None, :]
    c["c_caus"] = np.where(kk <= qq, 0.0, NEG).astype(np.float32)
    c["c_winb"] = np.where(qq < kk, 0.0, NEG).astype(np.float32)
    r = p[:, None, None]
    v = np.arange(4)[None, :, None]
    q = np.arange(512)[None, None, :]
    c["c_cmpb"] = np.where(16 * r + 15 <= 512 * v + q, 0.0, NEG).astype(np.float32)
    npr = np.arange(512)
    n = npr - 1
    j = np.arange(128)
    ovl = ((16 * n[:, None] < 64 * j[None, :] + 64) & (16 * n[:, None] + 32 > 64 * j[None, :])).astype(np.float32)
    ova = np.zeros((512, 130), np.float32)
    ova[:, :128] = ovl
    ova[:, 128] = 1.0
    ova[0, :] = 0.0
    c["c_ovl"] = ova.reshape(4, 128, 130).transpose(1, 0, 2).copy()
    m = np.arange(256)[None, :]
    hq = (p[:, None] >= 64).astype(np.int64)
    d = m - 126 - hq
    c["c_keepw"] = (d <= -2).astype(np.float32)
    c["c_addw"] = (np.where(d == 0, 2e4, 0.0) + np.where(d == -1, 4e4, 0.0) + np.where(d > 0, -1.0, 0.0)).astype(np.float32)
    jp = np.arange(32)[:, None, None]
    cc = np.arange(16)[None, :, None]
    k = np.arange(128)[None, None, :]
    c["c_esel"] = (jp == 2 * cc + k // 64).astype(np.float32)
    rr = np.arange(24)[:, None, None]
    hb = np.arange(24)[None, :, None]
    c["c_selg"] = np.broadcast_to((rr == hb), (24, 24, 64)).astype(np.float32).copy()
    c["c_bd64"] = (same.astype(np.float32) / 64.0).astype(np.float32)
    return c


CONST_SHAPES = None


def build(S, dbg=None):
    NT = S // T
    NCH = S // 128
    nc = bass.Bass("TRN2", target_bir_lowering=False)
    st = ExitStack()
    Sd = Sched(nc)

    def dram_in(name, shape, dt=F32):
        return nc.dram_tensor(name, list(shape), dt, kind="ExternalInput")

    def dram_scr(name, shape, dt=BF16):
        return nc.dram_tensor(name, list(shape), dt, kind="Internal")

    x_d = dram_in("x", [S, D])
    mem_d = dram_in("mem", [256, D])
    out_d = nc.dram_tensor("out", [S, D], F32, kind="ExternalOutput")
    wnames = {
        "ffn1_w_gate": [D, DFF], "ffn1_w_up": [D, DFF], "ffn1_w_down": [DFF, D],
        "ffn2_w_gate": [D, DFF], "ffn2_w_up": [D, DFF], "ffn2_w_down": [DFF, D],
        "w_in": [D, INW], "w_out": [D, D], "cmp_w1_k": [2048, 256], "cmp_w1_v": [2048, 256],
        "mem_w_k": [D, 256], "mem_w_v": [D, 256],
    }
    wd_ = {k: dram_in(k, v) for k, v in wnames.items() if k != "w_in"}
    for gi_, (c0_, gw_) in enumerate(GROUPS):
        wd_["w_in_g%d" % gi_] = dram_in("w_in_g%d" % gi_, [D, gw_])
    ws_ = {k: dram_scr("s_" + k, [v[0] * v[1]]) for k, v in wnames.items()}
    ws_in = [dram_scr("s_w_in_g%d" % gi_, [1024 * 512]) for gi_, (c0_, gw_) in enumerate(GROUPS)]
    vecs = {
        "ffn1_norm": D, "mix_norm": D, "ffn2_norm": D, "mem_norm": D, "nsa_q_norm": 64, "nsa_k_norm": 64,
        "nsa_out_norm": 512, "hgrn_out_norm": 256, "mem_q_norm": 64, "mem_k_norm": 64, "mem_out_norm": 256,
    }
    vd_ = {k: dram_in(k, [v]) for k, v in vecs.items()}
    lb_d = dram_in("hgrn_lb_logits", [2, 256])
    posk_d = dram_in("cmp_pos_k", [32, 64])
    posv_d = dram_in("cmp_pos_v", [32, 64])
    w2k_d = dram_in("cmp_w2_k", [256, 64])
    w2v_d = dram_in("cmp_w2_v", [256, 64])
    hc = host_consts(512)
    cshape = {k: v.shape for k, v in hc.items()}
    cshape["c_cos"] = (S, 8)
    cshape["c_sin"] = (S, 8)
    cd_ = {k: dram_in(k, v) for k, v in cshape.items()}
    dbg_d = {}
    if dbg:
        for k, shp in dbg.items():
            if k.startswith("_"):
                continue
            dbg_d[k] = nc.dram_tensor("dbg_" + k, list(shp), F32, kind="ExternalOutput")

    def DAP(t, off, pat):
        return bass.AP(t, off, [list(x) for x in pat])

    def sbt(name, shape, dt):
        return st.enter_context(nc.sbuf_tensor(name, list(shape), dt))

    psum = [st.enter_context(nc.psum_tensor("ps%d" % i, [128, 512], F32)) for i in range(8)]
    psum_b = [p.bitcast(BF16) for p in psum]
    pa_ctr = [0]
    pb_ctr = [0]

    def PA():
        i = pa_ctr[0] % 5
        pa_ctr[0] += 1
        return i, "ps%d" % i

    def PB():
        i = 5 + pb_ctr[0] % 3
        pb_ctr[0] += 1
        return i, "ps%d" % i

    def A(eng, meth, r=(), w=(), **kw):
        return Sd.add(eng, lambda e, kw=kw, meth=meth: getattr(e, meth)(**kw), r=r, w=w)

    def MM(out, lhsT, rhs, start, stop, r, w, force=False, **kw):
        return Sd.add("pe", lambda e: e.matmul(out, lhsT=lhsT, rhs=rhs, start=start, stop=stop, **kw), r=r, w=w, force=force)

    def TR(out, in_, ident, r, w):
        return Sd.add("pe", lambda e: e.transpose(out=out, in_=in_, identity=ident), r=r, w=w)

    def DMA(out, in_, r, w, key, eng="sp", is_out=False):
        return Sd.add(eng, lambda e: e.dma_start(out=out, in_=in_), r=r, w=w, dma=key, is_out=is_out)

    def ACT(out, in_, func, r, w, **kw):
        return Sd.add("act", lambda e: e.activation(out=out, in_=in_, func=func, **kw), r=r, w=w)

    rr_ctr = [0]

    def any_copy(out, in_, r, w, psum_src=False):
        engs = ["dve", "act"] if psum_src else ["dve", "act", "pool"]
        e = engs[rr_ctr[0] % len(engs)]
        rr_ctr[0] += 1
        if e == "act":
            return ACT(out, in_, AF.Copy, r, w)
        return A(e, "tensor_copy", r=r, w=w, out=out, in_=in_)

    with ExitStack() as pst:
        stg_f = [pst.enter_context(nc.sbuf_tensor("stgf%d" % i, [128, DFF], F32)) for i in range(2)]
        stg_b = [pst.enter_context(nc.sbuf_tensor("stgb%d" % i, [128, DFF], BF16)) for i in range(2)]
        cnt = [0]

        def cast_rows(name, rows, C, writer, wkey=None):
            for rc in range(rows // 128):
                s = cnt[0] % 2
                cnt[0] += 1
                src = DAP(wd_[name], rc * 128 * C, [[C, 128], [1, C]])
                DMA(stg_f[s][:, 0:C], src, r=[], w=["stgf%d" % s], key="stgf%d" % s)
                any_copy(stg_b[s][:, 0:C], stg_f[s][:, 0:C], r=["stgf%d" % s], w=["stgb%d" % s])
                outs = writer(rc, stg_b[s])
                Sd.add("sp", lambda e, outs=outs: [e.dma_start(out=o, in_=i) for (o, i) in outs],
                       r=["stgb%d" % s], w=[wkey or ("scr_" + name)], dma="stgo%d" % s, ndma=len(outs))

        def w_gu(name):
            def wr(dc, sb_):
                o = DAP(ws_[name], dc * 128, [[1024, 128], [131072, NFC], [1, 128]])
                return [(o, sb_[:, 0:DFF].rearrange("p (a b) -> p a b", b=128))]
            return wr

        def w_plain(name, C):
            def wr(rc, sb_):
                o = DAP(ws_[name], rc * 128 * C, [[C, 128], [1, C]])
                return [(o, sb_[:, 0:C])]
            return wr

        def w_in_wr(dc, sb_):
            res = []
            for gi_, (c0, gw) in enumerate(GROUPS):
                o = DAP(ws_in[gi_], dc * gw, [[8 * gw, 128], [1, gw]])
                res.append((o, sb_[:, c0:c0 + gw]))
            return res

        for f in ("ffn1", "ffn2"):
            cast_rows(f + "_w_gate", D, DFF, w_gu(f + "_w_gate"))
            cast_rows(f + "_w_up", D, DFF, w_gu(f + "_w_up"))
            cast_rows(f + "_w_down", DFF, D, w_plain(f + "_w_down", D))
        for gi_, (c0_, gw_) in enumerate(GROUPS):
            def wr_g(dc, sb_, gi_=gi_, gw_=gw_):
                return [(DAP(ws_in[gi_], dc * gw_, [[8 * gw_, 128], [1, gw_]]), sb_[:, 0:gw_])]
            cast_rows("w_in_g%d" % gi_, D, gw_, wr_g, wkey="scr_w_in")
        cast_rows("w_out", D, D, w_plain("w_out", D))
        for nm in ("cmp_w1_k", "cmp_w1_v", "mem_w_k", "mem_w_v"):
            cast_rows(nm, wnames[nm][0], 256, w_plain(nm, 256))
    Sd.barrier()
    WKEYS = ["scr_" + k for k in wnames]
    if dbg and "_dumpg" in dbg:
        g3raw = nc.dram_tensor("dbg_g3raw", [128, 4096], BF16, kind="ExternalOutput")
        DMA(g3raw.ap(), DAP(ws_in[dbg["_dumpg"]], 0, [[4096, 128], [1, 4096]]), r=["scr_w_in"], w=[], key="dbg_g3raw", is_out=True)

    xt = sbt("xt", [128, 4, D], F32)
    hT = sbt("hT", [128, 8, T], BF16)
    actT = sbt("actT", [128, 11, T], BF16)
    wgu = [sbt("wgu%d" % i, [128, 2, 8, 128], BF16) for i in range(2)]
    wdb = [sbt("wdb%d" % i, [128, D], BF16) for i in range(2)]
    winb = sbt("winb", [128, 8, 512], BF16)
    KTs = sbt("KTs", [128, S], BF16)
    Vs = sbt("Vs", [128, NCH, 192], BF16)
    KTw = sbt("KTw", [128, 2, T], BF16)
    Vw = sbt("Vw", [128, 8, 192], BF16)
    KcT = sbt("KcT", [128, 512], BF16)
    Vc = sbt("Vc", [128, 4, 192], BF16)
    QT = sbt("QT", [128, 4, T], BF16)
    QmT = sbt("QmT", [128, 2, T], BF16)
    KmT = sbt("KmT", [128, 2, 256], BF16)
    Vm = sbt("Vm", [128, 2, 2, 192], BF16)
    NET = 6
    ETb = [sbt("ET%d" % i, [128, T], BF16) for i in range(NET)]
    YT = sbt("YT", [128, 4, T], F32)
    YM = sbt("YM", [128, 2, T], F32)
    tA = sbt("tA", [128, T], F32)
    tB = sbt("tB", [128, T], F32)
    tC = sbt("tC", [128, T], F32)
    tD = sbt("tD", [128, T], F32)
    tE = sbt("tE", [128, T], F32)
    bA = sbt("bA", [128, T], BF16)
    bB = sbt("bB", [128, T], BF16)
    bC = sbt("bC", [128, T], BF16)
    NMT = sbt("NMT", [32, 2, 4, T], BF16)
    impS = sbt("impS", [128, 2, 4, 128], F32)
    tk1 = sbt("tk1", [128, 128], F32)
    tk2 = sbt("tk2", [128, 128], F32)
    tkm = sbt("tkm", [128, 16], F32)
    negm = sbt("negm", [128, 128], BF16)
    sm1 = sbt("sm1", [128, 64], F32)
    sm2 = sbt("sm2", [128, 64], F32)
    sm3 = sbt("sm3", [128, 64], F32)
    Rk = [sbt("Rk%d" % g, [128, 530], BF16) for g in range(2)]
    Rv = [sbt("Rv%d" % g, [128, 530], BF16) for g in range(2)]
    hidT = sbt("hidT", [128, 2, 2, 2, 32], BF16)
    w2k = sbt("w2k", [128, 2, 64], BF16)
    w2v = sbt("w2v", [128, 2, 64], BF16)
    b1 = sbt("b1", [128, 4], F32)
    posS = sbt("posS", [128, 2, 16], F32)
    posSb = sbt("posSb", [128, 2, 16], BF16)
    HQ = sbt("HQ", [128, 2, T], BF16)
    HK = sbt("HK", [128, 2, T], BF16)
    HG = sbt("HG", [128, 2, T], BF16)
    VB = sbt("VB", [128, 4, 256], BF16)
    KHs = [sbt("KH%d" % i, [128, 256], BF16) for i in range(4)]
    STb = sbt("STb", [128, 9, 2, 64], BF16)
    ST = sbt("ST", [128, 2, 64], F32)
    AT = sbt("AT", [128, 4, 256], BF16)
    dec = sbt("dec", [128, 4, 8], F32)
    GT = sbt("GT", [24, T], BF16)
    gsig = sbt("gsig", [128, 24], BF16)
    ssq = sbt("ssq", [128, 16], F32)
    rstd = sbt("rstd", [128, 16], F32)
    cs_c = sbt("cs_c", [128, 4, 8], F32)
    cs_s = sbt("cs_s", [128, 4, 8], F32)
    f32c = {}
    for k in ("c_ident", "c_tri", "c_upp"):
        f32c[k] = sbt("sb_" + k, [128, 128], F32)
    hmask = sbt("hmask", [128, 64], F32)
    keepw = sbt("keepw", [128, 256], F32)
    addw = sbt("addw", [128, 256], F32)
    identb = sbt("identb", [128, 128], BF16)
    causb = sbt("causb", [128, 128], BF16)
    winbias = sbt("winbias", [128, 128], BF16)
    cmpb = sbt("cmpb", [128, 4, 512], BF16)
    ovl = sbt("ovl", [128, 4, 130], BF16)
    esel = sbt("esel", [32, 16, 128], BF16)
    selg = sbt("selg", [24, 24, 64], BF16)
    bd64 = sbt("bd64", [128, 128], BF16)
    onesb = sbt("onesb", [128, 128], BF16)
    onesf = sbt("onesf", [128, 2], F32)
    gcol = sbt("gcol", [128, 4, 8], F32)
    gq = sbt("gq", [128, 64], F32)
    gk = sbt("gk", [128, 64], F32)
    gmq = sbt("gmq", [128, 64], F32)
    gmk = sbt("gmk", [128, 64], F32)
    gkcol = sbt("gkcol", [128, 1], F32)
    gnsa = sbt("gnsa", [128, 4], F32)
    ghg = sbt("ghg", [128, 2], F32)
    gmo = sbt("gmo", [128, 2], F32)
    LBt = sbt("LBt", [128, 2, 256], F32)
    LB0 = sbt("LB0", [128, 256], F32)
    LB1 = sbt("LB1", [128, 256], F32)

    stage_ctr = [0]

    def load_const(dst, src_ap, key, eng="sp"):
        DMA(dst, src_ap, r=[], w=[key], key="c_" + key, eng=eng)

    def load_const_bf(dst_bf, name, shape_free, key):
        npart = cshape[name][0]
        nfree = int(np.prod(cshape[name][1:]))
        src = DAP(cd_[name], 0, [[nfree, npart], [1, nfree]])
        DMA(tA[0:npart, 0:nfree] if nfree <= T else None, src, r=[], w=["tA"], key="c_stage")
        A("dve", "tensor_copy", r=["tA"], w=[key], out=dst_bf, in_=tA[0:npart, 0:nfree])

    for k in ("c_ident", "c_tri", "c_upp"):
        load_const(f32c[k][:], cd_[k].ap(), k)
    load_const(hmask[:], cd_["c_hmask"].ap(), "hmask")
    load_const(keepw[:], cd_["c_keepw"].ap(), "keepw")
    load_const(addw[:], cd_["c_addw"].ap(), "addw")
    A("dve", "tensor_copy", r=["c_ident"], w=["identb"], out=identb[:], in_=f32c["c_ident"][:])
    load_const_bf(causb[:], "c_caus", None, "causb")
    load_const_bf(winbias[:], "c_winb", None, "winbias")
    load_const_bf(bd64[:], "c_bd64", None, "bd64")
    for v in range(4):
        src = DAP(cd_["c_cmpb"], v * 512, [[2048, 128], [1, 512]])
        DMA(tA[:, :], src, r=[], w=["tA"], key="c_stage")
        A("dve", "tensor_copy", r=["tA"], w=["cmpb"], out=cmpb[:, v, :], in_=tA[:, :])
    for c4 in range(4):
        src = DAP(cd_["c_ovl"], c4 * 130, [[520, 128], [1, 130]])
        DMA(tA[:, 0:130], src, r=[], w=["tA"], key="c_stage")
        A("dve", "tensor_copy", r=["tA"], w=["ovl"], out=ovl[:, c4, :], in_=tA[:, 0:130])
    for c4 in range(4):
        src = DAP(cd_["c_esel"], c4 * 512, [[2048, 32], [1, 512]])
        DMA(tA[0:32, 0:512], src, r=[], w=["tA"], key="c_stage")
        A("dve", "tensor_copy", r=["tA"], w=["esel"], out=esel[:, c4 * 4:(c4 + 1) * 4, :].rearrange("p a b -> p (a b)"),
          in_=tA[0:32, 0:512])
    for c4 in range(3):
        src = DAP(cd_["c_selg"], c4 * 512, [[1536, 24], [1, 512]])
        DMA(tA[0:24, 0:512], src, r=[], w=["tA"], key="c_stage")
        A("dve", "tensor_copy", r=["tA"], w=["selg"], out=selg[:, c4 * 8:(c4 + 1) * 8, :].rearrange("p a b -> p (a b)"),
          in_=tA[0:24, 0:512])
    A("dve", "memset", w=["onesb"], ap=onesb[:], constant=1.0)
    A("dve", "memset", w=["onesf"], ap=onesf[:], constant=1.0)
    for gi, nm in enumerate(("ffn1_norm", "mix_norm", "ffn2_norm", "mem_norm")):
        load_const(gcol[:, gi, :], DAP(vd_[nm], 0, [[1, 128], [128, 8]]), "gcol", eng="pool")
    for tl, nm in ((gq, "nsa_q_norm"), (gk, "nsa_k_norm"), (gmq, "mem_q_norm"), (gmk, "mem_k_norm")):
        load_const(tl[:], DAP(vd_[nm], 0, [[0, 128], [1, 64]]), "g64_" + nm, eng="pool")
    for hh in range(2):
        load_const(gkcol[hh * 64:(hh + 1) * 64, :], DAP(vd_["nsa_k_norm"], 0, [[1, 64], [1, 1]]), "gkcol", eng="pool")
    for g in range(2):
        load_const(gnsa[g * 64:(g + 1) * 64, :], DAP(vd_["nsa_out_norm"], g * 256, [[1, 64], [64, 4]]), "gnsa", eng="pool")
    load_const(ghg[:], DAP(vd_["hgrn_out_norm"], 0, [[1, 128], [128, 2]]), "ghg", eng="pool")
    load_const(gmo[:], DAP(vd_["mem_out_norm"], 0, [[1, 128], [128, 2]]), "gmo", eng="pool")
    load_const(LBt[:].rearrange("p a b -> p (a b)"), DAP(lb_d, 0, [[0, 128], [1, 512]]), "LBt", eng="pool")
    A("dve", "tensor_tensor", r=["LBt"], w=["LB0"], out=LB0[:], in0=LBt[:, 1, :], in1=LBt[:, 0, :], op=ALU.subtract)
    ACT(LB0[:], LB0[:], AF.Exp, r=["LB0"], w=["LB0"])
    A("dve", "tensor_scalar", r=["LB0"], w=["LB0"], out=LB0[:], in0=LB0[:], scalar1=1.0, scalar2=None, op0=ALU.add)
    A("dve", "reciprocal", r=["LB0"], w=["LB0"], out=LB0[:], in_=LB0[:])
    A("dve", "tensor_scalar", r=["LB0"], w=["LB1"], out=LB1[:], in0=LB0[:], scalar1=-1.0, scalar2=1.0, op0=ALU.mult, op1=ALU.add)
    for (w2, w2d, key) in ((w2k, w2k_d, "w2k"), (w2v, w2v_d, "w2v")):
        DMA(tA[:, 0:128].rearrange("p (a b) -> p a b", b=64), DAP(w2d, 0, [[64, 128], [8192, 2], [1, 64]]), r=[], w=["tA"], key="c_stage")
        A("dve", "tensor_copy", r=["tA"], w=[key], out=w2[:], in_=tA[:, 0:128].rearrange("p (a b) -> p a b", b=64))
    for kv, pd in ((0, posk_d), (1, posv_d)):
        for par in range(2):
            Sd.add("pool", lambda e, kv=kv, pd=pd, par=par: e.dma_start(
                out=posS[par * 64:(par + 1) * 64, kv, :], in_=DAP(pd, par * 64, [[1, 64], [128, 16]]),
                allow_slow_non_contiguous=True), r=[], w=["posS"], dma="c_posS")
    A("dve", "tensor_copy", r=["posS"], w=["posSb"], out=posSb[:], in_=posS[:])
    A("pool", "memset", w=["Vs"], ap=Vs[:, :, 64:128], constant=1.0)
    A("pool", "memset", w=["Vw"], ap=Vw[:, :, 64:128], constant=1.0)
    A("pool", "memset", w=["Vc"], ap=Vc[:], constant=0.0)
    A("pool", "memset", w=["Vm"], ap=Vm[:, :, :, 64:128], constant=1.0)
    A("pool", "memset", w=["KcT"], ap=KcT[:], constant=0.0)
    for g in range(2):
        A("pool", "memset", w=["Rk%d" % g], ap=Rk[g][:], constant=0.0)
        A("pool", "memset", w=["Rv%d" % g], ap=Rv[g][:], constant=0.0)
    A("pool", "memset", w=["ST"], ap=ST[:], constant=0.0)
    A("pool", "memset", w=["STb"], ap=STb[:], constant=0.0)

    def norm_transpose(src, src_key, nsub, dstT, dst_key, gidx):
        for sub in range(nsub):
            ACT(tA[:, :].bitcast(BF16), src[:, sub, :], AF.Square, r=[src_key], w=["tA", "ssq"], accum_out=ssq[:, sub:sub + 1])
        ACT(rstd[:, 0:nsub], ssq[:, 0:nsub], AF.Ln, r=["ssq"], w=["rstd"], scale=1.0 / D, bias=eps_col[:, 0:1])
        ACT(rstd[:, 0:nsub], rstd[:, 0:nsub], AF.Exp, r=["rstd"], w=["rstd"], scale=-0.5)
        for sub in range(nsub):
            hb = bA if sub % 2 == 0 else bB
            hk = "bA" if sub % 2 == 0 else "bB"
            for half in range(2):
                A("dve", "tensor_scalar", r=[src_key, "rstd"], w=[hk], out=hb[:, :], in0=src[:, sub, half * 512:(half + 1) * 512],
                  scalar1=rstd[:, sub:sub + 1], scalar2=None, op0=ALU.mult)
                pi, pk = PA()
                for q4 in range(4):
                    TR(psum_b[pi][:, q4 * 128:(q4 + 1) * 128], hb[:, q4 * 128:(q4 + 1) * 128], identb[:], r=[hk, "identb"], w=[pk])
                A("dve", "tensor_tensor", r=[pk, "gcol"], w=[dst_key],
                  out=dstT[:, half * 4:(half + 1) * 4, sub * 128:(sub + 1) * 128],
                  in0=psum_b[pi][:, 0:512].rearrange("p (a b) -> p a b", b=128),
                  in1=gcol[:, gidx, half * 4:(half + 1) * 4].unsqueeze(2).broadcast_to([128, 4, 128]), op=ALU.mult)

    eps_col = sbt("eps_col", [128, 1], F32)
    A("dve", "memset", w=["eps_col"], ap=eps_col[:], constant=EPS)

    slot_ctr = {"wgu": 0, "wdb": 0}

    def ffn(pref, gidx):
        wg_s, wu_s, wd_s = ws_[pref + "_w_gate"], ws_[pref + "_w_up"], ws_[pref + "_w_down"]
        kg, ku, kd = "scr_" + pref + "_w_gate", "scr_" + pref + "_w_up", "scr_" + pref + "_w_down"
        norm_transpose(xt, "xt", 4, hT, "hT", gidx)
        for half in range(2):
            for fcl in range(11):
                fc = half * 11 + fcl
                s = slot_ctr["wgu"] % 2
                slot_ctr["wgu"] += 1
                wk = "wgu%d" % s
                Sd.add("sp", lambda e, s=s, fc=fc: [
                    e.dma_start(out=wgu[s][:, 0, :, :].rearrange("p a b -> p (a b)"), in_=DAP(wg_s, fc * 131072, [[1024, 128], [1, 1024]])),
                    e.dma_start(out=wgu[s][:, 1, :, :].rearrange("p a b -> p (a b)"), in_=DAP(wu_s, fc * 131072, [[1024, 128], [1, 1024]]))],
                    r=[kg, ku], w=[wk], dma=wk, ndma=2)
                pg, pgk = PA()
                pu, puk = PA()
                for dc in range(8):
                    MM(psum[pg][:, :], wgu[s][:, 0, dc, :], hT[:, dc, :], dc == 0, dc == 7, r=[wk, "hT"], w=[pgk])
                for dc in range(8):
                    MM(psum[pu][:, :], wgu[s][:, 1, dc, :], hT[:, dc, :], dc == 0, dc == 7, r=[wk, "hT"], w=[puk])
                sg = tB if fcl % 2 == 0 else tC
                sgk = "tB" if fcl % 2 == 0 else "tC"
                ACT(sg[:, :], psum[pg][:, :], AF.Silu, r=[pgk], w=[sgk])
                A("dve", "tensor_tensor", r=[sgk, puk], w=["actT"], out=actT[:, fcl, :], in0=sg[:, :], in1=psum[pu][:, :], op=ALU.mult)
            down_like(actT, "actT", 11, lambda fcl, half=half: [(slice(0, 128), DAP(wd_s, (half * 11 + fcl) * 128 * D, [[D, 128], [1, D]]))], kd, 0.5)

    def down_like(srcT, src_key, nchunk, wsrc, wkey, scale):
        for ps_ in range(2):
            banks = [PA() for _ in range(4)]
            for c in range(nchunk):
                s = slot_ctr["wdb"] % 2
                slot_ctr["wdb"] += 1
                wk = "wdb%d" % s
                parts = wsrc(c)
                Sd.add("sp", lambda e, s=s, parts=parts: [e.dma_start(out=wdb[s][sl, :], in_=ap) for (sl, ap) in parts],
                       r=[wkey], w=[wk], dma=wk, ndma=len(parts))
                for si in range(2):
                    sub = ps_ * 2 + si
                    for ch in range(2):
                        bi, bk = banks[si * 2 + ch]
                        MM(psum[bi][:, :], srcT[:, c, sub * 128:(sub + 1) * 128], wdb[s][:, ch * 512:(ch + 1) * 512],
                           c == 0, c == nchunk - 1, r=[src_key, wk], w=[bk])
            for si in range(2):
                sub = ps_ * 2 + si
                for ch in range(2):
                    bi, bk = banks[si * 2 + ch]
                    A("dve", "scalar_tensor_tensor", r=[bk, "xt"], w=["xt"], out=xt[:, sub, ch * 512:(ch + 1) * 512],
                      in0=psum[bi][:, :], scalar=scale, in1=xt[:, sub, ch * 512:(ch + 1) * 512], op0=ALU.mult, op1=ALU.add)

    def headnorm(src, src_key, nh, gain_t, gain_key, dst, dst_key):
        ACT(tE[:, 0:nh * 64].rearrange("p (a b) -> p a b", b=64), src, AF.Square, r=[src_key], w=["tE"])
        A("dve", "tensor_reduce", r=["tE"], w=["ssq"], out=ssq[:, 0:nh], in_=tE[:, 0:nh * 64].rearrange("p (a b) -> p a b", b=64),
          axis=AX.X, op=ALU.add)
        ACT(rstd[:, 0:nh], ssq[:, 0:nh], AF.Ln, r=["ssq"], w=["rstd"], scale=1.0 / 64, bias=eps_col[:, 0:1])
        ACT(rstd[:, 0:nh], rstd[:, 0:nh], AF.Exp, r=["rstd"], w=["rstd"], scale=-0.5)
        A("dve", "tensor_tensor", r=[src_key, "rstd"], w=[dst_key], out=dst, in0=src,
          in1=rstd[:, 0:nh].unsqueeze(2).broadcast_to([128, nh, 64]), op=ALU.mult)
        A("dve", "tensor_tensor", r=[dst_key, gain_key], w=[dst_key], out=dst, in0=dst,
          in1=gain_t[:, :].unsqueeze(1).broadcast_to([128, nh, 64]), op=ALU.mult)

    def rope(buf, key, nh, sub):
        c = cs_c[:, sub, :].unsqueeze(1).broadcast_to([128, nh, 8])
        s_ = cs_s[:, sub, :].unsqueeze(1).broadcast_to([128, nh, 8])
        x1 = buf[:, :, 0:8]
        x2 = buf[:, :, 8:16]
        v1 = sm1[:, 0:nh * 8].rearrange("p (a b) -> p a b", b=8)
        v2 = sm2[:, 0:nh * 8].rearrange("p (a b) -> p a b", b=8)
        v3 = sm3[:, 0:nh * 8].rearrange("p (a b) -> p a b", b=8)
        A("dve", "tensor_tensor", r=[key, "cs"], w=["sm1"], out=v1, in0=x1, in1=s_, op=ALU.mult)
        A("dve", "tensor_tensor", r=[key, "cs"], w=["sm2"], out=v2, in0=x2, in1=s_, op=ALU.mult)
        A("dve", "tensor_tensor", r=[key, "cs"], w=["sm3"], out=v3, in0=x1, in1=c, op=ALU.mult)
        A("dve", "tensor_tensor", r=["sm3", "sm2"], w=["sm3"], out=v3, in0=v3, in1=v2, op=ALU.subtract)
        A("dve", "tensor_tensor", r=[key, "cs"], w=["sm2"], out=v2, in0=x2, in1=c, op=ALU.mult)
        A("dve", "tensor_tensor", r=["sm2", "sm1"], w=[key], out=x2, in0=v2, in1=v1, op=ALU.add)
        A("dve", "tensor_copy", r=["sm3", key], w=[key], out=x1, in_=v3)

    DMA(xt[:, 0:2, :], DAP(mem_d, 0, [[D, 128], [128 * D, 2], [1, D]]), r=[], w=["xt"], key="xt")
    norm_transpose(xt, "xt", 2, hT, "hT", 3)
    for nm, dstk in (("mem_w_k", 0), ("mem_w_v", 1)):
        DMA(winb[:, :, 0:256], DAP(ws_[nm], 0, [[256, 128], [128 * 256, 8], [1, 256]]), r=["scr_" + nm], w=["winb"], key="winb")
        for sub in range(2):
            pi, pk = PA()
            for dc in range(8):
                MM(psum[pi][:, 0:256], hT[:, dc, sub * 128:(sub + 1) * 128], winb[:, dc, 0:256], dc == 0, dc == 7, r=["hT", "winb"], w=[pk])
            if dstk == 0:
                dstv = tD[:, 0:256].rearrange("p (a b) -> p a b", b=64)
                headnorm(psum[pi][:, 0:256].rearrange("p (a b) -> p a b", b=64), pk, 4, gmk, "g64_mem_k_norm", dstv, "tD")
                A("dve", "tensor_copy", r=["tD"], w=["bC"], out=bC[:, 0:256], in_=tD[:, 0:256])
                p2, p2k = PA()
                for pm in range(2):
                    TR(psum_b[p2][:, pm * 128:(pm + 1) * 128], bC[:, pm * 128:(pm + 1) * 128], identb[:], r=["bC", "identb"], w=[p2k])
                A("dve", "tensor_copy", r=[p2k], w=["KmT"], out=KmT[:, :, sub * 128:(sub + 1) * 128],
                  in_=psum_b[p2][:, 0:256].rearrange("p (a b) -> p a b", b=128))
            else:
                for hh_ in range(2):
                    A("dve", "tensor_copy", r=[pk], w=["Vm"], out=Vm[:, sub, :, hh_ * 128:hh_ * 128 + 64],
                      in_=psum[pi][:, 0:256].rearrange("p (a h d) -> p a h d", h=2, d=64)[:, :, hh_, :])
    for kv, nm in ((0, "cmp_w1_k"), (1, "cmp_w1_v")):
        DMA(winb[:, :, :].rearrange("p a b -> p (a b)").rearrange("p (a b) -> p a b", b=256),
            DAP(ws_[nm], 0, [[256, 128], [128 * 256, 16], [1, 256]]), r=["scr_" + nm], w=["winb"], key="winb")
        w1v_ = winb[:, :, :].rearrange("p a b -> p (a b)").rearrange("p (a b) -> p a b", b=256)
        pi, pk = PA()
        for hc_ in range(2):
            for a in range(16):
                MM(psum[pi][:, hc_ * 2:hc_ * 2 + 1], w1v_[:, a, hc_ * 128:(hc_ + 1) * 128], posSb[:, kv, a:a + 1], a == 0, a == 15,
                   r=["winb", "posSb"], w=[pk])
        A("dve", "tensor_copy", r=[pk], w=["b1"], out=b1[:, kv * 2:kv * 2 + 2], in_=psum[pi][:, 0:4:2])

    et_ctr = [0]

    def next_et():
        i = et_ctr[0] % NET
        et_ctr[0] += 1
        return ETb[i], "ET%d" % i

    def dbg_out(name, ap_sb, key, dram_ap):
        if dbg and name in dbg:
            DMA(dram_ap, ap_sb, r=[key], w=[], key="dbg_" + name, is_out=True)

    stop_after = (dbg or {}).get("_stop", None)

    for ti in range(NT):
        t0 = ti * T
        DMA(xt[:, :, :], DAP(x_d, t0 * D, [[D, 128], [128 * D, 4], [1, D]]), r=[], w=["xt"], key="xt")
        DMA(cs_c[:, :, :], DAP(cd_["c_cos"], t0 * 8, [[8, 128], [1024, 4], [1, 8]]), r=[], w=["cs"], key="cs")
        DMA(cs_s[:, :, :], DAP(cd_["c_sin"], t0 * 8, [[8, 128], [1024, 4], [1, 8]]), r=[], w=["cs"], key="cs")
        ffn("ffn1", 0)
        if dbg and "x1" in dbg:
            DMA(DAP(dbg_d["x1"], t0 * D, [[D, 128], [128 * D, 4], [1, D]]), xt[:, :, :], r=["xt"], w=[], key="dbg_x1", is_out=True)
        if stop_after == "ffn1":
            continue
        norm_transpose(xt, "xt", 4, hT, "hT", 1)
        wslot = ti % 2
        pproj = {}
        if stop_after == "mixnorm":
            continue
        for gi, (c0, gw) in enumerate(GROUPS):
            if dbg and "_maxg" in dbg and gi >= dbg["_maxg"]:
                continue
            if dbg and "_skipg" in dbg and gi in dbg["_skipg"]:
                continue
            DMA(winb[:, :, 0:gw], DAP(ws_in[(dbg or {}).get("_srcg", gi)], 0, [[8 * gw, 128], [gw, 8], [1, gw]]), r=["scr_w_in"], w=["winb"], key="winb")
            for sub in range(4):
                if dbg and "_nomm" in dbg:
                    continue
                if dbg and "_onlysub" in dbg and sub not in dbg["_onlysub"]:
                    continue
                pi, pk = PA()
                dcs = (dbg or {}).get("_dcs", list(range(8)))
                ncs = (dbg or {}).get("_ncs", 2)
                cw = (gw + ncs - 1) // ncs
                cw += cw % 2
                for c_lo in range(0, gw, cw):
                    c_hi = min(gw, c_lo + cw)
                    for dc in dcs:
                        MM(psum[pi][:, c_lo:c_hi], hT[:, dc, sub * 128:(sub + 1) * 128], winb[:, dc, c_lo:c_hi], dc == dcs[0], dc == dcs[-1], r=["hT", "winb"], w=[pk])
                P_ = psum[pi]
                tsl = slice(sub * 128, (sub + 1) * 128)
                if dbg and "_groups" in dbg and gi not in dbg["_groups"]:
                    continue
                if gi == 0:
                    qv = tD[:, :].rearrange("p (a b) -> p a b", b=64)
                    headnorm(P_[:, 0:512].rearrange("p (a b) -> p a b", b=64), pk, 8, gq, "g64_nsa_q_norm", qv, "tD")
                    rope(qv, "tD", 8, sub)
                    for g_ in range(2):
                        A("dve", "tensor_copy", r=["tD"], w=["bC"],
                          out=bC[:, :].rearrange("p (j g d) -> p j g d", g=2, d=64)[:, :, g_, :],
                          in_=tD[:, g_ * 256:(g_ + 1) * 256].rearrange("p (j d) -> p j d", d=64))
                    p2, p2k = PA()
                    for j in range(4):
                        TR(psum_b[p2][:, j * 128:(j + 1) * 128], bC[:, j * 128:(j + 1) * 128], identb[:], r=["bC", "identb"], w=[p2k])
                    any_copy(QT[:, :, tsl], psum_b[p2][:, 0:512].rearrange("p (a b) -> p a b", b=128), r=[p2k], w=["QT"], psum_src=True)
                elif gi == 1:
                    A("dve", "tensor_copy", r=[pk], w=["tD"], out=tD[:, 0:256], in_=P_[:, 0:256])
                    rope(tD[:, 0:128].rearrange("p (a b) -> p a b", b=64), "tD", 2, sub)
                    for dup in range(2):
                        A("dve", "tensor_copy", r=["tD"], w=["bC"],
                          out=bC[:, :].rearrange("p (a u d) -> p a u d", u=2, d=64)[:, :, dup, :],
                          in_=tD[:, 0:256].rearrange("p (a d) -> p a d", d=64))
                    p2, p2k = PA()
                    for q4 in range(4):
                        TR(psum_b[p2][:, q4 * 128:(q4 + 1) * 128], bC[:, q4 * 128:(q4 + 1) * 128], identb[:], r=["bC", "identb"], w=[p2k])
                    for q4 in range(4):
                        Rt = (Rk if q4 < 2 else Rv)[q4 % 2]
                        Rkey = ("Rk%d" if q4 < 2 else "Rv%d") % (q4 % 2)
                        m0 = 16 + sub * 128
                        any_copy(Rt[0:64, m0:m0 + 128], psum_b[p2][0:64, q4 * 128:(q4 + 1) * 128], r=[p2k], w=[Rkey], psum_src=True)
                        any_copy(Rt[64:128, m0 - 1:m0 + 127], psum_b[p2][64:128, q4 * 128:(q4 + 1) * 128], r=[p2k], w=[Rkey], psum_src=True)
                    kv_ = tD[:, 256:384].rearrange("p (a b) -> p a b", b=64)
                    headnorm(P_[:, 256:384].rearrange("p (a b) -> p a b", b=64), pk, 2, gk, "g64_nsa_k_norm", kv_, "tD")
                    rope(kv_, "tD", 2, sub)
                    A("dve", "tensor_copy", r=["tD"], w=["bC"], out=bC[:, 256:384], in_=tD[:, 256:384])
                    p3, p3k = PA()
                    TR(psum_b[p3][:, 0:128], bC[:, 256:384], identb[:], r=["bC", "identb"], w=[p3k])
                    any_copy(KTs[:, t0 + sub * 128:t0 + (sub + 1) * 128], psum_b[p3][:, 0:128], r=[p3k], w=["KTs"], psum_src=True)
                    cchunk = ti * 4 + sub
                    any_copy(Vs[:, cchunk, 0:64], P_[:, 384:448], r=[pk], w=["Vs"], psum_src=True)
                    any_copy(Vs[:, cchunk, 128:192], P_[:, 448:512], r=[pk], w=["Vs"], psum_src=True)
                elif gi == 2:
                    kv_ = tD[:, 0:128].rearrange("p (a b) -> p a b", b=64)
                    headnorm(P_[:, 0:128].rearrange("p (a b) -> p a b", b=64), pk, 2, gk, "g64_nsa_k_norm", kv_, "tD")
                    rope(kv_, "tD", 2, sub)
                    A("dve", "tensor_copy", r=["tD"], w=["bC"], out=bC[:, 0:128], in_=tD[:, 0:128])
                    ACT(gsig[:, :], P_[:, 256:280], AF.Sigmoid, r=[pk], w=["gsig"])
                    p3, p3k = PA()
                    TR(psum_b[p3][:, 0:128], bC[:, 0:128], identb[:], r=["bC", "identb"], w=[p3k])
                    TR(psum_b[p3][0:24, 128:256], gsig[:, :], identb[:], r=["gsig", "identb"], w=[p3k])
                    any_copy(KTw[:, wslot, tsl], psum_b[p3][:, 0:128], r=[p3k], w=["KTw"], psum_src=True)
                    any_copy(GT[:, tsl], psum_b[p3][0:24, 128:256], r=[p3k], w=["GT"], psum_src=True)
                    wch = wslot * 4 + sub
                    any_copy(Vw[:, wch, 0:64], P_[:, 128:192], r=[pk], w=["Vw"], psum_src=True)
                    any_copy(Vw[:, wch, 128:192], P_[:, 192:256], r=[pk], w=["Vw"], psum_src=True)
                elif gi == 3:
                    pproj[(3, sub)] = (pi, pk)
                    ACT(tB[:, 0:256], P_[:, 256:512], AF.Sigmoid, r=[pk], w=["tB"])
                    A("dve", "tensor_tensor", r=["tB", "LB1"], w=["tB"], out=tB[:, 0:256], in0=tB[:, 0:256], in1=LB1[:, :], op=ALU.mult)
                    A("dve", "tensor_tensor", r=["tB", "LB0"], w=["tB"], out=tB[:, 0:256], in0=tB[:, 0:256], in1=LB0[:, :], op=ALU.add)
                    ACT(tC[:, 0:256], tB[:, 0:256], AF.Ln, r=["tB"], w=["tC"])
                    A("dve", "tensor_scalar", r=["tB"], w=["tB"], out=tB[:, 0:256], in0=tB[:, 0:256], scalar1=-1.0, scalar2=1.0,
                      op0=ALU.mult, op1=ALU.add)
                    pbk_i, pbk = PA()
                    MM(psum[pbk_i][:, 0:256], f32c["c_tri"][:], tC[:, 0:256], True, True, r=["c_tri", "tC"], w=[pbk])
                    MM(psum[pbk_i][:, 256:512], f32c["c_upp"][:], tC[:, 0:256], True, True, r=["c_upp", "tC"], w=[pbk])
                    pdk_i, pdk = PA()
                    for ch in range(2):
                        for pp in range(2):
                            col = (ch * 2 + pp) * 2
                            MM(psum[pdk_i][:, col:col + 2], tC[ch * 64:(ch + 1) * 64, pp * 128:(pp + 1) * 128], onesf[ch * 64:(ch + 1) * 64, 0:2],
                               True, True, r=["tC", "onesf"], w=[pdk], force=True)
                    ACT(dec[:, sub, :], psum[pdk_i][:, 0:8], AF.Exp, r=[pdk], w=["dec"])
                    ACT(tC[:, 256:512], psum[pbk_i][:, 0:256], AF.Exp, r=[pbk, "tC"], w=["tC"])
                    ACT(tE[:, 0:256], psum[pbk_i][:, 0:256], AF.Exp, r=[pbk], w=["tE"], scale=-1.0)
                    ACT(tE[:, 256:512], psum[pbk_i][:, 256:512], AF.Exp, r=[pbk], w=["tE"])
                    ACT(tC[:, 0:256], P_[:, 0:256], AF.Silu, r=[pk, "tC"], w=["tC"])
                    A("dve", "scalar_tensor_tensor", r=["tC"], w=["bC"], out=bC[:, 0:256], in0=tC[:, 0:256], scalar=0.125, in1=tC[:, 256:512],
                      op0=ALU.mult, op1=ALU.mult)
                    A("dve", "tensor_tensor", r=["tB", "tE"], w=["bC"], out=bC[:, 256:512], in0=tB[:, 0:256], in1=tE[:, 0:256], op=ALU.mult)
                    A("dve", "tensor_tensor", r=["tB", "tE"], w=["KH%d" % sub], out=KHs[sub][:, :], in0=tB[:, 0:256], in1=tE[:, 256:512], op=ALU.mult)
                    p3, p3k = PA()
                    for q4 in range(4):
                        TR(psum_b[p3][:, q4 * 128:(q4 + 1) * 128], bC[:, q4 * 128:(q4 + 1) * 128], identb[:], r=["bC", "identb"], w=[p3k])
                    any_copy(HQ[:, :, tsl], psum_b[p3][:, 0:256].rearrange("p (a b) -> p a b", b=128), r=[p3k], w=["HQ"], psum_src=True)
                    any_copy(HK[:, :, tsl], psum_b[p3][:, 256:512].rearrange("p (a b) -> p a b", b=128), r=[p3k], w=["HK"], psum_src=True)
                elif gi == 4:
                    any_copy(VB[:, sub, :], P_[:, 0:256], r=[pk], w=["VB"], psum_src=True)
                    ACT(bC[:, 0:256], P_[:, 256:512], AF.Silu, r=[pk], w=["bC"])
                    p3, p3k = PA()
                    for q4 in range(2):
                        TR(psum_b[p3][:, q4 * 128:(q4 + 1) * 128], bC[:, q4 * 128:(q4 + 1) * 128], identb[:], r=["bC", "identb"], w=[p3k])
                    any_copy(HG[:, :, tsl], psum_b[p3][:, 0:256].rearrange("p (a b) -> p a b", b=128), r=[p3k], w=["HG"], psum_src=True)
                else:
                    dstv = tD[:, 0:256].rearrange("p (a b) -> p a b", b=64)
                    headnorm(P_[:, 0:256].rearrange("p (a b) -> p a b", b=64), pk, 4, gmq, "g64_mem_q_norm", dstv, "tD")
                    A("dve", "tensor_copy", r=["tD"], w=["bC"], out=bC[:, 0:256], in_=tD[:, 0:256])
                    p3, p3k = PA()
                    for pm in range(2):
                        TR(psum_b[p3][:, pm * 128:(pm + 1) * 128], bC[:, pm * 128:(pm + 1) * 128], identb[:], r=["bC", "identb"], w=[p3k])
                    any_copy(QmT[:, :, tsl], psum_b[p3][:, 0:256].rearrange("p (a b) -> p a b", b=128), r=[p3k], w=["QmT"], psum_src=True)

        if stop_after == "proj":
            continue
        for kv, nm in ((0, "cmp_w1_k"), (1, "cmp_w1_v")):
            DMA(winb[:, :, :].rearrange("p a b -> p (a b)").rearrange("p (a b) -> p a b", b=256),
                DAP(ws_[nm], 0, [[256, 128], [128 * 256, 16], [1, 256]]), r=["scr_" + nm], w=["winb"], key="winb")
            w1v_ = winb[:, :, :].rearrange("p a b -> p (a b)").rearrange("p (a b) -> p a b", b=256)
            R_ = Rk if kv == 0 else Rv
            ph, phk = PA()
            for g in range(2):
                Rkey = ("Rk%d" if kv == 0 else "Rv%d") % g
                for hc_ in range(2):
                    col = (g * 2 + hc_) * 32
                    for a in range(16):
                        MM(psum[ph][:, col:col + 32], w1v_[:, a, hc_ * 128:(hc_ + 1) * 128], R_[g][:, 2 * a:2 * a + 16 * 31 + 1:16],
                           a == 0, a == 15, r=["winb", Rkey], w=[phk])
            for g in range(2):
                for hc_ in range(2):
                    col = (g * 2 + hc_) * 32
                    ACT(hidT[:, kv, g, hc_, :], psum[ph][:, col:col + 32], AF.Silu, r=[phk, "b1"], w=["hidT"],
                        bias=b1[:, kv * 2 + hc_:kv * 2 + hc_ + 1])
            if kv == 0:
                pk_i, pkk = PA()
                for g in range(2):
                    for hc_ in range(2):
                        MM(psum[pk_i][g * 64:(g + 1) * 64, 0:32], w2k[:, hc_, :], hidT[:, 0, g, hc_, :], hc_ == 0, hc_ == 1,
                           r=["w2k", "hidT"], w=[pkk], tile_position=(0, g * 64))
                A("dve", "tensor_copy", r=[pkk], w=["tD"], out=tD[:, 0:32], in_=psum[pk_i][:, 0:32])
                A("dve", "tensor_tensor", r=["tD"], w=["bC"], out=bC[:, 0:32], in0=tD[:, 0:32], in1=tD[:, 0:32], op=ALU.mult)
                pn, pnk = PA()
                MM(psum[pn][:, 0:32], bd64[:], bC[:, 0:32], True, True, r=["bd64", "bC"], w=[pnk])
                ACT(tD[:, 32:64], psum[pn][:, 0:32], AF.Ln, r=[pnk, "tD"], w=["tD"], bias=eps_col[:, 0:1])
                ACT(tD[:, 32:64], tD[:, 32:64], AF.Exp, r=["tD"], w=["tD"], scale=-0.5)
                A("dve", "scalar_tensor_tensor", r=["tD", "gkcol"], w=["KcT"], out=KcT[:, 32 * ti:32 * ti + 32], in0=tD[:, 0:32],
                  scalar=gkcol[:, 0:1], in1=tD[:, 32:64], op0=ALU.mult, op1=ALU.mult)
            else:
                pv_i, pvk = PA()
                base = 32 * (ti % 4)
                for g in range(2):
                    for hc_ in range(2):
                        MM(psum[pv_i][base:base + 32, g * 64:(g + 1) * 64], hidT[:, 1, g, hc_, :], w2v[:, hc_, :], hc_ == 0, hc_ == 1,
                           r=["w2v", "hidT"], w=[pvk], tile_position=(0, base))
                cch = ti // 4
                A("dve", "tensor_copy", r=[pvk], w=["Vc"], out=Vc[base:base + 32, cch, 0:64], in_=psum[pv_i][base:base + 32, 0:64])
                A("dve", "tensor_copy", r=[pvk], w=["Vc"], out=Vc[base:base + 32, cch, 128:192], in_=psum[pv_i][base:base + 32, 64:128])
                A("pool", "memset", r=[], w=["Vc"], ap=Vc[base:base + 32, cch, 64:128], constant=1.0)
                if ti == 0:
                    A("pool", "memset", r=[], w=["Vc"], ap=Vc[0:1, 0, :], constant=0.0)
        for g in range(2):
            A("pool", "tensor_copy", r=["Rk%d" % g], w=["Rk%d" % g], out=Rk[g][:, 0:16], in_=Rk[g][:, 512:528])
            A("pool", "tensor_copy", r=["Rv%d" % g], w=["Rv%d" % g], out=Rv[g][:, 0:16], in_=Rv[g][:, 512:528])

        if stop_after == "compress":
            continue
        def combine(ob, obk, g, dst, gate_idx, first, guard=False):
            vs, ds = g * 64, 64 - g * 64
            if guard:
                A("dve", "tensor_scalar", r=[obk], w=["tA"], out=tA[ds:ds + 64, :], in0=psum[ob][ds:ds + 64, :], scalar1=1e-30, scalar2=None, op0=ALU.max)
                A("dve", "reciprocal", r=["tA"], w=["tA"], out=tA[ds:ds + 64, :], in_=tA[ds:ds + 64, :])
            else:
                A("dve", "reciprocal", r=[obk], w=["tA"], out=tA[ds:ds + 64, :], in_=psum[ob][ds:ds + 64, :])
            if gate_idx is None:
                A("dve", "tensor_tensor", r=[obk, "tA"], w=["YM"], out=dst, in0=psum[ob][vs:vs + 64, :], in1=tA[ds:ds + 64, :], op=ALU.mult)
                return
            A("dve", "tensor_tensor", r=[obk, "tA"], w=["tB"], out=tB[vs:vs + 64, :], in0=psum[ob][vs:vs + 64, :], in1=tA[ds:ds + 64, :], op=ALU.mult)
            pi, pk = PA()
            MM(psum[pi][vs:vs + 64, :], selg[:, gate_idx, :], GT[:, :], True, True, r=["selg", "GT"], w=[pk])
            if first:
                A("dve", "tensor_tensor", r=["tB", pk], w=["YT"], out=dst, in0=tB[vs:vs + 64, :], in1=psum[pi][vs:vs + 64, :], op=ALU.mult)
            else:
                A("dve", "tensor_tensor", r=["tB", pk], w=["tC"], out=tC[vs:vs + 64, :], in0=tB[vs:vs + 64, :], in1=psum[pi][vs:vs + 64, :], op=ALU.mult)
                A("pool", "tensor_tensor", r=["tC", "YT"], w=["YT"], out=dst, in0=dst, in1=tC[vs:vs + 64, :], op=ALU.add)

        def v_lhsT(cache, pitch_, vcol, o_start, o_end, g):
            if g == 0:
                return bass.AP(cache, vcol, [[pitch_, 128], [o_end - vcol, 2], [1, 64]])
            return bass.AP(cache, o_start, [[pitch_, 128], [vcol - o_start, 2], [1, 64]])

        nch_c = ti // 4 + 1
        for j in range(4):
            for g in range(2):
                gs = slice(g * 64, (g + 1) * 64)
                ob, obk = PB()
                ets = []
                for c in range(nch_c):
                    last = (c == nch_c - 1)
                    pi, pk = PA()
                    MM(psum[pi][:, :], KcT[gs, c * 128:(c + 1) * 128], QT[gs, j, :], True, not last, r=["KcT", "QT"], w=[pk])
                    if last:
                        MM(psum[pi][:, :], identb[:], cmpb[:, ti % 4, :], False, True, r=["identb", "cmpb"], w=[pk], force=True)
                    et, etk = next_et()
                    ACT(et[:, :], psum[pi][:, :], AF.Exp, r=[pk], w=[etk], scale=0.125)
                    ets.append((et, etk))
                    lh = Vc[:, c, g * 64:g * 64 + 128]
                    MM(psum[ob][:, :], lh, et[:, :], c == 0, last, r=["Vc", etk], w=[obk])
                for sp in range(2):
                    pi, pk = PA()
                    for s2 in range(2):
                        sub = sp * 2 + s2
                        for c in range(nch_c):
                            et, etk = ets[c]
                            MM(psum[pi][:, s2 * 130:(s2 + 1) * 130], et[:, sub * 128:(sub + 1) * 128], ovl[:, c, :], c == 0, c == nch_c - 1,
                               r=[etk, "ovl"], w=[pk])
                    for s2 in range(2):
                        sub = sp * 2 + s2
                        A("dve", "tensor_scalar", r=[pk], w=["sm1"], out=sm1[:, 0:1], in0=psum[pi][:, s2 * 130 + 128:s2 * 130 + 129],
                          scalar1=1e-30, scalar2=None, op0=ALU.max)
                        A("dve", "reciprocal", r=["sm1"], w=["sm1"], out=sm1[:, 0:1], in_=sm1[:, 0:1])
                        if j == 0:
                            A("dve", "tensor_scalar", r=[pk, "sm1"], w=["impS"], out=impS[:, g, sub, :], in0=psum[pi][:, s2 * 130:s2 * 130 + 128],
                              scalar1=sm1[:, 0:1], scalar2=None, op0=ALU.mult)
                        else:
                            A("dve", "scalar_tensor_tensor", r=[pk, "sm1", "impS"], w=["impS"], out=impS[:, g, sub, :],
                              in0=psum[pi][:, s2 * 130:s2 * 130 + 128], scalar=sm1[:, 0:1], in1=impS[:, g, sub, :], op0=ALU.mult, op1=ALU.add)
                combine(ob, obk, g, YT[gs, j, :], (g * 4 + j) * 3 + 0, True, guard=True)

        if stop_after == "C":
            continue
        nm_ = (4 * ti + 3) // 16 + 1
        for g in range(2):
            for sub in range(4):
                off = 126 - 2 * (ti * 4 + sub)
                A("dve", "tensor_tensor", r=["impS", "keepw"], w=["tk1"], out=tk1[:, :], in0=impS[:, g, sub, :], in1=keepw[:, off:off + 128], op=ALU.mult)
                A("dve", "tensor_tensor", r=["tk1", "addw"], w=["tk1"], out=tk1[:, :], in0=tk1[:, :], in1=addw[:, off:off + 128], op=ALU.add)
                A("dve", "memset", r=["tk1"], w=["tk1"], ap=tk1[:, 0:1], constant=1e4)
                A("dve", "max", r=["tk1"], w=["tkm"], out=tkm[:, 0:8], in_=tk1[:, :])
                A("dve", "match_replace", r=["tk1", "tkm"], w=["tk2"], out=tk2[:, :], in_to_replace=tkm[:, 0:8], in_values=tk1[:, :], imm_value=-2.0)
                A("dve", "max", r=["tk2", "tkm"], w=["tkm"], out=tkm[:, 8:16], in_=tk2[:, :])
                A("dve", "tensor_scalar", r=["tk1", "tkm"], w=["negm"], out=negm[:, :], in0=tk1[:, :], scalar1=tkm[:, 15:16], scalar2=NEG,
                  op0=ALU.is_lt, op1=ALU.mult)
                pi, pk = PA()
                for m in range(nm_):
                    TR(psum_b[pi][0:32, m * 128:(m + 1) * 128], negm[:, m * 32:(m + 1) * 32], identb[:], r=["negm", "identb"], w=[pk])
                any_copy(NMT[:, g, 0:nm_, sub * 128:(sub + 1) * 128], psum_b[pi][0:32, 0:nm_ * 128].rearrange("p (a b) -> p a b", b=128),
                         r=[pk], w=["NMT"], psum_src=True)

        if stop_after == "K":
            continue
        wpitch = 8 * 128 + 128
        for j in range(4):
            for g in range(2):
                gs = slice(g * 64, (g + 1) * 64)
                ob, obk = PB()
                rs = [r for r in (-1, 0, -4, -3, -2, 1, 2, 3) if 4 * ti + r >= 0]
                pend = None
                for idx, r in enumerate(rs):
                    c = 4 * ti + r
                    slot, within = (c // 4) % 2, c % 4
                    kl = KTw[gs, slot, within * 128:(within + 1) * 128]
                    if r < 0:
                        qa, qb, bq, bias, bkey = 0, 128 * (r + 5), 128 * (r + 4), winbias, "winbias"
                    else:
                        qa, qb, bq, bias, bkey = 128 * r, 512, 128 * r, causb, "causb"
                    pi, pk = PA()
                    MM(psum[pi][:, bq:bq + 128], kl, QT[gs, j, bq:bq + 128], True, False, r=["KTw", "QT"], w=[pk])
                    MM(psum[pi][:, bq:bq + 128], identb[:], bias[:], False, True, r=["identb", bkey], w=[pk], force=True)
                    if r < 0 and bq > 0:
                        MM(psum[pi][:, 0:bq], kl, QT[gs, j, 0:bq], True, True, r=["KTw", "QT"], w=[pk], force=True)
                    if r >= 0 and bq + 128 < 512:
                        MM(psum[pi][:, bq + 128:512], kl, QT[gs, j, bq + 128:512], True, True, r=["KTw", "QT"], w=[pk], force=True)
                    et, etk = next_et()
                    ACT(et[:, qa:qb], psum[pi][:, qa:qb], AF.Exp, r=[pk], w=[etk], scale=0.125)
                    lh = Vw[:, slot * 4 + within, g * 64:g * 64 + 128]
                    if pend is not None:
                        MM(*pend[0], **pend[1])
                    pend = ((psum[ob][:, qa:qb], lh, et[:, qa:qb], idx == 0, idx == len(rs) - 1), dict(r=["Vw", etk], w=[obk]))
                MM(*pend[0], **pend[1])
                combine(ob, obk, g, YT[gs, j, :], (g * 4 + j) * 3 + 2, False)

        if stop_after == "W":
            continue
        for hm in range(4):
            pm, hh = hm // 2, hm % 2
            hs = slice(hh * 64, hh * 64 + 64)
            ob, obk = PB()
            for c in range(2):
                pi, pk = PA()
                MM(psum[pi][:, :], KmT[hs, pm, c * 128:(c + 1) * 128], QmT[hs, pm, :], True, True, r=["KmT", "QmT"], w=[pk])
                et, etk = next_et()
                ACT(et[:, :], psum[pi][:, :], AF.Exp, r=[pk], w=[etk], scale=0.125)
                lh = Vm[:, c, pm, hh * 64:hh * 64 + 128]
                MM(psum[ob][:, :], lh, et[:, :], c == 0, c == 1, r=["Vm", etk], w=[obk])
            combine(ob, obk, hh, YM[hs, pm, :], None, True)

        if stop_after == "M":
            continue
        A("pool", "tensor_copy", r=["STb"], w=["STb"], out=STb[:, 0, :, :], in_=STb[:, 8, :, :])
        pos_ = [PB(), PB()]
        for sub in range(4):
            pU, pUk = PA()
            for ch in range(2):
                for pp in range(2):
                    col = ch * 2 + pp
                    MM(psum[pU][:, col * 128:(col + 1) * 128], KHs[sub][ch * 64:(ch + 1) * 64, pp * 128:(pp + 1) * 128],
                       VB[ch * 64:(ch + 1) * 64, sub, pp * 128:(pp + 1) * 128], True, True, r=["KH%d" % sub, "VB"], w=[pUk], force=True)
            pa_, pak = PA()
            for ch in range(2):
                c = sub * 2 + ch
                for pp in range(2):
                    for hh in range(2):
                        hs = slice(hh * 64, hh * 64 + 64)
                        MM(psum[pa_][ch * 64:(ch + 1) * 64, (pp * 2 + hh) * 64:(pp * 2 + hh + 1) * 64], HK[hs, pp, c * 64:(c + 1) * 64],
                           HQ[hs, pp, c * 64:(c + 1) * 64], True, True, r=["HK", "HQ"], w=[pak], force=True)
            A("dve", "tensor_tensor", r=[pak, "hmask"], w=["AT"], out=AT[:, sub, :].rearrange("p (a b) -> p a b", b=64),
              in0=psum[pa_][:, 0:256].rearrange("p (a b) -> p a b", b=64), in1=hmask[:, :].unsqueeze(1).broadcast_to([128, 4, 64]), op=ALU.mult)
            for ch in range(2):
                c = sub * 2 + ch
                for pp in range(2):
                    for hh in range(2):
                        hs = slice(hh * 64, hh * 64 + 64)
                        cc_ = slice(c * 64, (c + 1) * 64)
                        MM(psum[pos_[pp][0]][hs, cc_], STb[hs, c, pp, :], HQ[hs, pp, cc_], True, False, r=["STb", "HQ"], w=[pos_[pp][1]], force=True)
                        MM(psum[pos_[pp][0]][hs, cc_], VB[ch * 64:(ch + 1) * 64, sub, pp * 128 + hh * 64:pp * 128 + hh * 64 + 64],
                           AT[ch * 64:(ch + 1) * 64, sub, (pp * 2 + hh) * 64:(pp * 2 + hh + 1) * 64], False, True, r=["VB", "AT"], w=[pos_[pp][1]], force=True)
                for pp in range(2):
                    col = ch * 2 + pp
                    for hh in range(2):
                        hs = slice(hh * 64, hh * 64 + 64)
                        A("dve", "scalar_tensor_tensor", r=["ST", pUk, "dec"], w=["ST"], out=ST[hs, pp, :], in0=ST[hs, pp, :],
                          scalar=dec[hs, sub, col * 2:col * 2 + 1], in1=psum[pU][hs, col * 128 + hh * 64:col * 128 + hh * 64 + 64],
                          op0=ALU.mult, op1=ALU.add)
                A("pool", "tensor_copy", r=["ST", "STb"], w=["STb"], out=STb[:, c + 1, :, :], in_=ST[:, :, :])
        for pp in range(2):
            ob, obk = pos_[pp]
            ACT(bA[:, :], psum[ob][:, :], AF.Square, r=[obk], w=["bA"])
            pn, pnk = PA()
            MM(psum[pn][:, :], bd64[:], bA[:, :], True, True, r=["bd64", "bA"], w=[pnk])
            ACT(tA[:, :], psum[pn][:, :], AF.Ln, r=[pnk], w=["tA"], bias=eps_col[:, 0:1])
            ACT(tA[:, :], tA[:, :], AF.Exp, r=["tA"], w=["tA"], scale=-0.5)
            A("dve", "tensor_tensor", r=[obk, "tA"], w=["tB"], out=tB[:, :], in0=psum[ob][:, :], in1=tA[:, :], op=ALU.mult)
            A("dve", "scalar_tensor_tensor", r=["tB", "ghg", "HG"], w=["hT"], out=hT[:, 4 + pp, :], in0=tB[:, :], scalar=ghg[:, pp:pp + 1],
              in1=HG[:, pp, :], op0=ALU.mult, op1=ALU.mult)

        if stop_after == "H":
            continue
        spitch = NCH * 128 + 128
        nchs = 4 * ti + 4
        for j in range(4):
            for g in range(2):
                gs = slice(g * 64, (g + 1) * 64)
                ob, obk = PB()
                pend = None
                for c in range(nchs):
                    m, cc = c // 16, c % 16
                    r = c - 4 * ti
                    kl = KTs[gs, c * 128:(c + 1) * 128]
                    pi, pk = PA()
                    if r < 0:
                        qa = 0
                        MM(psum[pi][:, :], kl, QT[gs, j, :], True, False, r=["KTs", "QT"], w=[pk])
                        MM(psum[pi][:, :], esel[:, cc, :], NMT[:, g, m, :], False, True, r=["esel", "NMT"], w=[pk], force=True)
                    else:
                        qa = 128 * r
                        MM(psum[pi][:, qa:qa + 128], kl, QT[gs, j, qa:qa + 128], True, False, r=["KTs", "QT"], w=[pk])
                        MM(psum[pi][:, qa:qa + 128], esel[:, cc, :], NMT[:, g, m, qa:qa + 128], False, False, r=["esel", "NMT"], w=[pk], force=True)
                        MM(psum[pi][:, qa:qa + 128], identb[:], causb[:], False, True, r=["identb", "causb"], w=[pk], force=True)
                        if qa + 128 < 512:
                            MM(psum[pi][:, qa + 128:512], kl, QT[gs, j, qa + 128:512], True, False, r=["KTs", "QT"], w=[pk], force=True)
                            MM(psum[pi][:, qa + 128:512], esel[:, cc, :], NMT[:, g, m, qa + 128:512], False, True, r=["esel", "NMT"], w=[pk], force=True)
                    et, etk = next_et()
                    ACT(et[:, qa:512], psum[pi][:, qa:512], AF.Exp, r=[pk], w=[etk], scale=0.125)
                    lh = Vs[:, c, g * 64:g * 64 + 128]
                    if pend is not None:
                        MM(*pend[0], **pend[1])
                    pend = ((psum[ob][:, qa:512], lh, et[:, qa:512], c == 0, c == nchs - 1), dict(r=["Vs", etk], w=[obk]))
                MM(*pend[0], **pend[1])
                combine(ob, obk, g, YT[gs, j, :], (g * 4 + j) * 3 + 1, False)

        if stop_after == "S":
            continue
        pn, pnk = PA()
        for j in range(4):
            bx, bxk = (bA, "bA") if j % 2 == 0 else (bB, "bB")
            ACT(bx[:, :], YT[:, j, :], AF.Square, r=["YT"], w=[bxk])
            MM(psum[pn][:, :], onesb[:], bx[:, :], j == 0, j == 3, r=["onesb", bxk], w=[pnk])
        ACT(tA[:, :], psum[pn][:, :], AF.Ln, r=[pnk], w=["tA"], scale=1.0 / 512, bias=eps_col[:, 0:1])
        ACT(tA[:, :], tA[:, :], AF.Exp, r=["tA"], w=["tA"], scale=-0.5)
        for j in range(4):
            A("dve", "scalar_tensor_tensor", r=["YT", "gnsa", "tA"], w=["hT"], out=hT[:, j, :], in0=YT[:, j, :], scalar=gnsa[:, j:j + 1],
              in1=tA[:, :], op0=ALU.mult, op1=ALU.mult)
        pn, pnk = PA()
        for pm in range(2):
            bx, bxk = (bA, "bA") if pm % 2 == 0 else (bB, "bB")
            ACT(bx[:, :], YM[:, pm, :], AF.Square, r=["YM"], w=[bxk])
            MM(psum[pn][:, :], onesb[:], bx[:, :], pm == 0, pm == 1, r=["onesb", bxk], w=[pnk])
        ACT(tA[:, :], psum[pn][:, :], AF.Ln, r=[pnk], w=["tA"], scale=1.0 / 256, bias=eps_col[:, 0:1])
        ACT(tA[:, :], tA[:, :], AF.Exp, r=["tA"], w=["tA"], scale=-0.5)
        for pm in range(2):
            A("dve", "scalar_tensor_tensor", r=["YM", "gmo", "tA"], w=["hT"], out=hT[:, 6 + pm, :], in0=YM[:, pm, :], scalar=gmo[:, pm:pm + 1],
              in1=tA[:, :], op0=ALU.mult, op1=ALU.mult)
        if dbg and "mixT" in dbg:
            for c8 in range(8):
                A("dve", "tensor_copy", r=["hT"], w=["tD"], out=tD[:, :], in_=hT[:, c8, :])
                DMA(DAP(dbg_d["mixT"], c8 * 128 * S + t0, [[S, 128], [1, T]]), tD[:, :], r=["tD"], w=[], key="dbg_mixT", is_out=True)

        if stop_after == "norm":
            continue
        def wout_src(c):
            if c < 4:
                return [(slice(g * 64, g * 64 + 64), DAP(ws_["w_out"], ((g * 4 + c) * 64) * D, [[D, 64], [1, D]])) for g in range(2)]
            return [(slice(0, 128), DAP(ws_["w_out"], (512 + (c - 4) * 128) * D, [[D, 128], [1, D]]))]
        down_like(hT, "hT", 8, wout_src, "scr_w_out", 1.0)
        if dbg and "x2" in dbg:
            DMA(DAP(dbg_d["x2"], t0 * D, [[D, 128], [128 * D, 4], [1, D]]), xt[:, :, :], r=["xt"], w=[], key="dbg_x2", is_out=True)
        ffn("ffn2", 2)
        DMA(DAP(out_d, t0 * D, [[D, 128], [128 * D, 4], [1, D]]), xt[:, :, :], r=["xt"], w=[], key="out", is_out=True)

    Sd.finish()
    with nc.allow_non_contiguous_dma(reason="small constant / layout loads"):
        Sd.emit(st)
    return nc, st


_CACHE = {}


def _get(S):
    if S not in _CACHE:
        _CACHE[S] = build(S)
    return _CACHE[S]


def make_in_map(inputs, b, S):
    m = {"x": np.ascontiguousarray(inputs["x"][b]), "mem": np.ascontiguousarray(inputs["mem"][b])}
    for k in ("ffn1_w_gate", "ffn1_w_up", "ffn1_w_down", "ffn2_w_gate", "ffn2_w_up", "ffn2_w_down", "w_in", "w_out",
              "cmp_w1_k", "cmp_w1_v", "mem_w_k", "mem_w_v", "ffn1_norm", "mix_norm", "ffn2_norm", "mem_norm", "nsa_q_norm",
              "nsa_k_norm", "nsa_out_norm", "hgrn_out_norm", "mem_q_norm", "mem_k_norm", "mem_out_norm",
              "cmp_pos_k", "cmp_pos_v", "cmp_w2_k", "cmp_w2_v"):
        m[k] = np.ascontiguousarray(np.asarray(inputs[k])[0], dtype=np.float32)
    m["hgrn_lb_logits"] = np.ascontiguousarray(inputs["hgrn_lb_logits"], dtype=np.float32)
    w_in_full = m.pop("w_in")
    for gi_, (c0_, gw_) in enumerate(GROUPS):
        m["w_in_g%d" % gi_] = np.ascontiguousarray(w_in_full[:, c0_:c0_ + gw_])
    m.update(host_consts(S))
    return m


def kernel(**inputs):
    x = np.asarray(inputs["x"])
    B, S, _ = x.shape
    nc, _st = _get(S)
    in_maps = [make_in_map(inputs, b, S) for b in range(B)]
    res = run_bass_kernel_spmd(nc, in_maps, core_ids=list(range(B)))
    return np.stack([np.asarray(r["out"]) for r in res.results], axis=0).astype(np.float32)
```

```python
import numpy as np
from contextlib import ExitStack
import concourse.bass as bass
import concourse.mybir as mybir
from concourse.bass_utils import run_bass_kernel_spmd

F32 = mybir.dt.float32
BF16 = mybir.dt.bfloat16
AF = mybir.ActivationFunctionType
ALU = mybir.AluOpType
AX = mybir.AxisListType

D = 1024
DFF = 2816
NFC = 22
T = 512
INW = 2584
NEG = -30000.0
EPS = 1e-6
GROUPS = [(0, 512), (512, 512), (1024, 280), (1304, 512), (1816, 512), (2328, 256)]


class Op:
    __slots__ = ("eng", "idx", "fn", "deps", "signal", "sigval", "dma", "ndma", "dmaval", "name", "force")


class Sched:
    SAME_ENG_DIST = 4

    def __init__(self, nc):
        self.nc = nc
        self.engs = {"pe": nc.tensor, "act": nc.scalar, "dve": nc.vector, "pool": nc.gpsimd, "sp": nc.sync}
        self.ops = {e: [] for e in self.engs}
        self.last_w = {}
        self.rd_eng = {}
        self.rd_dma = {}
        self.dma_count = {}
        self.dma_last = {}
        self.pending_barrier = {}
        self.out_dma_ops = []

    def add(self, eng, fn, r=(), w=(), dma=None, ndma=1, name=None, is_out=False, force=False):
        op = Op()
        op.force = force
        op.eng = eng
        op.idx = len(self.ops[eng])
        op.fn = fn
        op.signal = False
        op.sigval = None
        op.dma = dma
        op.ndma = ndma
        op.dmaval = None
        op.name = name
        w = list(w) + [k for k in r if k.startswith("ps")]
        r = [k for k in r if not k.startswith("ps")]
        deps = []
        for k in r:
            p = self.last_w.get(k)
            if p is not None:
                deps.append(p)
        for k in w:
            p = self.last_w.get(k)
            if p is not None:
                deps.append(p)
            d = self.rd_eng.get(k)
            if d:
                deps.extend(d.values())
            d = self.rd_dma.get(k)
            if d:
                deps.extend(d)
        if eng in self.pending_barrier:
            deps.extend(self.pending_barrier.pop(eng))
        op.deps = self._filter(op, deps)
        if dma is not None:
            c = self.dma_count.get(dma, 0) + ndma
            self.dma_count[dma] = c
            op.dmaval = 16 * c
            self.dma_last[dma] = op
            if is_out:
                self.out_dma_ops.append(op)
        for k in r:
            if dma is not None:
                self.rd_dma.setdefault(k, []).append(op)
            else:
                self.rd_eng.setdefault(k, {})[eng] = op
        for k in w:
            self.last_w[k] = op
            self.rd_eng[k] = {}
            self.rd_dma[k] = []
        self.ops[eng].append(op)
        return op

    def _filter(self, op, deps):
        best_eng = {}
        best_dma = {}
        for p in deps:
            if p is op:
                continue
            if p.dma is not None:
                q = best_dma.get(p.dma)
                if q is None or p.dmaval > q.dmaval:
                    best_dma[p.dma] = p
                continue
            if p.eng == op.eng and op.dma is None:
                if op.eng == "pe" and not op.force:
                    continue
                if op.idx - p.idx > self.SAME_ENG_DIST:
                    continue
            q = best_eng.get(p.eng)
            if q is None or p.idx > q.idx:
                best_eng[p.eng] = p
        out = list(best_eng.values()) + list(best_dma.values())
        for p in out:
            if p.dma is None:
                p.signal = True
        return out

    def barrier(self):
        snap = []
        for e, lst in self.ops.items():
            for p in reversed(lst):
                if p.dma is None and p.fn is not None:
                    snap.append(p)
                    break
        snap.extend(self.dma_last.values())
        for e in self.engs:
            self.pending_barrier[e] = list(snap)

    def finish(self):
        op = Op()
        op.eng = "sp"
        op.idx = len(self.ops["sp"])
        op.fn = None
        op.signal = False
        op.dma = None
        op.ndma = 0
        op.dmaval = None
        op.name = "final"
        op.force = False
        op.sigval = None
        op.deps = self._filter(op, list(self.out_dma_ops))
        self.ops["sp"].append(op)

    def emit(self, stack):
        nc = self.nc
        for e, lst in self.ops.items():
            c = 0
            for p in lst:
                if p.signal:
                    c += 1
                    p.sigval = c
        esem = {e: stack.enter_context(nc.semaphore("s_" + e)) for e in self.engs if e != "sp"}
        dsem = {k: stack.enter_context(nc.semaphore("d_%d" % i)) for i, k in enumerate(self.dma_count)}

        def body(ename):
            def f(eh):
                wm = {}
                for p in self.ops[ename]:
                    for d in p.deps:
                        if d.dma is not None:
                            sem, val = dsem[d.dma], d.dmaval
                        else:
                            sem, val = esem[d.eng], d.sigval
                        key = id(sem)
                        if wm.get(key, 0) >= val:
                            continue
                        wm[key] = val
                        eh.wait_ge(sem, val)
                    if p.fn is None:
                        continue
                    ins = p.fn(eh)
                    if p.dma is not None:
                        if not isinstance(ins, (list, tuple)):
                            ins = [ins]
                        assert len(ins) == p.ndma, (p.name, len(ins), p.ndma)
                        for i_ in ins:
                            i_.then_inc(dsem[p.dma], 16)
                    elif p.signal:
                        if isinstance(ins, (list, tuple)):
                            ins = ins[-1]
                        ins.then_inc(esem[ename], 1)
            return f

        with nc.Block() as blk:
            blk.sync(body("sp"))
            blk.tensor(body("pe"))
            blk.scalar(body("act"))
            blk.vector(body("dve"))
            blk.gpsimd(body("pool"))


def host_consts(S):
    NT = S // T
    c = {}
    pos = np.arange(S, dtype=np.float32)
    inv = (np.float32(500000.0) ** (-(np.arange(0, 16, 2, dtype=np.float32) / np.float32(16)))).astype(np.float32)
    ang = (pos[:, None] * inv[None, :]).astype(np.float32)
    c["c_cos"] = np.cos(ang).astype(np.float32)
    c["c_sin"] = np.sin(ang).astype(np.float32)
    c["c_ident"] = np.eye(128, dtype=np.float32)
    p = np.arange(128)
    same = (p[:, None] // 64) == (p[None, :] // 64)
    c["c_tri"] = (same & (p[:, None] <= p[None, :])).astype(np.float32)
    c["c_upp"] = (same & (p[:, None] > p[None, :])).astype(np.float32)
    c["c_hmask"] = ((p[:, None] % 64) <= np.arange(64)[None, :]).astype(np.float32)
    kk = p[:, None]
    qq = p[None, :]
    c["c_caus"] = np.where(kk <= qq, 0.0, NEG).astype(np.float32)
    c["c_winb"] = np.where(qq < kk, 0.0, NEG).astype(np.float32)
    r = p[:, None, None]
    v = np.arange(4)[None, :, None]
    q = np.arange(512)[None, None, :]
    c["c_cmpb"] = np.where(16 * r + 15 <= 512 * v + q, 0.0, NEG).astype(np.float32)
    npr = np.arange(512)
    n = npr - 1
    j = np.arange(128)
    ovl = ((16 * n[:, None] < 64 * j[None, :] + 64) & (16 * n[:, None] + 32 > 64 * j[None, :])).astype(np.float32)
    ova = np.zeros((512, 130), np.float32)
    ova[:, :128] = ovl
    ova[:, 128] = 1.0
    ova[0, :] = 0.0
    c["c_ovl"] = ova.reshape(4, 128, 130).transpose(1, 0, 2).copy()
    m = np.arange(256)[None, :]
    hq = (p[:, None] >= 64).astype(np.int64)
    d = m - 126 - hq
    c["c_keepw"] = (d <= -2).astype(np.float32)
    c["c_addw"] = (np.where(d == 0, 2e4, 0.0) + np.where(d == -1, 4e4, 0.0) + np.where(d > 0, -1.0, 0.0)).astype(np.float32)
    jp = np.arange(32)[:, None, None]
    cc = np.arange(16)[None, :, None]
    k = np.arange(128)[None, None, :]
    c["c_esel"] = (jp == 2 * cc + k // 64).astype(np.float32)
    rr = np.arange(24)[:, None, None]
    hb = np.arange(24)[None, :, None]
    c["c_selg"] = np.broadcast_to((rr == hb), (24, 24, 64)).astype(np.float32).copy()
    c["c_bd64"] = (same.astype(np.float32) / 64.0).astype(np.float32)
    return c


CONST_SHAPES = None


def build(S, dbg=None):
    NT = S // T
    NCH = S // 128
    nc = bass.Bass("TRN2", target_bir_lowering=False)
    st = ExitStack()
    Sd = Sched(nc)

    def dram_in(name, shape, dt=F32):
        return nc.dram_tensor(name, list(shape), dt, kind="ExternalInput")

    def dram_scr(name, shape, dt=BF16):
        return nc.dram_tensor(name, list(shape), dt, kind="Internal")

    x_d = dram_in("x", [S, D])
    mem_d = dram_in("mem", [256, D])
    out_d = nc.dram_tensor("out", [S, D], F32, kind="ExternalOutput")
    wnames = {
        "ffn1_w_gate": [D, DFF], "ffn1_w_up": [D, DFF], "ffn1_w_down": [DFF, D],
        "ffn2_w_gate": [D, DFF], "ffn2_w_up": [D, DFF], "ffn2_w_down": [DFF, D],
        "w_in": [D, INW], "w_out": [D, D], "cmp_w1_k": [2048, 256], "cmp_w1_v": [2048, 256],
        "mem_w_k": [D, 256], "mem_w_v": [D, 256],
    }
    wd_ = {k: dram_in(k, v) for k, v in wnames.items() if k != "w_in"}
    for gi_, (c0_, gw_) in enumerate(GROUPS):
        wd_["w_in_g%d" % gi_] = dram_in("w_in_g%d" % gi_, [D, gw_])
    ws_ = {k: dram_scr("s_" + k, [v[0] * v[1]]) for k, v in wnames.items()}
    ws_in = [dram_scr("s_w_in_g%d" % gi_, [1024 * 512]) for gi_, (c0_, gw_) in enumerate(GROUPS)]
    vecs = {
        "ffn1_norm": D, "mix_norm": D, "ffn2_norm": D, "mem_norm": D, "nsa_q_norm": 64, "nsa_k_norm": 64,
        "nsa_out_norm": 512, "hgrn_out_norm": 256, "mem_q_norm": 64, "mem_k_norm": 64, "mem_out_norm": 256,
    }
    vd_ = {k: dram_in(k, [v]) for k, v in vecs.items()}
    lb_d = dram_in("hgrn_lb_logits", [2, 256])
    posk_d = dram_in("cmp_pos_k", [32, 64])
    posv_d = dram_in("cmp_pos_v", [32, 64])
    w2k_d = dram_in("cmp_w2_k", [256, 64])
    w2v_d = dram_in("cmp_w2_v", [256, 64])
    hc = host_consts(512)
    cshape = {k: v.shape for k, v in hc.items()}
    cshape["c_cos"] = (S, 8)
    cshape["c_sin"] = (S, 8)
    cd_ = {k: dram_in(k, v) for k, v in cshape.items()}
    dbg_d = {}
    if dbg:
        for k, shp in dbg.items():
            if k.startswith("_"):
                continue
            dbg_d[k] = nc.dram_tensor("dbg_" + k, list(shp), F32, kind="ExternalOutput")

    def DAP(t, off, pat):
        return bass.AP(t, off, [list(x) for x in pat])

    def sbt(name, shape, dt):
        return st.enter_context(nc.sbuf_tensor(name, list(shape), dt))

    psum = [st.enter_context(nc.psum_tensor("ps%d" % i, [128, 512], F32)) for i in range(8)]
    psum_b = [p.bitcast(BF16) for p in psum]
    pa_ctr = [0]
    pb_ctr = [0]

    def PA():
        i = pa_ctr[0] % 5
        pa_ctr[0] += 1
        return i, "ps%d" % i

    def PB():
        i = 5 + pb_ctr[0] % 3
        pb_ctr[0] += 1
        return i, "ps%d" % i

    def A(eng, meth, r=(), w=(), **kw):
        return Sd.add(eng, lambda e, kw=kw, meth=meth: getattr(e, meth)(**kw), r=r, w=w)

    def MM(out, lhsT, rhs, start, stop, r, w, force=False, **kw):
        return Sd.add("pe", lambda e: e.matmul(out, lhsT=lhsT, rhs=rhs, start=start, stop=stop, **kw), r=r, w=w, force=force)

    def TR(out, in_, ident, r, w):
        return Sd.add("pe", lambda e: e.transpose(out=out, in_=in_, identity=ident), r=r, w=w)

    def DMA(out, in_, r, w, key, eng="sp", is_out=False):
        return Sd.add(eng, lambda e: e.dma_start(out=out, in_=in_), r=r, w=w, dma=key, is_out=is_out)

    def ACT(out, in_, func, r, w, **kw):
        return Sd.add("act", lambda e: e.activation(out=out, in_=in_, func=func, **kw), r=r, w=w)

    rr_ctr = [0]

    def any_copy(out, in_, r, w, psum_src=False):
        engs = ["dve", "act"] if psum_src else ["dve", "act", "pool"]
        e = engs[rr_ctr[0] % len(engs)]
        rr_ctr[0] += 1
        if e == "act":
            return ACT(out, in_, AF.Copy, r, w)
        return A(e, "tensor_copy", r=r, w=w, out=out, in_=in_)

    with ExitStack() as pst:
        stg_f = [pst.enter_context(nc.sbuf_tensor("stgf%d" % i, [128, DFF], F32)) for i in range(2)]
        stg_b = [pst.enter_context(nc.sbuf_tensor("stgb%d" % i, [128, DFF], BF16)) for i in range(2)]
        cnt = [0]

        def cast_rows(name, rows, C, writer, wkey=None):
            for rc in range(rows // 128):
                s = cnt[0] % 2
                cnt[0] += 1
                src = DAP(wd_[name], rc * 128 * C, [[C, 128], [1, C]])
                DMA(stg_f[s][:, 0:C], src, r=[], w=["stgf%d" % s], key="stgf%d" % s)
                any_copy(stg_b[s][:, 0:C], stg_f[s][:, 0:C], r=["stgf%d" % s], w=["stgb%d" % s])
                outs = writer(rc, stg_b[s])
                Sd.add("sp", lambda e, outs=outs: [e.dma_start(out=o, in_=i) for (o, i) in outs],
                       r=["stgb%d" % s], w=[wkey or ("scr_" + name)], dma="stgo%d" % s, ndma=len(outs))

        def w_gu(name):
            def wr(dc, sb_):
                o = DAP(ws_[name], dc * 128, [[1024, 128], [131072, NFC], [1, 128]])
                return [(o, sb_[:, 0:DFF].rearrange("p (a b) -> p a b", b=128))]
            return wr

        def w_plain(name, C):
            def wr(rc, sb_):
                o = DAP(ws_[name], rc * 128 * C, [[C, 128], [1, C]])
                return [(o, sb_[:, 0:C])]
            return wr

        def w_in_wr(dc, sb_):
            res = []
            for gi_, (c0, gw) in enumerate(GROUPS):
                o = DAP(ws_in[gi_], dc * gw, [[8 * gw, 128], [1, gw]])
                res.append((o, sb_[:, c0:c0 + gw]))
            return res

        for f in ("ffn1", "ffn2"):
            cast_rows(f + "_w_gate", D, DFF, w_gu(f + "_w_gate"))
            cast_rows(f + "_w_up", D, DFF, w_gu(f + "_w_up"))
            cast_rows(f + "_w_down", DFF, D, w_plain(f + "_w_down", D))
        for gi_, (c0_, gw_) in enumerate(GROUPS):
            def wr_g(dc, sb_, gi_=gi_, gw_=gw_):
                return [(DAP(ws_in[gi_], dc * gw_, [[8 * gw_, 128], [1, gw_]]), sb_[:, 0:gw_])]
            cast_rows("w_in_g%d" % gi_, D, gw_, wr_g, wkey="scr_w_in")
        cast_rows("w_out", D, D, w_plain("w_out", D))
        for nm in ("cmp_w1_k", "cmp_w1_v", "mem_w_k", "mem_w_v"):
            cast_rows(nm, wnames[nm][0], 256, w_plain(nm, 256))
    Sd.barrier()
    WKEYS = ["scr_" + k for k in wnames]
    if dbg and "_dumpg" in dbg:
        g3raw = nc.dram_tensor("dbg_g3raw", [128, 4096], BF16, kind="ExternalOutput")
        DMA(g3raw.ap(), DAP(ws_in[dbg["_dumpg"]], 0, [[4096, 128], [1, 4096]]), r=["scr_w_in"], w=[], key="dbg_g3raw", is_out=True)

    xt = sbt("xt", [128, 4, D], F32)
    hT = sbt("hT", [128, 8, T], BF16)
    actT = sbt("actT", [128, 11, T], BF16)
    wgu = [sbt("wgu%d" % i, [128, 2, 8, 128], BF16) for i in range(3)]
    wdb = [sbt("wdb%d" % i, [128, D], BF16) for i in range(3)]
    winb = sbt("winb", [128, 8, 512], BF16)
    KTs = sbt("KTs", [128, S], BF16)
    Vs = sbt("Vs", [128, NCH, 192], BF16)
    KTw = sbt("KTw", [128, 2, T], BF16)
    Vw = sbt("Vw", [128, 8, 192], BF16)
    KcT = sbt("KcT", [128, 512], BF16)
    Vc = sbt("Vc", [128, 4, 192], BF16)
    QT = sbt("QT", [128, 4, T], BF16)
    QmT = sbt("QmT", [128, 2, T], BF16)
    KmT = sbt("KmT", [128, 2, 256], BF16)
    Vm = sbt("Vm", [128, 2, 2, 192], BF16)
    NET = 6
    ETb = [sbt("ET%d" % i, [128, T], BF16) for i in range(NET)]
    YT = sbt("YT", [128, 4, T], F32)
    YM = sbt("YM", [128, 2, T], F32)
    tA = sbt("tA", [128, T], F32)
    tB = sbt("tB", [128, T], F32)
    tC = sbt("tC", [128, T], F32)
    tD = sbt("tD", [128, T], F32)
    tE = sbt("tE", [128, T], F32)
    bA = sbt("bA", [128, T], BF16)
    bB = sbt("bB", [128, T], BF16)
    bC = sbt("bC", [128, T], BF16)
    NMT = sbt("NMT", [32, 2, 4, T], BF16)
    impS = sbt("impS", [128, 2, 4, 128], F32)
    tk1 = sbt("tk1", [128, 128], F32)
    tk2 = sbt("tk2", [128, 128], F32)
    tkm = sbt("tkm", [128, 16], F32)
    negm = sbt("negm", [128, 128], BF16)
    sm1 = sbt("sm1", [128, 64], F32)
    sm2 = sbt("sm2", [128, 64], F32)
    sm3 = sbt("sm3", [128, 64], F32)
    Rk = [sbt("Rk%d" % g, [128, 530], BF16) for g in range(2)]
    Rv = [sbt("Rv%d" % g, [128, 530], BF16) for g in range(2)]
    hidT = sbt("hidT", [128, 2, 2, 2, 32], BF16)
    w2k = sbt("w2k", [128, 2, 64], BF16)
    w2v = sbt("w2v", [128, 2, 64], BF16)
    b1 = sbt("b1", [128, 4], F32)
    posS = sbt("posS", [128, 2, 16], F32)
    posSb = sbt("posSb", [128, 2, 16], BF16)
    HQ = sbt("HQ", [128, 2, T], BF16)
    HK = sbt("HK", [128, 2, T], BF16)
    HG = sbt("HG", [128, 2, T], BF16)
    VB = sbt("VB", [128, 4, 256], BF16)
    KHs = [sbt("KH%d" % i, [128, 256], BF16) for i in range(4)]
    STb = sbt("STb", [128, 9, 2, 64], BF16)
    ST = sbt("ST", [128, 2, 64], F32)
    AT = sbt("AT", [128, 4, 256], BF16)
    dec = sbt("dec", [128, 4, 8], F32)
    GT = sbt("GT", [24, T], BF16)
    gsig = sbt("gsig", [128, 24], BF16)
    ssq = sbt("ssq", [128, 16], F32)
    rstd = sbt("rstd", [128, 16], F32)
    cs_c = sbt("cs_c", [128, 4, 8], F32)
    cs_s = sbt("cs_s", [128, 4, 8], F32)
    f32c = {}
    for k in ("c_ident", "c_tri", "c_upp"):
        f32c[k] = sbt("sb_" + k, [128, 128], F32)
    hmask = sbt("hmask", [128, 64], F32)
    keepw = sbt("keepw", [128, 256], F32)
    addw = sbt("addw", [128, 256], F32)
    identb = sbt("identb", [128, 128], BF16)
    causb = sbt("causb", [128, 128], BF16)
    winbias = sbt("winbias", [128, 128], BF16)
    cmpb = sbt("cmpb", [128, 4, 512], BF16)
    ovl = sbt("ovl", [128, 4, 130], BF16)
    esel = sbt("esel", [32, 16, 128], BF16)
    selg = sbt("selg", [24, 24, 64], BF16)
    bd64 = sbt("bd64", [128, 128], BF16)
    onesb = sbt("onesb", [128, 128], BF16)
    onesf = sbt("onesf", [128, 2], F32)
    gcol = sbt("gcol", [128, 4, 8], F32)
    gq = sbt("gq", [128, 64], F32)
    gk = sbt("gk", [128, 64], F32)
    gmq = sbt("gmq", [128, 64], F32)
    gmk = sbt("gmk", [128, 64], F32)
    gkcol = sbt("gkcol", [128, 1], F32)
    gnsa = sbt("gnsa", [128, 4], F32)
    ghg = sbt("ghg", [128, 2], F32)
    gmo = sbt("gmo", [128, 2], F32)
    LBt = sbt("LBt", [128, 2, 256], F32)
    LB0 = sbt("LB0", [128, 256], F32)
    LB1 = sbt("LB1", [128, 256], F32)

    stage_ctr = [0]

    def load_const(dst, src_ap, key, eng="sp"):
        DMA(dst, src_ap, r=[], w=[key], key="c_" + key, eng=eng)

    def load_const_bf(dst_bf, name, shape_free, key):
        npart = cshape[name][0]
        nfree = int(np.prod(cshape[name][1:]))
        src = DAP(cd_[name], 0, [[nfree, npart], [1, nfree]])
        DMA(tA[0:npart, 0:nfree] if nfree <= T else None, src, r=[], w=["tA"], key="c_stage")
        A("dve", "tensor_copy", r=["tA"], w=[key], out=dst_bf, in_=tA[0:npart, 0:nfree])

    for k in ("c_ident", "c_tri", "c_upp"):
        load_const(f32c[k][:], cd_[k].ap(), k)
    load_const(hmask[:], cd_["c_hmask"].ap(), "hmask")
    load_const(keepw[:], cd_["c_keepw"].ap(), "keepw")
    load_const(addw[:], cd_["c_addw"].ap(), "addw")
    A("dve", "tensor_copy", r=["c_ident"], w=["identb"], out=identb[:], in_=f32c["c_ident"][:])
    load_const_bf(causb[:], "c_caus", None, "causb")
    load_const_bf(winbias[:], "c_winb", None, "winbias")
    load_const_bf(bd64[:], "c_bd64", None, "bd64")
    for v in range(4):
        src = DAP(cd_["c_cmpb"], v * 512, [[2048, 128], [1, 512]])
        DMA(tA[:, :], src, r=[], w=["tA"], key="c_stage")
        A("dve", "tensor_copy", r=["tA"], w=["cmpb"], out=cmpb[:, v, :], in_=tA[:, :])
    for c4 in range(4):
        src = DAP(cd_["c_ovl"], c4 * 130, [[520, 128], [1, 130]])
        DMA(tA[:, 0:130], src, r=[], w=["tA"], key="c_stage")
        A("dve", "tensor_copy", r=["tA"], w=["ovl"], out=ovl[:, c4, :], in_=tA[:, 0:130])
    for c4 in range(4):
        src = DAP(cd_["c_esel"], c4 * 512, [[2048, 32], [1, 512]])
        DMA(tA[0:32, 0:512], src, r=[], w=["tA"], key="c_stage")
        A("dve", "tensor_copy", r=["tA"], w=["esel"], out=esel[:, c4 * 4:(c4 + 1) * 4, :].rearrange("p a b -> p (a b)"),
          in_=tA[0:32, 0:512])
    for c4 in range(3):
        src = DAP(cd_["c_selg"], c4 * 512, [[1536, 24], [1, 512]])
        DMA(tA[0:24, 0:512], src, r=[], w=["tA"], key="c_stage")
        A("dve", "tensor_copy", r=["tA"], w=["selg"], out=selg[:, c4 * 8:(c4 + 1) * 8, :].rearrange("p a b -> p (a b)"),
          in_=tA[0:24, 0:512])
    A("dve", "memset", w=["onesb"], ap=onesb[:], constant=1.0)
    A("dve", "memset", w=["onesf"], ap=onesf[:], constant=1.0)
    for gi, nm in enumerate(("ffn1_norm", "mix_norm", "ffn2_norm", "mem_norm")):
        load_const(gcol[:, gi, :], DAP(vd_[nm], 0, [[1, 128], [128, 8]]), "gcol", eng="pool")
    for tl, nm in ((gq, "nsa_q_norm"), (gk, "nsa_k_norm"), (gmq, "mem_q_norm"), (gmk, "mem_k_norm")):
        load_const(tl[:], DAP(vd_[nm], 0, [[0, 128], [1, 64]]), "g64_" + nm, eng="pool")
    for hh in range(2):
        load_const(gkcol[hh * 64:(hh + 1) * 64, :], DAP(vd_["nsa_k_norm"], 0, [[1, 64], [1, 1]]), "gkcol", eng="pool")
    for g in range(2):
        load_const(gnsa[g * 64:(g + 1) * 64, :], DAP(vd_["nsa_out_norm"], g * 256, [[1, 64], [64, 4]]), "gnsa", eng="pool")
    load_const(ghg[:], DAP(vd_["hgrn_out_norm"], 0, [[1, 128], [128, 2]]), "ghg", eng="pool")
    load_const(gmo[:], DAP(vd_["mem_out_norm"], 0, [[1, 128], [128, 2]]), "gmo", eng="pool")
    load_const(LBt[:].rearrange("p a b -> p (a b)"), DAP(lb_d, 0, [[0, 128], [1, 512]]), "LBt", eng="pool")
    A("dve", "tensor_tensor", r=["LBt"], w=["LB0"], out=LB0[:], in0=LBt[:, 1, :], in1=LBt[:, 0, :], op=ALU.subtract)
    ACT(LB0[:], LB0[:], AF.Exp, r=["LB0"], w=["LB0"])
    A("dve", "tensor_scalar", r=["LB0"], w=["LB0"], out=LB0[:], in0=LB0[:], scalar1=1.0, scalar2=None, op0=ALU.add)
    A("dve", "reciprocal", r=["LB0"], w=["LB0"], out=LB0[:], in_=LB0[:])
    A("dve", "tensor_scalar", r=["LB0"], w=["LB1"], out=LB1[:], in0=LB0[:], scalar1=-1.0, scalar2=1.0, op0=ALU.mult, op1=ALU.add)
    for (w2, w2d, key) in ((w2k, w2k_d, "w2k"), (w2v, w2v_d, "w2v")):
        DMA(tA[:, 0:128].rearrange("p (a b) -> p a b", b=64), DAP(w2d, 0, [[64, 128], [8192, 2], [1, 64]]), r=[], w=["tA"], key="c_stage")
        A("dve", "tensor_copy", r=["tA"], w=[key], out=w2[:], in_=tA[:, 0:128].rearrange("p (a b) -> p a b", b=64))
    for kv, pd in ((0, posk_d), (1, posv_d)):
        for par in range(2):
            Sd.add("pool", lambda e, kv=kv, pd=pd, par=par: e.dma_start(
                out=posS[par * 64:(par + 1) * 64, kv, :], in_=DAP(pd, par * 64, [[1, 64], [128, 16]]),
                allow_slow_non_contiguous=True), r=[], w=["posS"], dma="c_posS")
    A("dve", "tensor_copy", r=["posS"], w=["posSb"], out=posSb[:], in_=posS[:])
    A("pool", "memset", w=["Vs"], ap=Vs[:, :, 64:128], constant=1.0)
    A("pool", "memset", w=["Vw"], ap=Vw[:, :, 64:128], constant=1.0)
    A("pool", "memset", w=["Vc"], ap=Vc[:], constant=0.0)
    A("pool", "memset", w=["Vm"], ap=Vm[:, :, :, 64:128], constant=1.0)
    A("pool", "memset", w=["KcT"], ap=KcT[:], constant=0.0)
    for g in range(2):
        A("pool", "memset", w=["Rk%d" % g], ap=Rk[g][:], constant=0.0)
        A("pool", "memset", w=["Rv%d" % g], ap=Rv[g][:], constant=0.0)
    A("pool", "memset", w=["ST"], ap=ST[:], constant=0.0)
    A("pool", "memset", w=["STb"], ap=STb[:], constant=0.0)

    def norm_transpose(src, src_key, nsub, dstT, dst_key, gidx):
        for sub in range(nsub):
            ACT(tA[:, :].bitcast(BF16), src[:, sub, :], AF.Square, r=[src_key], w=["tA", "ssq"], accum_out=ssq[:, sub:sub + 1])
        ACT(rstd[:, 0:nsub], ssq[:, 0:nsub], AF.Ln, r=["ssq"], w=["rstd"], scale=1.0 / D, bias=eps_col[:, 0:1])
        ACT(rstd[:, 0:nsub], rstd[:, 0:nsub], AF.Exp, r=["rstd"], w=["rstd"], scale=-0.5)
        for sub in range(nsub):
            hb = bA if sub % 2 == 0 else bB
            hk = "bA" if sub % 2 == 0 else "bB"
            for half in range(2):
                A("dve", "tensor_scalar", r=[src_key, "rstd"], w=[hk], out=hb[:, :], in0=src[:, sub, half * 512:(half + 1) * 512],
                  scalar1=rstd[:, sub:sub + 1], scalar2=None, op0=ALU.mult)
                pi, pk = PA()
                for q4 in range(4):
                    TR(psum_b[pi][:, q4 * 128:(q4 + 1) * 128], hb[:, q4 * 128:(q4 + 1) * 128], identb[:], r=[hk, "identb"], w=[pk])
                A("dve", "tensor_tensor", r=[pk, "gcol"], w=[dst_key],
                  out=dstT[:, half * 4:(half + 1) * 4, sub * 128:(sub + 1) * 128],
                  in0=psum_b[pi][:, 0:512].rearrange("p (a b) -> p a b", b=128),
                  in1=gcol[:, gidx, half * 4:(half + 1) * 4].unsqueeze(2).broadcast_to([128, 4, 128]), op=ALU.mult)

    eps_col = sbt("eps_col", [128, 1], F32)
    A("dve", "memset", w=["eps_col"], ap=eps_col[:], constant=EPS)

    slot_ctr = {"wgu": 0, "wdb": 0}

    def ffn(pref, gidx):
        wg_s, wu_s, wd_s = ws_[pref + "_w_gate"], ws_[pref + "_w_up"], ws_[pref + "_w_down"]
        kg, ku, kd = "scr_" + pref + "_w_gate", "scr_" + pref + "_w_up", "scr_" + pref + "_w_down"
        norm_transpose(xt, "xt", 4, hT, "hT", gidx)
        for half in range(2):
            for fcl in range(11):
                fc = half * 11 + fcl
                s = slot_ctr["wgu"] % 3
                slot_ctr["wgu"] += 1
                wk = "wgu%d" % s
                Sd.add("sp", lambda e, s=s, fc=fc: [
                    e.dma_start(out=wgu[s][:, 0, :, :].rearrange("p a b -> p (a b)"), in_=DAP(wg_s, fc * 131072, [[1024, 128], [1, 1024]])),
                    e.dma_start(out=wgu[s][:, 1, :, :].rearrange("p a b -> p (a b)"), in_=DAP(wu_s, fc * 131072, [[1024, 128], [1, 1024]]))],
                    r=[kg, ku], w=[wk], dma=wk, ndma=2)
                pg, pgk = PA()
                pu, puk = PA()
                for dc in range(8):
                    MM(psum[pg][:, :], wgu[s][:, 0, dc, :], hT[:, dc, :], dc == 0, dc == 7, r=[wk, "hT"], w=[pgk])
                for dc in range(8):
                    MM(psum[pu][:, :], wgu[s][:, 1, dc, :], hT[:, dc, :], dc == 0, dc == 7, r=[wk, "hT"], w=[puk])
                sg = tB if fcl % 2 == 0 else tC
                sgk = "tB" if fcl % 2 == 0 else "tC"
                ACT(sg[:, :], psum[pg][:, :], AF.Silu, r=[pgk], w=[sgk])
                A("dve", "tensor_tensor", r=[sgk, puk], w=["actT"], out=actT[:, fcl, :], in0=sg[:, :], in1=psum[pu][:, :], op=ALU.mult)
            down_like(actT, "actT", 11, lambda fcl, half=half: [(slice(0, 128), DAP(wd_s, (half * 11 + fcl) * 128 * D, [[D, 128], [1, D]]))], kd, 0.5)

    def down_like(srcT, src_key, nchunk, wsrc, wkey, scale):
        for ps_ in range(2):
            banks = [PA() for _ in range(4)]
            for c in range(nchunk):
                s = slot_ctr["wdb"] % 3
                slot_ctr["wdb"] += 1
                wk = "wdb%d" % s
                parts = wsrc(c)
                Sd.add("sp", lambda e, s=s, parts=parts: [e.dma_start(out=wdb[s][sl, :], in_=ap) for (sl, ap) in parts],
                       r=[wkey], w=[wk], dma=wk, ndma=len(parts))
                for si in range(2):
                    sub = ps_ * 2 + si
                    for ch in range(2):
                        bi, bk = banks[si * 2 + ch]
                        MM(psum[bi][:, :], srcT[:, c, sub * 128:(sub + 1) * 128], wdb[s][:, ch * 512:(ch + 1) * 512],
                           c == 0, c == nchunk - 1, r=[src_key, wk], w=[bk])
            for si in range(2):
                sub = ps_ * 2 + si
                for ch in range(2):
                    bi, bk = banks[si * 2 + ch]
                    A("dve", "scalar_tensor_tensor", r=[bk, "xt"], w=["xt"], out=xt[:, sub, ch * 512:(ch + 1) * 512],
                      in0=psum[bi][:, :], scalar=scale, in1=xt[:, sub, ch * 512:(ch + 1) * 512], op0=ALU.mult, op1=ALU.add)

    def headnorm(src, src_key, nh, gain_t, gain_key, dst, dst_key):
        ACT(tE[:, 0:nh * 64].rearrange("p (a b) -> p a b", b=64), src, AF.Square, r=[src_key], w=["tE"])
        A("dve", "tensor_reduce", r=["tE"], w=["ssq"], out=ssq[:, 0:nh], in_=tE[:, 0:nh * 64].rearrange("p (a b) -> p a b", b=64),
          axis=AX.X, op=ALU.add)
        ACT(rstd[:, 0:nh], ssq[:, 0:nh], AF.Ln, r=["ssq"], w=["rstd"], scale=1.0 / 64, bias=eps_col[:, 0:1])
        ACT(rstd[:, 0:nh], rstd[:, 0:nh], AF.Exp, r=["rstd"], w=["rstd"], scale=-0.5)
        A("dve", "tensor_tensor", r=[src_key, "rstd"], w=[dst_key], out=dst, in0=src,
          in1=rstd[:, 0:nh].unsqueeze(2).broadcast_to([128, nh, 64]), op=ALU.mult)
        A("dve", "tensor_tensor", r=[dst_key, gain_key], w=[dst_key], out=dst, in0=dst,
          in1=gain_t[:, :].unsqueeze(1).broadcast_to([128, nh, 64]), op=ALU.mult)

    def rope(buf, key, nh, sub):
        c = cs_c[:, sub, :].unsqueeze(1).broadcast_to([128, nh, 8])
        s_ = cs_s[:, sub, :].unsqueeze(1).broadcast_to([128, nh, 8])
        x1 = buf[:, :, 0:8]
        x2 = buf[:, :, 8:16]
        v1 = sm1[:, 0:nh * 8].rearrange("p (a b) -> p a b", b=8)
        v2 = sm2[:, 0:nh * 8].rearrange("p (a b) -> p a b", b=8)
        v3 = sm3[:, 0:nh * 8].rearrange("p (a b) -> p a b", b=8)
        A("dve", "tensor_tensor", r=[key, "cs"], w=["sm1"], out=v1, in0=x1, in1=s_, op=ALU.mult)
        A("dve", "tensor_tensor", r=[key, "cs"], w=["sm2"], out=v2, in0=x2, in1=s_, op=ALU.mult)
        A("dve", "tensor_tensor", r=[key, "cs"], w=["sm3"], out=v3, in0=x1, in1=c, op=ALU.mult)
        A("dve", "tensor_tensor", r=["sm3", "sm2"], w=["sm3"], out=v3, in0=v3, in1=v2, op=ALU.subtract)
        A("dve", "tensor_tensor", r=[key, "cs"], w=["sm2"], out=v2, in0=x2, in1=c, op=ALU.mult)
        A("dve", "tensor_tensor", r=["sm2", "sm1"], w=[key], out=x2, in0=v2, in1=v1, op=ALU.add)
        A("dve", "tensor_copy", r=["sm3", key], w=[key], out=x1, in_=v3)

    DMA(xt[:, 0:2, :], DAP(mem_d, 0, [[D, 128], [128 * D, 2], [1, D]]), r=[], w=["xt"], key="xt")
    norm_transpose(xt, "xt", 2, hT, "hT", 3)
    for nm, dstk in (("mem_w_k", 0), ("mem_w_v", 1)):
        DMA(winb[:, :, 0:256], DAP(ws_[nm], 0, [[256, 128], [128 * 256, 8], [1, 256]]), r=["scr_" + nm], w=["winb"], key="winb")
        for sub in range(2):
            pi, pk = PA()
            for dc in range(8):
                MM(psum[pi][:, 0:256], hT[:, dc, sub * 128:(sub + 1) * 128], winb[:, dc, 0:256], dc == 0, dc == 7, r=["hT", "winb"], w=[pk])
            if dstk == 0:
                dstv = tD[:, 0:256].rearrange("p (a b) -> p a b", b=64)
                headnorm(psum[pi][:, 0:256].rearrange("p (a b) -> p a b", b=64), pk, 4, gmk, "g64_mem_k_norm", dstv, "tD")
                A("dve", "tensor_copy", r=["tD"], w=["bC"], out=bC[:, 0:256], in_=tD[:, 0:256])
                p2, p2k = PA()
                for pm in range(2):
                    TR(psum_b[p2][:, pm * 128:(pm + 1) * 128], bC[:, pm * 128:(pm + 1) * 128], identb[:], r=["bC", "identb"], w=[p2k])
                A("dve", "tensor_copy", r=[p2k], w=["KmT"], out=KmT[:, :, sub * 128:(sub + 1) * 128],
                  in_=psum_b[p2][:, 0:256].rearrange("p (a b) -> p a b", b=128))
            else:
                for hh_ in range(2):
                    A("dve", "tensor_copy", r=[pk], w=["Vm"], out=Vm[:, sub, :, hh_ * 128:hh_ * 128 + 64],
                      in_=psum[pi][:, 0:256].rearrange("p (a h d) -> p a h d", h=2, d=64)[:, :, hh_, :])
    for kv, nm in ((0, "cmp_w1_k"), (1, "cmp_w1_v")):
        DMA(winb[:, :, :].rearrange("p a b -> p (a b)").rearrange("p (a b) -> p a b", b=256),
            DAP(ws_[nm], 0, [[256, 128], [128 * 256, 16], [1, 256]]), r=["scr_" + nm], w=["winb"], key="winb")
        w1v_ = winb[:, :, :].rearrange("p a b -> p (a b)").rearrange("p (a b) -> p a b", b=256)
        pi, pk = PA()
        for hc_ in range(2):
            for a in range(16):
                MM(psum[pi][:, hc_ * 2:hc_ * 2 + 1], w1v_[:, a, hc_ * 128:(hc_ + 1) * 128], posSb[:, kv, a:a + 1], a == 0, a == 15,
                   r=["winb", "posSb"], w=[pk])
        A("dve", "tensor_copy", r=[pk], w=["b1"], out=b1[:, kv * 2:kv * 2 + 2], in_=psum[pi][:, 0:4:2])

    et_ctr = [0]

    def next_et():
        i = et_ctr[0] % NET
        et_ctr[0] += 1
        return ETb[i], "ET%d" % i

    def dbg_out(name, ap_sb, key, dram_ap):
        if dbg and name in dbg:
            DMA(dram_ap, ap_sb, r=[key], w=[], key="dbg_" + name, is_out=True)

    stop_after = (dbg or {}).get("_stop", None)

    for ti in range(NT):
        t0 = ti * T
        DMA(xt[:, :, :], DAP(x_d, t0 * D, [[D, 128], [128 * D, 4], [1, D]]), r=[], w=["xt"], key="xt")
        DMA(cs_c[:, :, :], DAP(cd_["c_cos"], t0 * 8, [[8, 128], [1024, 4], [1, 8]]), r=[], w=["cs"], key="cs")
        DMA(cs_s[:, :, :], DAP(cd_["c_sin"], t0 * 8, [[8, 128], [1024, 4], [1, 8]]), r=[], w=["cs"], key="cs")
        ffn("ffn1", 0)
        if dbg and "x1" in dbg:
            DMA(DAP(dbg_d["x1"], t0 * D, [[D, 128], [128 * D, 4], [1, D]]), xt[:, :, :], r=["xt"], w=[], key="dbg_x1", is_out=True)
        if stop_after == "ffn1":
            continue
        norm_transpose(xt, "xt", 4, hT, "hT", 1)
        wslot = ti % 2
        pproj = {}
        if stop_after == "mixnorm":
            continue
        for gi, (c0, gw) in enumerate(GROUPS):
            if dbg and "_maxg" in dbg and gi >= dbg["_maxg"]:
                continue
            if dbg and "_skipg" in dbg and gi in dbg["_skipg"]:
                continue
            DMA(winb[:, :, 0:gw], DAP(ws_in[(dbg or {}).get("_srcg", gi)], 0, [[8 * gw, 128], [gw, 8], [1, gw]]), r=["scr_w_in"], w=["winb"], key="winb")
            for sub in range(4):
                if dbg and "_nomm" in dbg:
                    continue
                if dbg and "_onlysub" in dbg and sub not in dbg["_onlysub"]:
                    continue
                pi, pk = PA()
                dcs = (dbg or {}).get("_dcs", list(range(8)))
                ncs = (dbg or {}).get("_ncs", 2)
                cw = (gw + ncs - 1) // ncs
                cw += cw % 2
                for c_lo in range(0, gw, cw):
                    c_hi = min(gw, c_lo + cw)
                    for dc in dcs:
                        MM(psum[pi][:, c_lo:c_hi], hT[:, dc, sub * 128:(sub + 1) * 128], winb[:, dc, c_lo:c_hi], dc == dcs[0], dc == dcs[-1], r=["hT", "winb"], w=[pk])
                P_ = psum[pi]
                tsl = slice(sub * 128, (sub + 1) * 128)
                if dbg and "_groups" in dbg and gi not in dbg["_groups"]:
                    continue
                if gi == 0:
                    qv = tD[:, :].rearrange("p (a b) -> p a b", b=64)
                    headnorm(P_[:, 0:512].rearrange("p (a b) -> p a b", b=64), pk, 8, gq, "g64_nsa_q_norm", qv, "tD")
                    rope(qv, "tD", 8, sub)
                    for g_ in range(2):
                        A("dve", "tensor_copy", r=["tD"], w=["bC"],
                          out=bC[:, :].rearrange("p (j g d) -> p j g d", g=2, d=64)[:, :, g_, :],
                          in_=tD[:, g_ * 256:(g_ + 1) * 256].rearrange("p (j d) -> p j d", d=64))
                    p2, p2k = PA()
                    for j in range(4):
                        TR(psum_b[p2][:, j * 128:(j + 1) * 128], bC[:, j * 128:(j + 1) * 128], identb[:], r=["bC", "identb"], w=[p2k])
                    any_copy(QT[:, :, tsl], psum_b[p2][:, 0:512].rearrange("p (a b) -> p a b", b=128), r=[p2k], w=["QT"], psum_src=True)
                elif gi == 1:
                    A("dve", "tensor_copy", r=[pk], w=["tD"], out=tD[:, 0:256], in_=P_[:, 0:256])
                    rope(tD[:, 0:128].rearrange("p (a b) -> p a b", b=64), "tD", 2, sub)
                    for dup in range(2):
                        A("dve", "tensor_copy", r=["tD"], w=["bC"],
                          out=bC[:, :].rearrange("p (a u d) -> p a u d", u=2, d=64)[:, :, dup, :],
                          in_=tD[:, 0:256].rearrange("p (a d) -> p a d", d=64))
                    p2, p2k = PA()
                    for q4 in range(4):
                        TR(psum_b[p2][:, q4 * 128:(q4 + 1) * 128], bC[:, q4 * 128:(q4 + 1) * 128], identb[:], r=["bC", "identb"], w=[p2k])
                    for q4 in range(4):
                        Rt = (Rk if q4 < 2 else Rv)[q4 % 2]
                        Rkey = ("Rk%d" if q4 < 2 else "Rv%d") % (q4 % 2)
                        m0 = 16 + sub * 128
                        any_copy(Rt[0:64, m0:m0 + 128], psum_b[p2][0:64, q4 * 128:(q4 + 1) * 128], r=[p2k], w=[Rkey], psum_src=True)
                        any_copy(Rt[64:128, m0 - 1:m0 + 127], psum_b[p2][64:128, q4 * 128:(q4 + 1) * 128], r=[p2k], w=[Rkey], psum_src=True)
                    kv_ = tD[:, 256:384].rearrange("p (a b) -> p a b", b=64)
                    headnorm(P_[:, 256:384].rearrange("p (a b) -> p a b", b=64), pk, 2, gk, "g64_nsa_k_norm", kv_, "tD")
                    rope(kv_, "tD", 2, sub)
                    A("dve", "tensor_copy", r=["tD"], w=["bC"], out=bC[:, 256:384], in_=tD[:, 256:384])
                    p3, p3k = PA()
                    TR(psum_b[p3][:, 0:128], bC[:, 256:384], identb[:], r=["bC", "identb"], w=[p3k])
                    any_copy(KTs[:, t0 + sub * 128:t0 + (sub + 1) * 128], psum_b[p3][:, 0:128], r=[p3k], w=["KTs"], psum_src=True)
                    cchunk = ti * 4 + sub
                    any_copy(Vs[:, cchunk, 0:64], P_[:, 384:448], r=[pk], w=["Vs"], psum_src=True)
                    any_copy(Vs[:, cchunk, 128:192], P_[:, 448:512], r=[pk], w=["Vs"], psum_src=True)
                elif gi == 2:
                    kv_ = tD[:, 0:128].rearrange("p (a b) -> p a b", b=64)
                    headnorm(P_[:, 0:128].rearrange("p (a b) -> p a b", b=64), pk, 2, gk, "g64_nsa_k_norm", kv_, "tD")
                    rope(kv_, "tD", 2, sub)
                    A("dve", "tensor_copy", r=["tD"], w=["bC"], out=bC[:, 0:128], in_=tD[:, 0:128])
                    ACT(gsig[:, :], P_[:, 256:280], AF.Sigmoid, r=[pk], w=["gsig"])
                    p3, p3k = PA()
                    TR(psum_b[p3][:, 0:128], bC[:, 0:128], identb[:], r=["bC", "identb"], w=[p3k])
                    TR(psum_b[p3][0:24, 128:256], gsig[:, :], identb[:], r=["gsig", "identb"], w=[p3k])
                    any_copy(KTw[:, wslot, tsl], psum_b[p3][:, 0:128], r=[p3k], w=["KTw"], psum_src=True)
                    any_copy(GT[:, tsl], psum_b[p3][0:24, 128:256], r=[p3k], w=["GT"], psum_src=True)
                    wch = wslot * 4 + sub
                    any_copy(Vw[:, wch, 0:64], P_[:, 128:192], r=[pk], w=["Vw"], psum_src=True)
                    any_copy(Vw[:, wch, 128:192], P_[:, 192:256], r=[pk], w=["Vw"], psum_src=True)
                elif gi == 3:
                    pproj[(3, sub)] = (pi, pk)
                    ACT(tB[:, 0:256], P_[:, 256:512], AF.Sigmoid, r=[pk], w=["tB"])
                    A("dve", "tensor_tensor", r=["tB", "LB1"], w=["tB"], out=tB[:, 0:256], in0=tB[:, 0:256], in1=LB1[:, :], op=ALU.mult)
                    A("dve", "tensor_tensor", r=["tB", "LB0"], w=["tB"], out=tB[:, 0:256], in0=tB[:, 0:256], in1=LB0[:, :], op=ALU.add)
                    ACT(tC[:, 0:256], tB[:, 0:256], AF.Ln, r=["tB"], w=["tC"])
                    A("dve", "tensor_scalar", r=["tB"], w=["tB"], out=tB[:, 0:256], in0=tB[:, 0:256], scalar1=-1.0, scalar2=1.0,
                      op0=ALU.mult, op1=ALU.add)
                    pbk_i, pbk = PA()
                    MM(psum[pbk_i][:, 0:256], f32c["c_tri"][:], tC[:, 0:256], True, True, r=["c_tri", "tC"], w=[pbk])
                    MM(psum[pbk_i][:, 256:512], f32c["c_upp"][:], tC[:, 0:256], True, True, r=["c_upp", "tC"], w=[pbk])
                    pdk_i, pdk = PA()
                    for ch in range(2):
                        for pp in range(2):
                            col = (ch * 2 + pp) * 2
                            MM(psum[pdk_i][:, col:col + 2], tC[ch * 64:(ch + 1) * 64, pp * 128:(pp + 1) * 128], onesf[ch * 64:(ch + 1) * 64, 0:2],
                               True, True, r=["tC", "onesf"], w=[pdk], force=True)
                    ACT(dec[:, sub, :], psum[pdk_i][:, 0:8], AF.Exp, r=[pdk], w=["dec"])
                    ACT(tC[:, 256:512], psum[pbk_i][:, 0:256], AF.Exp, r=[pbk, "tC"], w=["tC"])
                    ACT(tE[:, 0:256], psum[pbk_i][:, 0:256], AF.Exp, r=[pbk], w=["tE"], scale=-1.0)
                    ACT(tE[:, 256:512], psum[pbk_i][:, 256:512], AF.Exp, r=[pbk], w=["tE"])
                    ACT(tC[:, 0:256], P_[:, 0:256], AF.Silu, r=[pk, "tC"], w=["tC"])
                    A("dve", "scalar_tensor_tensor", r=["tC"], w=["bC"], out=bC[:, 0:256], in0=tC[:, 0:256], scalar=0.125, in1=tC[:, 256:512],
                      op0=ALU.mult, op1=ALU.mult)
                    A("dve", "tensor_tensor", r=["tB", "tE"], w=["bC"], out=bC[:, 256:512], in0=tB[:, 0:256], in1=tE[:, 0:256], op=ALU.mult)
                    A("dve", "tensor_tensor", r=["tB", "tE"], w=["KH%d" % sub], out=KHs[sub][:, :], in0=tB[:, 0:256], in1=tE[:, 256:512], op=ALU.mult)
                    p3, p3k = PA()
                    for q4 in range(4):
                        TR(psum_b[p3][:, q4 * 128:(q4 + 1) * 128], bC[:, q4 * 128:(q4 + 1) * 128], identb[:], r=["bC", "identb"], w=[p3k])
                    any_copy(HQ[:, :, tsl], psum_b[p3][:, 0:256].rearrange("p (a b) -> p a b", b=128), r=[p3k], w=["HQ"], psum_src=True)
                    any_copy(HK[:, :, tsl], psum_b[p3][:, 256:512].rearrange("p (a b) -> p a b", b=128), r=[p3k], w=["HK"], psum_src=True)
                elif gi == 4:
                    any_copy(VB[:, sub, :], P_[:, 0:256], r=[pk], w=["VB"], psum_src=True)
                    ACT(bC[:, 0:256], P_[:, 256:512], AF.Silu, r=[pk], w=["bC"])
                    p3, p3k = PA()
                    for q4 in range(2):
                        TR(psum_b[p3][:, q4 * 128:(q4 + 1) * 128], bC[:, q4 * 128:(q4 + 1) * 128], identb[:], r=["bC", "identb"], w=[p3k])
                    any_copy(HG[:, :, tsl], psum_b[p3][:, 0:256].rearrange("p (a b) -> p a b", b=128), r=[p3k], w=["HG"], psum_src=True)
                else:
                    dstv = tD[:, 0:256].rearrange("p (a b) -> p a b", b=64)
                    headnorm(P_[:, 0:256].rearrange("p (a b) -> p a b", b=64), pk, 4, gmq, "g64_mem_q_norm", dstv, "tD")
                    A("dve", "tensor_copy", r=["tD"], w=["bC"], out=bC[:, 0:256], in_=tD[:, 0:256])
                    p3, p3k = PA()
                    for pm in range(2):
                        TR(psum_b[p3][:, pm * 128:(pm + 1) * 128], bC[:, pm * 128:(pm + 1) * 128], identb[:], r=["bC", "identb"], w=[p3k])
                    any_copy(QmT[:, :, tsl], psum_b[p3][:, 0:256].rearrange("p (a b) -> p a b", b=128), r=[p3k], w=["QmT"], psum_src=True)

        if stop_after == "proj":
            continue
        for kv, nm in ((0, "cmp_w1_k"), (1, "cmp_w1_v")):
            DMA(winb[:, :, :].rearrange("p a b -> p (a b)").rearrange("p (a b) -> p a b", b=256),
                DAP(ws_[nm], 0, [[256, 128], [128 * 256, 16], [1, 256]]), r=["scr_" + nm], w=["winb"], key="winb")
            w1v_ = winb[:, :, :].rearrange("p a b -> p (a b)").rearrange("p (a b) -> p a b", b=256)
            R_ = Rk if kv == 0 else Rv
            ph, phk = PA()
            for g in range(2):
                Rkey = ("Rk%d" if kv == 0 else "Rv%d") % g
                for hc_ in range(2):
                    col = (g * 2 + hc_) * 32
                    for a in range(16):
                        MM(psum[ph][:, col:col + 32], w1v_[:, a, hc_ * 128:(hc_ + 1) * 128], R_[g][:, 2 * a:2 * a + 16 * 31 + 1:16],
                           a == 0, a == 15, r=["winb", Rkey], w=[phk])
            for g in range(2):
                for hc_ in range(2):
                    col = (g * 2 + hc_) * 32
                    ACT(hidT[:, kv, g, hc_, :], psum[ph][:, col:col + 32], AF.Silu, r=[phk, "b1"], w=["hidT"],
                        bias=b1[:, kv * 2 + hc_:kv * 2 + hc_ + 1])
            if kv == 0:
                pk_i, pkk = PA()
                for g in range(2):
                    for hc_ in range(2):
                        MM(psum[pk_i][g * 64:(g + 1) * 64, 0:32], w2k[:, hc_, :], hidT[:, 0, g, hc_, :], hc_ == 0, hc_ == 1,
                           r=["w2k", "hidT"], w=[pkk], tile_position=(0, g * 64))
                A("dve", "tensor_copy", r=[pkk], w=["tD"], out=tD[:, 0:32], in_=psum[pk_i][:, 0:32])
                A("dve", "tensor_tensor", r=["tD"], w=["bC"], out=bC[:, 0:32], in0=tD[:, 0:32], in1=tD[:, 0:32], op=ALU.mult)
                pn, pnk = PA()
                MM(psum[pn][:, 0:32], bd64[:], bC[:, 0:32], True, True, r=["bd64", "bC"], w=[pnk])
                ACT(tD[:, 32:64], psum[pn][:, 0:32], AF.Ln, r=[pnk, "tD"], w=["tD"], bias=eps_col[:, 0:1])
                ACT(tD[:, 32:64], tD[:, 32:64], AF.Exp, r=["tD"], w=["tD"], scale=-0.5)
                A("dve", "scalar_tensor_tensor", r=["tD", "gkcol"], w=["KcT"], out=KcT[:, 32 * ti:32 * ti + 32], in0=tD[:, 0:32],
                  scalar=gkcol[:, 0:1], in1=tD[:, 32:64], op0=ALU.mult, op1=ALU.mult)
            else:
                pv_i, pvk = PA()
                base = 32 * (ti % 4)
                for g in range(2):
                    for hc_ in range(2):
                        MM(psum[pv_i][base:base + 32, g * 64:(g + 1) * 64], hidT[:, 1, g, hc_, :], w2v[:, hc_, :], hc_ == 0, hc_ == 1,
                           r=["w2v", "hidT"], w=[pvk], tile_position=(0, base))
                cch = ti // 4
                A("dve", "tensor_copy", r=[pvk], w=["Vc"], out=Vc[base:base + 32, cch, 0:64], in_=psum[pv_i][base:base + 32, 0:64])
                A("dve", "tensor_copy", r=[pvk], w=["Vc"], out=Vc[base:base + 32, cch, 128:192], in_=psum[pv_i][base:base + 32, 64:128])
                A("pool", "memset", r=[], w=["Vc"], ap=Vc[base:base + 32, cch, 64:128], constant=1.0)
                if ti == 0:
                    A("pool", "memset", r=[], w=["Vc"], ap=Vc[0:1, 0, :], constant=0.0)
        for g in range(2):
            A("pool", "tensor_copy", r=["Rk%d" % g], w=["Rk%d" % g], out=Rk[g][:, 0:16], in_=Rk[g][:, 512:528])
            A("pool", "tensor_copy", r=["Rv%d" % g], w=["Rv%d" % g], out=Rv[g][:, 0:16], in_=Rv[g][:, 512:528])

        if stop_after == "compress":
            continue
        def combine(ob, obk, g, dst, gate_idx, first, guard=False):
            vs, ds = g * 64, 64 - g * 64
            if guard:
                A("dve", "tensor_scalar", r=[obk], w=["tA"], out=tA[ds:ds + 64, :], in0=psum[ob][ds:ds + 64, :], scalar1=1e-30, scalar2=None, op0=ALU.max)
                A("dve", "reciprocal", r=["tA"], w=["tA"], out=tA[ds:ds + 64, :], in_=tA[ds:ds + 64, :])
            else:
                A("dve", "reciprocal", r=[obk], w=["tA"], out=tA[ds:ds + 64, :], in_=psum[ob][ds:ds + 64, :])
            if gate_idx is None:
                A("dve", "tensor_tensor", r=[obk, "tA"], w=["YM"], out=dst, in0=psum[ob][vs:vs + 64, :], in1=tA[ds:ds + 64, :], op=ALU.mult)
                return
            A("dve", "tensor_tensor", r=[obk, "tA"], w=["tB"], out=tB[vs:vs + 64, :], in0=psum[ob][vs:vs + 64, :], in1=tA[ds:ds + 64, :], op=ALU.mult)
            pi, pk = PA()
            MM(psum[pi][vs:vs + 64, :], selg[:, gate_idx, :], GT[:, :], True, True, r=["selg", "GT"], w=[pk])
            if first:
                A("dve", "tensor_tensor", r=["tB", pk], w=["YT"], out=dst, in0=tB[vs:vs + 64, :], in1=psum[pi][vs:vs + 64, :], op=ALU.mult)
            else:
                A("dve", "tensor_tensor", r=["tB", pk], w=["tC"], out=tC[vs:vs + 64, :], in0=tB[vs:vs + 64, :], in1=psum[pi][vs:vs + 64, :], op=ALU.mult)
                A("pool", "tensor_tensor", r=["tC", "YT"], w=["YT"], out=dst, in0=dst, in1=tC[vs:vs + 64, :], op=ALU.add)

        def v_lhsT(cache, pitch_, vcol, o_start, o_end, g):
            if g == 0:
                return bass.AP(cache, vcol, [[pitch_, 128], [o_end - vcol, 2], [1, 64]])
            return bass.AP(cache, o_start, [[pitch_, 128], [vcol - o_start, 2], [1, 64]])

        nch_c = ti // 4 + 1
        for j in range(4):
            for g in range(2):
                gs = slice(g * 64, (g + 1) * 64)
                ob, obk = PB()
                ets = []
                for c in range(nch_c):
                    last = (c == nch_c - 1)
                    pi, pk = PA()
                    MM(psum[pi][:, :], KcT[gs, c * 128:(c + 1) * 128], QT[gs, j, :], True, not last, r=["KcT", "QT"], w=[pk])
                    if last:
                        MM(psum[pi][:, :], identb[:], cmpb[:, ti % 4, :], False, True, r=["identb", "cmpb"], w=[pk], force=True)
                    et, etk = next_et()
                    ACT(et[:, :], psum[pi][:, :], AF.Exp, r=[pk], w=[etk], scale=0.125)
                    ets.append((et, etk))
                    lh = Vc[:, c, g * 64:g * 64 + 128]
                    MM(psum[ob][:, :], lh, et[:, :], c == 0, last, r=["Vc", etk], w=[obk])
                for sp in range(2):
                    pi, pk = PA()
                    for s2 in range(2):
                        sub = sp * 2 + s2
                        for c in range(nch_c):
                            et, etk = ets[c]
                            MM(psum[pi][:, s2 * 130:(s2 + 1) * 130], et[:, sub * 128:(sub + 1) * 128], ovl[:, c, :], c == 0, c == nch_c - 1,
                               r=[etk, "ovl"], w=[pk])
                    for s2 in range(2):
                        sub = sp * 2 + s2
                        A("dve", "tensor_scalar", r=[pk], w=["sm1"], out=sm1[:, 0:1], in0=psum[pi][:, s2 * 130 + 128:s2 * 130 + 129],
                          scalar1=1e-30, scalar2=None, op0=ALU.max)
                        A("dve", "reciprocal", r=["sm1"], w=["sm1"], out=sm1[:, 0:1], in_=sm1[:, 0:1])
                        if j == 0:
                            A("dve", "tensor_scalar", r=[pk, "sm1"], w=["impS"], out=impS[:, g, sub, :], in0=psum[pi][:, s2 * 130:s2 * 130 + 128],
                              scalar1=sm1[:, 0:1], scalar2=None, op0=ALU.mult)
                        else:
                            A("dve", "scalar_tensor_tensor", r=[pk, "sm1", "impS"], w=["impS"], out=impS[:, g, sub, :],
                              in0=psum[pi][:, s2 * 130:s2 * 130 + 128], scalar=sm1[:, 0:1], in1=impS[:, g, sub, :], op0=ALU.mult, op1=ALU.add)
                combine(ob, obk, g, YT[gs, j, :], (g * 4 + j) * 3 + 0, True, guard=True)

        if stop_after == "C":
            continue
        nm_ = (4 * ti + 3) // 16 + 1
        for g in range(2):
            for sub in range(4):
                off = 126 - 2 * (ti * 4 + sub)
                A("dve", "tensor_tensor", r=["impS", "keepw"], w=["tk1"], out=tk1[:, :], in0=impS[:, g, sub, :], in1=keepw[:, off:off + 128], op=ALU.mult)
                A("dve", "tensor_tensor", r=["tk1", "addw"], w=["tk1"], out=tk1[:, :], in0=tk1[:, :], in1=addw[:, off:off + 128], op=ALU.add)
                A("dve", "memset", r=["tk1"], w=["tk1"], ap=tk1[:, 0:1], constant=1e4)
                A("dve", "max", r=["tk1"], w=["tkm"], out=tkm[:, 0:8], in_=tk1[:, :])
                A("dve", "match_replace", r=["tk1", "tkm"], w=["tk2"], out=tk2[:, :], in_to_replace=tkm[:, 0:8], in_values=tk1[:, :], imm_value=-2.0)
                A("dve", "max", r=["tk2", "tkm"], w=["tkm"], out=tkm[:, 8:16], in_=tk2[:, :])
                A("dve", "tensor_scalar", r=["tk1", "tkm"], w=["negm"], out=negm[:, :], in0=tk1[:, :], scalar1=tkm[:, 15:16], scalar2=NEG,
                  op0=ALU.is_lt, op1=ALU.mult)
                pi, pk = PA()
                for m in range(nm_):
                    TR(psum_b[pi][0:32, m * 128:(m + 1) * 128], negm[:, m * 32:(m + 1) * 32], identb[:], r=["negm", "identb"], w=[pk])
                any_copy(NMT[:, g, 0:nm_, sub * 128:(sub + 1) * 128], psum_b[pi][0:32, 0:nm_ * 128].rearrange("p (a b) -> p a b", b=128),
                         r=[pk], w=["NMT"], psum_src=True)

        if stop_after == "K":
            continue
        wpitch = 8 * 128 + 128
        for j in range(4):
            for g in range(2):
                gs = slice(g * 64, (g + 1) * 64)
                ob, obk = PB()
                rs = [r for r in (-1, 0, -4, -3, -2, 1, 2, 3) if 4 * ti + r >= 0]
                pend = None
                for idx, r in enumerate(rs):
                    c = 4 * ti + r
                    slot, within = (c // 4) % 2, c % 4
                    kl = KTw[gs, slot, within * 128:(within + 1) * 128]
                    if r < 0:
                        qa, qb, bq, bias, bkey = 0, 128 * (r + 5), 128 * (r + 4), winbias, "winbias"
                    else:
                        qa, qb, bq, bias, bkey = 128 * r, 512, 128 * r, causb, "causb"
                    pi, pk = PA()
                    MM(psum[pi][:, bq:bq + 128], kl, QT[gs, j, bq:bq + 128], True, False, r=["KTw", "QT"], w=[pk])
                    MM(psum[pi][:, bq:bq + 128], identb[:], bias[:], False, True, r=["identb", bkey], w=[pk], force=True)
                    if r < 0 and bq > 0:
                        MM(psum[pi][:, 0:bq], kl, QT[gs, j, 0:bq], True, True, r=["KTw", "QT"], w=[pk], force=True)
                    if r >= 0 and bq + 128 < 512:
                        MM(psum[pi][:, bq + 128:512], kl, QT[gs, j, bq + 128:512], True, True, r=["KTw", "QT"], w=[pk], force=True)
                    et, etk = next_et()
                    ACT(et[:, qa:qb], psum[pi][:, qa:qb], AF.Exp, r=[pk], w=[etk], scale=0.125)
                    lh = Vw[:, slot * 4 + within, g * 64:g * 64 + 128]
                    if pend is not None:
                        MM(*pend[0], **pend[1])
                    pend = ((psum[ob][:, qa:qb], lh, et[:, qa:qb], idx == 0, idx == len(rs) - 1), dict(r=["Vw", etk], w=[obk]))
                MM(*pend[0], **pend[1])
                combine(ob, obk, g, YT[gs, j, :], (g * 4 + j) * 3 + 2, False)

        if stop_after == "W":
            continue
        for hm in range(4):
            pm, hh = hm // 2, hm % 2
            hs = slice(hh * 64, hh * 64 + 64)
            ob, obk = PB()
            for c in range(2):
                pi, pk = PA()
                MM(psum[pi][:, :], KmT[hs, pm, c * 128:(c + 1) * 128], QmT[hs, pm, :], True, True, r=["KmT", "QmT"], w=[pk])
                et, etk = next_et()
                ACT(et[:, :], psum[pi][:, :], AF.Exp, r=[pk], w=[etk], scale=0.125)
                lh = Vm[:, c, pm, hh * 64:hh * 64 + 128]
                MM(psum[ob][:, :], lh, et[:, :], c == 0, c == 1, r=["Vm", etk], w=[obk])
            combine(ob, obk, hh, YM[hs, pm, :], None, True)

        if stop_after == "M":
            continue
        A("pool", "tensor_copy", r=["STb"], w=["STb"], out=STb[:, 0, :, :], in_=STb[:, 8, :, :])
        pos_ = [PB(), PB()]
        for sub in range(4):
            pU, pUk = PA()
            for ch in range(2):
                for pp in range(2):
                    col = ch * 2 + pp
                    MM(psum[pU][:, col * 128:(col + 1) * 128], KHs[sub][ch * 64:(ch + 1) * 64, pp * 128:(pp + 1) * 128],
                       VB[ch * 64:(ch + 1) * 64, sub, pp * 128:(pp + 1) * 128], True, True, r=["KH%d" % sub, "VB"], w=[pUk], force=True)
            pa_, pak = PA()
            for ch in range(2):
                c = sub * 2 + ch
                for pp in range(2):
                    for hh in range(2):
                        hs = slice(hh * 64, hh * 64 + 64)
                        MM(psum[pa_][ch * 64:(ch + 1) * 64, (pp * 2 + hh) * 64:(pp * 2 + hh + 1) * 64], HK[hs, pp, c * 64:(c + 1) * 64],
                           HQ[hs, pp, c * 64:(c + 1) * 64], True, True, r=["HK", "HQ"], w=[pak], force=True)
            A("dve", "tensor_tensor", r=[pak, "hmask"], w=["AT"], out=AT[:, sub, :].rearrange("p (a b) -> p a b", b=64),
              in0=psum[pa_][:, 0:256].rearrange("p (a b) -> p a b", b=64), in1=hmask[:, :].unsqueeze(1).broadcast_to([128, 4, 64]), op=ALU.mult)
            for ch in range(2):
                c = sub * 2 + ch
                for pp in range(2):
                    for hh in range(2):
                        hs = slice(hh * 64, hh * 64 + 64)
                        cc_ = slice(c * 64, (c + 1) * 64)
                        MM(psum[pos_[pp][0]][hs, cc_], STb[hs, c, pp, :], HQ[hs, pp, cc_], True, False, r=["STb", "HQ"], w=[pos_[pp][1]], force=True)
                        MM(psum[pos_[pp][0]][hs, cc_], VB[ch * 64:(ch + 1) * 64, sub, pp * 128 + hh * 64:pp * 128 + hh * 64 + 64],
                           AT[ch * 64:(ch + 1) * 64, sub, (pp * 2 + hh) * 64:(pp * 2 + hh + 1) * 64], False, True, r=["VB", "AT"], w=[pos_[pp][1]], force=True)
                for pp in range(2):
                    col = ch * 2 + pp
                    for hh in range(2):
                        hs = slice(hh * 64, hh * 64 + 64)
                        A("dve", "scalar_tensor_tensor", r=["ST", pUk, "dec"], w=["ST"], out=ST[hs, pp, :], in0=ST[hs, pp, :],
                          scalar=dec[hs, sub, col * 2:col * 2 + 1], in1=psum[pU][hs, col * 128 + hh * 64:col * 128 + hh * 64 + 64],
                          op0=ALU.mult, op1=ALU.add)
                A("pool", "tensor_copy", r=["ST", "STb"], w=["STb"], out=STb[:, c + 1, :, :], in_=ST[:, :, :])
        for pp in range(2):
            ob, obk = pos_[pp]
            ACT(bA[:, :], psum[ob][:, :], AF.Square, r=[obk], w=["bA"])
            pn, pnk = PA()
            MM(psum[pn][:, :], bd64[:], bA[:, :], True, True, r=["bd64", "bA"], w=[pnk])
            ACT(tA[:, :], psum[pn][:, :], AF.Ln, r=[pnk], w=["tA"], bias=eps_col[:, 0:1])
            ACT(tA[:, :], tA[:, :], AF.Exp, r=["tA"], w=["tA"], scale=-0.5)
            A("dve", "tensor_tensor", r=[obk, "tA"], w=["tB"], out=tB[:, :], in0=psum[ob][:, :], in1=tA[:, :], op=ALU.mult)
            A("dve", "scalar_tensor_tensor", r=["tB", "ghg", "HG"], w=["hT"], out=hT[:, 4 + pp, :], in0=tB[:, :], scalar=ghg[:, pp:pp + 1],
              in1=HG[:, pp, :], op0=ALU.mult, op1=ALU.mult)

        if stop_after == "H":
            continue
        spitch = NCH * 128 + 128
        nchs = 4 * ti + 4
        for j in range(4):
            for g in range(2):
                gs = slice(g * 64, (g + 1) * 64)
                ob, obk = PB()
                pend = None
                for c in range(nchs):
                    m, cc = c // 16, c % 16
                    r = c - 4 * ti
                    kl = KTs[gs, c * 128:(c + 1) * 128]
                    pi, pk = PA()
                    if r < 0:
                        qa = 0
                        MM(psum[pi][:, :], kl, QT[gs, j, :], True, False, r=["KTs", "QT"], w=[pk])
                        MM(psum[pi][:, :], esel[:, cc, :], NMT[:, g, m, :], False, True, r=["esel", "NMT"], w=[pk], force=True)
                    else:
                        qa = 128 * r
                        MM(psum[pi][:, qa:qa + 128], kl, QT[gs, j, qa:qa + 128], True, False, r=["KTs", "QT"], w=[pk])
                        MM(psum[pi][:, qa:qa + 128], esel[:, cc, :], NMT[:, g, m, qa:qa + 128], False, False, r=["esel", "NMT"], w=[pk], force=True)
                        MM(psum[pi][:, qa:qa + 128], identb[:], causb[:], False, True, r=["identb", "causb"], w=[pk], force=True)
                        if qa + 128 < 512:
                            MM(psum[pi][:, qa + 128:512], kl, QT[gs, j, qa + 128:512], True, False, r=["KTs", "QT"], w=[pk], force=True)
                            MM(psum[pi][:, qa + 128:512], esel[:, cc, :], NMT[:, g, m, qa + 128:512], False, True, r=["esel", "NMT"], w=[pk], force=True)
                    et, etk = next_et()
                    ACT(et[:, qa:512], psum[pi][:, qa:512], AF.Exp, r=[pk], w=[etk], scale=0.125)
                    lh = Vs[:, c, g * 64:g * 64 + 128]
                    if pend is not None:
                        MM(*pend[0], **pend[1])
                    pend = ((psum[ob][:, qa:512], lh, et[:, qa:512], c == 0, c == nchs - 1), dict(r=["Vs", etk], w=[obk]))
                MM(*pend[0], **pend[1])
                combine(ob, obk, g, YT[gs, j, :], (g * 4 + j) * 3 + 1, False)

        if stop_after == "S":
            continue
        pn, pnk = PA()
        for j in range(4):
            bx, bxk = (bA, "bA") if j % 2 == 0 else (bB, "bB")
            ACT(bx[:, :], YT[:, j, :], AF.Square, r=["YT"], w=[bxk])
            MM(psum[pn][:, :], onesb[:], bx[:, :], j == 0, j == 3, r=["onesb", bxk], w=[pnk])
        ACT(tA[:, :], psum[pn][:, :], AF.Ln, r=[pnk], w=["tA"], scale=1.0 / 512, bias=eps_col[:, 0:1])
        ACT(tA[:, :], tA[:, :], AF.Exp, r=["tA"], w=["tA"], scale=-0.5)
        for j in range(4):
            A("dve", "scalar_tensor_tensor", r=["YT", "gnsa", "tA"], w=["hT"], out=hT[:, j, :], in0=YT[:, j, :], scalar=gnsa[:, j:j + 1],
              in1=tA[:, :], op0=ALU.mult, op1=ALU.mult)
        pn, pnk = PA()
        for pm in range(2):
            bx, bxk = (bA, "bA") if pm % 2 == 0 else (bB, "bB")
            ACT(bx[:, :], YM[:, pm, :], AF.Square, r=["YM"], w=[bxk])
            MM(psum[pn][:, :], onesb[:], bx[:, :], pm == 0, pm == 1, r=["onesb", bxk], w=[pnk])
        ACT(tA[:, :], psum[pn][:, :], AF.Ln, r=[pnk], w=["tA"], scale=1.0 / 256, bias=eps_col[:, 0:1])
        ACT(tA[:, :], tA[:, :], AF.Exp, r=["tA"], w=["tA"], scale=-0.5)
        for pm in range(2):
            A("dve", "scalar_tensor_tensor", r=["YM", "gmo", "tA"], w=["hT"], out=hT[:, 6 + pm, :], in0=YM[:, pm, :], scalar=gmo[:, pm:pm + 1],
              in1=tA[:, :], op0=ALU.mult, op1=ALU.mult)
        if dbg and "mixT" in dbg:
            for c8 in range(8):
                A("dve", "tensor_copy", r=["hT"], w=["tD"], out=tD[:, :], in_=hT[:, c8, :])
                DMA(DAP(dbg_d["mixT"], c8 * 128 * S + t0, [[S, 128], [1, T]]), tD[:, :], r=["tD"], w=[], key="dbg_mixT", is_out=True)

        if stop_after == "norm":
            continue
        def wout_src(c):
            if c < 4:
                return [(slice(g * 64, g * 64 + 64), DAP(ws_["w_out"], ((g * 4 + c) * 64) * D, [[D, 64], [1, D]])) for g in range(2)]
            return [(slice(0, 128), DAP(ws_["w_out"], (512 + (c - 4) * 128) * D, [[D, 128], [1, D]]))]
        down_like(hT, "hT", 8, wout_src, "scr_w_out", 1.0)
        if dbg and "x2" in dbg:
            DMA(DAP(dbg_d["x2"], t0 * D, [[D, 128], [128 * D, 4], [1, D]]), xt[:, :, :], r=["xt"], w=[], key="dbg_x2", is_out=True)
        ffn("ffn2", 2)
        DMA(DAP(out_d, t0 * D, [[D, 128], [128 * D, 4], [1, D]]), xt[:, :, :], r=["xt"], w=[], key="out", is_out=True)

    Sd.finish()
    with nc.allow_non_contiguous_dma(reason="small constant / layout loads"):
        Sd.emit(st)
    return nc, st


_CACHE = {}


def _get(S):
    if S not in _CACHE:
        _CACHE[S] = build(S)
    return _CACHE[S]


def make_in_map(inputs, b, S):
    m = {"x": np.ascontiguousarray(inputs["x"][b]), "mem": np.ascontiguousarray(inputs["mem"][b])}
    for k in ("ffn1_w_gate", "ffn1_w_up", "ffn1_w_down", "ffn2_w_gate", "ffn2_w_up", "ffn2_w_down", "w_in", "w_out",
              "cmp_w1_k", "cmp_w1_v", "mem_w_k", "mem_w_v", "ffn1_norm", "mix_norm", "ffn2_norm", "mem_norm", "nsa_q_norm",
              "nsa_k_norm", "nsa_out_norm", "hgrn_out_norm", "mem_q_norm", "mem_k_norm", "mem_out_norm",
              "cmp_pos_k", "cmp_pos_v", "cmp_w2_k", "cmp_w2_v"):
        m[k] = np.ascontiguousarray(np.asarray(inputs[k])[0], dtype=np.float32)
    m["hgrn_lb_logits"] = np.ascontiguousarray(inputs["hgrn_lb_logits"], dtype=np.float32)
    w_in_full = m.pop("w_in")
    for gi_, (c0_, gw_) in enumerate(GROUPS):
        m["w_in_g%d" % gi_] = np.ascontiguousarray(w_in_full[:, c0_:c0_ + gw_])
    m.update(host_consts(S))
    return m


def kernel(**inputs):
    x = np.asarray(inputs["x"])
    B, S, _ = x.shape
    nc, _st = _get(S)
    in_maps = [make_in_map(inputs, b, S) for b in range(B)]
    res = run_bass_kernel_spmd(nc, in_maps, core_ids=list(range(B)))
    return np.stack([np.asarray(r["out"]) for r in res.results], axis=0).astype(np.float32)
```
